# Optimizing a Trainium2 kernel written in Bass

```python
import math
import jax, jax.numpy as jnp
from jax import lax
import numpy as np

D_MODEL = 1024
BATCH = 16
SEQ = 2048
DEPTH = 2

HEAD_DIM = 64
DIFF_HEADS = 4
DIFF_V_DIM = 2 * HEAD_DIM
DSA_HEADS = 4
DSA_LATENT = 128
DSA_V_DIM = 64
IDX_HEADS = 8
IDX_DIM = 32
DSA_TOPK_MAX = 256
MOBA_HEADS = 4
MOBA_BLOCK = 256
MOBA_TOPK_MAX = 3
N_BUCKETS = 32
MAX_DISTANCE = 128
N_BIAS_HEADS = DIFF_HEADS + DSA_HEADS + MOBA_HEADS
D_FF = 4 * D_MODEL
Q_BLOCK = 128
MOBA_Q_CHUNK = 32
EPS = 1e-6

A_Q = DIFF_HEADS * 2 * HEAD_DIM
A_K = DIFF_HEADS * 2 * HEAD_DIM
A_V = DIFF_HEADS * DIFF_V_DIM
B_Q = DSA_HEADS * DSA_LATENT
B_KV = DSA_LATENT
B_IQ = IDX_HEADS * IDX_DIM
B_IK = IDX_DIM
B_IW = IDX_HEADS
C_QKV = MOBA_HEADS * HEAD_DIM
G_COLS = 3 * D_MODEL
SPLIT_SIZES = (A_Q, A_K, A_V, B_Q, B_KV, B_IQ, B_IK, B_IW, C_QKV, C_QKV, C_QKV, G_COLS)
IN_COLS = A_Q + A_K + A_V + B_Q + B_KV + B_IQ + B_IK + B_IW + 3 * C_QKV + G_COLS

kernel_name = 'hybrid_gated_diff_dsa_moba_block'


def rmsnorm(x, g):
    xf = x.astype(jnp.float32)
    y = xf * lax.rsqrt(jnp.mean(xf * xf, axis=-1, keepdims=True) + EPS)
    return y.astype(x.dtype) * g


def split_cols(p):
    out = []
    off = 0
    for n in SPLIT_SIZES:
        out.append(p[..., off:off + n])
        off += n
    return out


def t5_bucket(dist):
    max_exact = N_BUCKETS // 2
    n = jnp.maximum(dist, 0)
    nf = jnp.maximum(n, 1).astype(jnp.float32)
    large = max_exact + (jnp.log(nf / max_exact) / math.log(MAX_DISTANCE / max_exact)
                         * (N_BUCKETS - max_exact)).astype(jnp.int32)
    large = jnp.minimum(large, N_BUCKETS - 1)
    return jnp.where(n < max_exact, n, large)


def diff_attention(q, k, v, lam, lam_init, subln_g, bias_a):
    B, S, H = q.shape[0], q.shape[1], q.shape[2]
    E = v.shape[-1]
    kpos = jnp.arange(S)
    scale = HEAD_DIM ** -0.5
    bias_t = bias_a.T

    def block(i):
        q0 = i * Q_BLOCK
        qb = lax.dynamic_slice_in_dim(q, q0, Q_BLOCK, axis=1)
        qpos = q0 + jnp.arange(Q_BLOCK)
        dist = qpos[:, None] - kpos[None, :]
        bias = bias_t[:, t5_bucket(dist)]
        logits = jnp.einsum('bqhmd,bkhmd->bhmqk', qb, k).astype(jnp.float32) * scale + bias[None, :, None]
        logits = jnp.where(dist >= 0, logits, -jnp.inf)
        p = jax.nn.softmax(logits, axis=-1)
        attn = p[:, :, 0] - lam * p[:, :, 1]
        return jnp.einsum('bhqk,bkhe->bqhe', attn.astype(v.dtype), v)

    o = lax.map(block, jnp.arange(S // Q_BLOCK))
    o = jnp.moveaxis(o, 0, 1).reshape(B, S, H, E)
    o = rmsnorm(o, subln_g) * (1.0 - lam_init)
    return o.reshape(B, S, H * E)


def dsa_attention(q_lat, kv_lat, iq, ik, iw, w_uv, bias_b):
    B, S, H, R = q_lat.shape
    topk = min(DSA_TOPK_MAX, S // 4)
    kpos = jnp.arange(S)
    bidx = jnp.arange(B)[:, None, None]
    hidx = jnp.arange(H)[None, None, :, None]
    bias_t = bias_b.T
    idx_scale = (IDX_HEADS ** -0.5) * (IDX_DIM ** -0.5)

    def block(i):
        q0 = i * Q_BLOCK
        qpos = q0 + jnp.arange(Q_BLOCK)
        qb = lax.dynamic_slice_in_dim(q_lat, q0, Q_BLOCK, axis=1)
        iqb = lax.dynamic_slice_in_dim(iq, q0, Q_BLOCK, axis=1)
        iwb = lax.dynamic_slice_in_dim(iw, q0, Q_BLOCK, axis=1)
        causal = qpos[:, None] >= kpos[None, :]
        s_idx = jax.nn.relu(jnp.einsum('bqhd,bkd->bqhk', iqb, ik))
        score = jnp.einsum('bqhk,bqh->bqk', s_idx, iwb).astype(jnp.float32) * idx_scale
        score = jnp.where(causal[None], score, -jnp.inf)
        _, sel = lax.top_k(score, topk)
        dist = qpos[None, :, None] - sel
        valid = dist >= 0
        kv_sel = kv_lat[bidx, sel]
        logits = jnp.einsum('bqhr,bqkr->bqhk', qb, kv_sel).astype(jnp.float32) * (R ** -0.5)
        bias = bias_t[hidx, t5_bucket(dist)[:, :, None, :]]
        logits = jnp.where(valid[:, :, None, :], logits + bias, -jnp.inf)
        p = jax.nn.softmax(logits, axis=-1)
        return jnp.einsum('bqhk,bqkr->bqhr', p.astype(kv_sel.dtype), kv_sel)

    o = lax.map(block, jnp.arange(S // Q_BLOCK))
    o = jnp.moveaxis(o, 0, 1).reshape(B, S, H, R)
    o = jnp.einsum('bshr,hre->bshe', o, w_uv)
    return o.reshape(B, S, H * DSA_V_DIM)


def moba_attention(q, k, v, bias_c):
    B, S, H, Dh = q.shape
    nb = -(-S // MOBA_BLOCK)
    sp = nb * MOBA_BLOCK
    pad = ((0, 0), (0, sp - S), (0, 0), (0, 0))
    k_bh = jnp.pad(k, pad).reshape(B, nb, MOBA_BLOCK, H, Dh).transpose(0, 3, 1, 2, 4)
    v_bh = jnp.pad(v, pad).reshape(B, nb, MOBA_BLOCK, H, Dh).transpose(0, 3, 1, 2, 4)
    k_mean = jnp.mean(k_bh, axis=3)
    topb = min(MOBA_TOPK_MAX, nb)
    bias_t = bias_c.T
    bi = jnp.arange(B)[:, None, None, None]
    hi = jnp.arange(H)[None, :, None, None]
    hi5 = jnp.arange(H)[None, :, None, None, None]
    blk_ids = jnp.arange(nb)
    in_blk = jnp.arange(MOBA_BLOCK)
    scale = Dh ** -0.5

    def chunk(i):
        q0 = i * MOBA_Q_CHUNK
        qpos = q0 + jnp.arange(MOBA_Q_CHUNK)
        j = q0 // MOBA_BLOCK
        qc = lax.dynamic_slice_in_dim(q, q0, MOBA_Q_CHUNK, axis=1).transpose(0, 2, 1, 3)
        gate = jnp.einsum('bhqd,bhnd->bhqn', qc, k_mean).astype(jnp.float32)
        gate = jnp.where(blk_ids < j, gate, -jnp.inf)
        _, sel = lax.top_k(gate, topb)
        sel_valid = sel < j
        k_sel = k_bh[bi, hi, sel]
        v_sel = v_bh[bi, hi, sel]
        lp = jnp.einsum('bhqd,bhqnkd->bhqnk', qc, k_sel).astype(jnp.float32) * scale
        dist_p = qpos[None, None, :, None, None] - (sel[..., None] * MOBA_BLOCK + in_blk)
        lp = lp + bias_t[hi5, t5_bucket(dist_p)]
        lp = jnp.where(sel_valid[..., None], lp, -jnp.inf).reshape(B, H, MOBA_Q_CHUNK, topb * MOBA_BLOCK)
        k_own = lax.dynamic_slice_in_dim(k_bh, j, 1, axis=2)[:, :, 0]
        v_own = lax.dynamic_slice_in_dim(v_bh, j, 1, axis=2)[:, :, 0]
        lo = jnp.einsum('bhqd,bhkd->bhqk', qc, k_own).astype(jnp.float32) * scale
        dist_o = qpos[:, None] - (j * MOBA_BLOCK + in_blk)[None, :]
        lo = jnp.where(dist_o >= 0, lo + bias_t[:, t5_bucket(dist_o)][None], -jnp.inf)
        p = jax.nn.softmax(jnp.concatenate([lp, lo], axis=-1), axis=-1)
        pp = p[..., :topb * MOBA_BLOCK].reshape(B, H, MOBA_Q_CHUNK, topb, MOBA_BLOCK)
        po = p[..., topb * MOBA_BLOCK:]
        o = (jnp.einsum('bhqnk,bhqnkd->bhqd', pp.astype(v.dtype), v_sel)
             + jnp.einsum('bhqk,bhkd->bhqd', po.astype(v.dtype), v_own))
        return o.transpose(0, 2, 1, 3)

    o = lax.map(chunk, jnp.arange(S // MOBA_Q_CHUNK))
    return jnp.moveaxis(o, 0, 1).reshape(B, S, H * Dh)


def setup_inputs(seed: int = 0) -> dict:
    key = jax.random.key(seed)
    ks = jax.random.split(key, 20)

    def nrm(k, shape, s):
        return jax.random.normal(k, shape, jnp.float32) * s

    return {
        'x': nrm(ks[0], (BATCH, SEQ, D_MODEL), 1.0),
        'c': nrm(ks[1], (BATCH, D_MODEL), 1.0),
        'rel_bias': nrm(ks[2], (N_BUCKETS, N_BIAS_HEADS), 0.5),
        'ada_w': nrm(ks[3], (DEPTH, D_MODEL, 6 * D_MODEL), D_MODEL ** -0.5),
        'ada_b': nrm(ks[4], (DEPTH, 6 * D_MODEL), 0.02),
        'norm_mix': 1.0 + nrm(ks[5], (DEPTH, D_MODEL), 0.02),
        'w_in': nrm(ks[6], (DEPTH, D_MODEL, IN_COLS), D_MODEL ** -0.5),
        'gate_b': nrm(ks[7], (DEPTH, G_COLS), 0.02),
        'diff_lambda': nrm(ks[8], (DEPTH, 4, HEAD_DIM), 0.1),
        'diff_subln': 1.0 + nrm(ks[9], (DEPTH, DIFF_V_DIM), 0.02),
        'dsa_kv_norm': 1.0 + nrm(ks[10], (DEPTH, DSA_LATENT), 0.02),
        'dsa_w_uv': nrm(ks[11], (DEPTH, DSA_HEADS, DSA_LATENT, DSA_V_DIM), DSA_LATENT ** -0.5),
        'w_br_a': nrm(ks[12], (DEPTH, A_V, D_MODEL), A_V ** -0.5),
        'w_br_b': nrm(ks[13], (DEPTH, DSA_HEADS * DSA_V_DIM, D_MODEL), (DSA_HEADS * DSA_V_DIM) ** -0.5),
        'w_br_c': nrm(ks[14], (DEPTH, C_QKV, D_MODEL), C_QKV ** -0.5),
        'w_o': nrm(ks[15], (DEPTH, D_MODEL, D_MODEL), D_MODEL ** -0.5),
        'norm_mlp': 1.0 + nrm(ks[16], (DEPTH, D_MODEL), 0.02),
        'w_ff1': nrm(ks[17], (DEPTH, D_MODEL, D_FF), D_MODEL ** -0.5),
        'w_ff2': nrm(ks[18], (DEPTH, D_FF, D_MODEL), D_FF ** -0.5),
        'norm_final': 1.0 + nrm(ks[19], (D_MODEL,), 0.02),
    }


def reference(x, c, rel_bias, ada_w, ada_b, norm_mix, w_in, gate_b, diff_lambda, diff_subln,
              dsa_kv_norm, dsa_w_uv, w_br_a, w_br_b, w_br_c, w_o, norm_mlp, w_ff1, w_ff2,
              norm_final):
    B, S, _ = x.shape
    bias_tab = rel_bias.astype(jnp.float32)
    bias_a = bias_tab[:, :DIFF_HEADS]
    bias_b = bias_tab[:, DIFF_HEADS:DIFF_HEADS + DSA_HEADS]
    bias_c = bias_tab[:, DIFF_HEADS + DSA_HEADS:]
    cond = jax.nn.silu(c)
    for l in range(DEPTH):
        mod = (cond @ ada_w[l] + ada_b[l]).reshape(B, 6, D_MODEL)[:, :, None, :]
        shift1, scale1, gate1 = mod[:, 0], mod[:, 1], mod[:, 2]
        shift2, scale2, gate2 = mod[:, 3], mod[:, 4], mod[:, 5]

        u = rmsnorm(x, norm_mix[l]) * (1.0 + scale1) + shift1
        aq, ak, av, bq, bkv, biq, bik, biw, cq, ck, cv, g = split_cols(u @ w_in[l])

        lam_init = 0.8 - 0.6 * math.exp(-0.3 * l)
        dl = diff_lambda[l].astype(jnp.float32)
        lam = jnp.exp(jnp.sum(dl[0] * dl[1])) - jnp.exp(jnp.sum(dl[2] * dl[3])) + lam_init
        y_a = diff_attention(aq.reshape(B, S, DIFF_HEADS, 2, HEAD_DIM),
                             ak.reshape(B, S, DIFF_HEADS, 2, HEAD_DIM),
                             av.reshape(B, S, DIFF_HEADS, DIFF_V_DIM),
                             lam, lam_init, diff_subln[l], bias_a) @ w_br_a[l]
        y_b = dsa_attention(bq.reshape(B, S, DSA_HEADS, DSA_LATENT),
                            rmsnorm(bkv, dsa_kv_norm[l]),
                            biq.reshape(B, S, IDX_HEADS, IDX_DIM), bik, biw,
                            dsa_w_uv[l], bias_b) @ w_br_b[l]
        y_c = moba_attention(cq.reshape(B, S, MOBA_HEADS, HEAD_DIM),
                             ck.reshape(B, S, MOBA_HEADS, HEAD_DIM),
                             cv.reshape(B, S, MOBA_HEADS, HEAD_DIM), bias_c) @ w_br_c[l]
        gates = jax.nn.sigmoid(g + gate_b[l]).reshape(B, S, 3, D_MODEL)
        merged = gates[:, :, 0] * y_a + gates[:, :, 1] * y_b + gates[:, :, 2] * y_c
        x = x + gate1 * (merged @ w_o[l])

        u2 = rmsnorm(x, norm_mlp[l]) * (1.0 + scale2) + shift2
        h = jnp.square(jax.nn.relu(u2 @ w_ff1[l]))
        x = x + gate2 * (h @ w_ff2[l])
    return rmsnorm(x, norm_final)
```

```python
import contextlib
import math
import numpy as np
import concourse.bass as bass
import concourse.mybir as mybir
from concourse.bass_utils import run_bass_kernel_spmd

F32 = mybir.dt.float32
BF16 = mybir.dt.bfloat16
U32 = mybir.dt.uint32
AF = mybir.ActivationFunctionType
ALU = mybir.AluOpType
AX = mybir.AxisListType

S = 2048
D = 1024
NEG = -30000.0
EPS = 1e-6
O_AQ, O_AK, O_AV, O_BQ, O_BKV, O_BIQ, O_BIK, O_BIW, O_CQ, O_CK, O_CV, O_G = (
    0, 512, 1024, 1536, 2048, 2176, 2432, 2464, 2472, 2728, 2984, 3240)
IN_COLS = 6312
BIS_ITERS = 13
SAME_DIST = 1 << 30
CSTOP = 0
SKIP = ()


class Buf:
    __slots__ = ("name", "w", "r")

    def __init__(self, name=""):
        self.name = name
        self.w = None
        self.r = {}


class Eng:
    def __init__(self, name, h, same_sync):
        self.name = name
        self.h = h
        self.sem = None
        self.count = 0
        self.seen = {}
        self.same_sync = same_sync
        self.ninst = 0
        self.pos = 0


class Fw:
    def __init__(self, nc, stack, n_dma_sems=8, same_sync=True):
        self.nc = nc
        self.pe = Eng("pe", nc.tensor, False)
        self.act = Eng("act", nc.scalar, same_sync)
        self.dve = Eng("dve", nc.vector, same_sync)
        self.pool = Eng("pool", nc.gpsimd, same_sync)
        self.sp = Eng("sp", nc.sync, False)
        self.engs = [self.pe, self.act, self.dve, self.pool, self.sp]
        for e in self.engs:
            e.sem = stack.enter_context(nc.semaphore("s_" + e.name))
        self.dma_pools = {}
        for q in ("sp", "pool"):
            sems = [stack.enter_context(nc.semaphore("d_%s%d" % (q, i))) for i in range(n_dma_sems)]
            self.dma_pools[q] = dict(sems=sems, cnt=[0] * n_dma_sems, nxt=0)

    def _need(self, eng, tok):
        sem, val, owner = tok[0], tok[1], tok[2]
        if owner is eng:
            if not eng.same_sync:
                return False
            if eng.pos - tok[3] >= SAME_DIST:
                return False
        return eng.seen.get(id(sem), 0) < val

    def _wait(self, eng, toks):
        best = {}
        for t in toks:
            if t is None or not self._need(eng, t):
                continue
            k = id(t[0])
            if k not in best or best[k][1] < t[1]:
                best[k] = t
        for k, t in best.items():
            eng.h.wait_ge(t[0], t[1])
            eng.seen[k] = t[1]
            eng.ninst += 1

    @staticmethod
    def _deps(reads, writes):
        toks = []
        for b in reads:
            toks.append(b.w)
        for b in writes:
            toks.append(b.w)
            toks.extend(b.r.values())
        return toks

    @staticmethod
    def _record(tok, reads, writes):
        k = id(tok[0])
        for b in reads:
            o = b.r.get(k)
            if o is None or o[1] < tok[1]:
                b.r[k] = tok
        for b in writes:
            b.w = tok
            b.r = {}

    def op(self, eng, fn, reads=(), writes=(), inc=True, nosync=False):
        toks = self._deps(reads, writes)
        if nosync:
            toks = [t for t in toks if t is not None and t[2] is not eng]
        self._wait(eng, toks)
        ins = fn()
        eng.ninst += 1
        eng.pos += 1
        if inc:
            ins.then_inc(eng.sem, 1)
            eng.count += 1
            tok = (eng.sem, eng.count, eng, eng.pos)
        else:
            tok = (eng.sem, eng.count + 1, eng, eng.pos + 1)
        self._record(tok, reads, writes)
        return tok

    def dma(self, eng, out, in_, reads=(), writes=(), **kw):
        pool = self.dma_pools[eng.name]
        self._wait(eng, self._deps(reads, writes))
        j = pool["nxt"]
        pool["nxt"] = (j + 1) % len(pool["sems"])
        sem = pool["sems"][j]
        if pool["cnt"][j] > 0 and eng.seen.get(id(sem), 0) < pool["cnt"][j]:
            eng.h.wait_ge(sem, pool["cnt"][j])
            eng.seen[id(sem)] = pool["cnt"][j]
        ins = eng.h.dma_start(out=out, in_=in_, **kw)
        ins.then_inc(sem, 16)
        eng.ninst += 1
        pool["cnt"][j] += 16
        tok = (sem, pool["cnt"][j], None, 0)
        self._record(tok, reads, writes)
        return tok

    def barrier(self):
        sp = self.sp
        toks = []
        for e in self.engs:
            if e is not sp and e.count > 0:
                toks.append((e.sem, e.count, e, e.pos))
        for q, pool in self.dma_pools.items():
            for j, sem in enumerate(pool["sems"]):
                if pool["cnt"][j] > 0:
                    toks.append((sem, pool["cnt"][j], None, 0))
        self._wait(sp, toks)
        ins = sp.h.nop()
        ins.then_inc(sp.sem, 1)
        sp.count += 1
        tok = (sp.sem, sp.count, sp, sp.pos)
        for e in self.engs:
            if e is sp:
                continue
            e.h.wait_ge(sp.sem, sp.count)
            e.seen[id(sp.sem)] = sp.count
            for t in toks:
                k = id(t[0])
                if e.seen.get(k, 0) < t[1]:
                    e.seen[k] = t[1]
        return tok


def _t5_onehot():
    dd = np.arange(1280, dtype=np.int64) - 511
    n = np.maximum(dd, 0)
    nf = np.maximum(n, 1).astype(np.float32)
    large = 16 + (np.log(nf / np.float32(16)) / np.float32(math.log(128 / 16)) * np.float32(16)).astype(np.int32)
    large = np.minimum(large, 31)
    bucket = np.where(n < 16, n, large)
    oh = np.zeros((33, 1280), np.float32)
    for j in range(1280):
        if dd[j] < 0:
            oh[32, j] = 1.0
        else:
            oh[bucket[j], j] = 1.0
    return oh


def build(nseq=2, nlayer=2, dbg=(), stop_after=None, same_sync=True):
    nc = bass.Bass("TRN2", target_bir_lowering=False)
    L = nlayer

    def din(name, shape, dt=F32):
        return nc.dram_tensor(name, list(shape), dt, kind="ExternalInput").ap()

    x_d = din("x", [nseq, S, D])
    c_d = din("c_lay", [128, 8, nseq])
    relb_d = din("rel_bias", [32, 12])
    adaw_d = din("ada_w", [L, D, 6 * D])
    adab_d = din("ada_bT", [L, 128, 48])
    nmix_d = din("norm_mixT", [L, 128, 8])
    win_d = din("w_in", [L, D, IN_COLS])
    gateb_d = din("gate_bT", [L, 128, 24])
    dlam_d = din("diff_lambda", [L, 256])
    subln_d = din("diff_subln", [L, 128, 1])
    kvn_d = din("dsa_kv_norm", [L, 128, 1])
    wuv_d = din("dsa_w_uv", [L, 4, 128, 64])
    wbra_d = din("w_br_a", [L, 512, D])
    wbrb_d = din("w_br_b", [L, 256, D])
    wbrc_d = din("w_br_c", [L, 256, D])
    wo_d = din("w_o", [L, D, D])
    nmlp_d = din("norm_mlpT", [L, 128, 8])
    wff1_d = din("w_ff1", [L, D, 4 * D])
    wff2_d = din("w_ff2", [L, 4 * D, D])
    nfin_d = din("norm_finalT", [128, 8])
    ident_d = din("k_ident", [128, 128])
    anti_d = din("k_anti", [128, 128])
    oh_d = din("k_onehot", [33, 1280])
    caus_d = din("k_caus", [128, 128])
    sel_d = din("k_sel", [32, 32 * 128])
    bm_d = din("k_bm", [8 * 32])
    own_d = din("k_own", [8 * 32])
    out_d = nc.dram_tensor("out", [nseq, S, D], F32, kind="ExternalOutput").ap()
    g_d = nc.dram_tensor("g_scr", [12, 1280], BF16, kind="Internal").ap()
    xsp_d = nc.dram_tensor("x_spill", [128, 8, S], F32, kind="Internal").ap()
    dbg_out = {}

    with contextlib.ExitStack() as st:
        fw = Fw(nc, st, same_sync=same_sync)
        pe, act, dve, pool, sp = fw.pe, fw.act, fw.dve, fw.pool, fw.sp
        T, V_, Sc, G = nc.tensor, nc.vector, nc.scalar, nc.gpsimd
        arena = st.enter_context(nc.sbuf_tensor("arena", [128, 51200], F32))
        psb = [st.enter_context(nc.psum_tensor("ps%d" % i, [128, 512], F32)) for i in range(8)]
        ps_b = [Buf("ps%d" % i) for i in range(8)]

        KW = 256

        def vf(off_k, nwords, parts=128, p0=0):
            o = int(round(off_k * KW))
            return arena[p0:p0 + parts, o:o + nwords]

        def vb(off_k, nelem, parts=128, p0=0):
            o = int(round(off_k * KW))
            return arena[p0:p0 + parts, o:o + nelem // 2].bitcast(BF16)

        identF = vf(0, 128)
        identB = vb(0.5, 128)
        antiB = vb(0.75, 128)
        onesB = vb(1.0, 128)
        epsT = vf(1.25, 1)
        halfT = vf(1.25, 1)
        neglam = [vf(1.26 + 0.01 * l, 1) for l in range(L)]
        def wv(word, n, parts=128):
            return arena[0:parts, word:word + n]
        W0 = 330
        epsT = wv(W0, 1)
        neglam = [wv(W0 + 1 + l, 1) for l in range(L)]
        subg = [wv(W0 + 4 + l, 1) for l in range(L)]
        kvg = [wv(W0 + 8 + l, 1) for l in range(L)]
        nfin = wv(W0 + 12, 8)
        zeroT = wv(W0 + 20, 1)
        c31 = wv(1000, 12)
        gateb = [wv(W0 + 24 + 24 * l, 24) for l in range(L)]
        PRM = {}
        w = W0 + 80
        for l in range(L):
            for b in range(nseq):
                PRM[(l, b)] = wv(w, 48).rearrange("p (j k) -> p j k", j=6)
                w += 48
        assert w <= 1024
        uT = vb(4, 8 * S).rearrange("p (k t) -> p k t", k=8)
        WST = [vb(36 + 8 * i, 8 * 512).rearrange("p (k n) -> p k n", k=8) for i in range(2)]
        PT = [vb(52 + i, 512) for i in range(4)]
        TMP = [vf(56 + 2 * i, 512) for i in range(8)]
        uT_b = [Buf("uT%d" % i) for i in range(4)]
        WST_b = [Buf("wst%d" % i) for i in range(2)]
        PT_b = [Buf("pt%d" % i) for i in range(4)]
        TMP_b = [Buf("tmp%d" % i) for i in range(8)]
        xT = vf(72, 8 * S).rearrange("p (k t) -> p k t", k=8)
        xT_b = [Buf("xT%d" % i) for i in range(4)]
        STR = vb(72, 12 * 1152).rearrange("p (h n) -> p h n", h=12)
        ARENA_K = 99
        merged = vb(136, 8 * S).rearrange("p (k t) -> p k t", k=8)
        merged_b = [Buf("mg%d" % i) for i in range(4)]
        oA = vb(168, 4 * S).rearrange("p (k t) -> p k t", k=4)
        oB = vb(184, 2 * S).rearrange("p (k t) -> p k t", k=2)
        oC = vb(192, 2 * S).rearrange("p (k t) -> p k t", k=2)
        oA_b, oB_b, oC_b = Buf("oA"), Buf("oB"), Buf("oC")
        xsp_b = Buf("xsp")
        gd_b = Buf("gd")

        state = {"pt": 0, "tmp": 0, "wst": 0, "lg": 0}

        def next_pt():
            i = state["pt"]
            state["pt"] = (i + 1) % 4
            return PT[i], PT_b[i]

        def next_tmp(avoid=()):
            i = state["tmp"]
            while any(TMP_b[i] is a for a in avoid):
                i = (i + 1) % 8
            state["tmp"] = (i + 1) % 8
            return TMP[i], TMP_b[i]

        def next_wst():
            i = state["wst"]
            state["wst"] = (i + 1) % 2
            return WST[i], WST_b[i]

        def dump(name, ap, shape, dt, reads):
            if name not in dbg:
                return
            d = nc.dram_tensor("dbg_" + name, list(shape), dt, kind="ExternalOutput").ap()
            dbg_out[name] = d
            fw.barrier()
            t = fw.dma(sp, d, ap, reads=reads)
            fw._wait(sp, [t])

        def load_w_slab(src_ap, ncols, eng=None):
            slab, sb = next_wst()
            fw.dma(pool, slab[:, :, 0:ncols], src_ap.rearrange("(k p) n -> p k n", p=128), writes=[sb])
            return slab, sb

        cb = Buf("consts")
        fw.dma(sp, identF, ident_d, writes=[cb])
        fw.dma(pool, identB, ident_d, writes=[cb])
        fw.dma(pool, antiB, anti_d, writes=[cb])
        fw.op(dve, lambda: V_.memset(onesB, 1.0), writes=[cb])
        fw.op(dve, lambda: V_.memset(epsT, EPS), writes=[cb])
        fw.op(dve, lambda: V_.memset(zeroT, 0.0), writes=[cb])
        fw.dma(sp, nfin, nfin_d, writes=[cb])
        fw.dma(sp, c31, relb_d[31].partition_broadcast(128), writes=[cb])
        for l in range(L):
            fw.dma(sp, subg[l], subln_d[l], writes=[cb])
            fw.dma(sp, kvg[l], kvn_d[l], writes=[cb])
            fw.dma(sp, gateb[l], gateb_d[l], writes=[cb])
        fw.barrier()
        sa = 72
        relb = vf(sa, 12, parts=33)
        ohT = vf(sa + 1, 1280, parts=33)
        grow = vb(sa + 7, 1280, parts=12)
        tb_ = Buf("setup")
        fw.op(dve, lambda: V_.memset(vf(sa, 12, parts=64)[32:64, :], NEG), writes=[tb_])
        fw.dma(sp, relb[0:32, :], relb_d, writes=[tb_])
        fw.dma(sp, ohT, oh_d, writes=[tb_])
        for ci, (c0, cn) in enumerate(((0, 512), (512, 512), (1024, 256))):
            fw.op(pe, lambda: T.matmul(psb[ci][0:12, 0:cn], relb, ohT[:, c0:c0 + cn], start=True, stop=True),
                  reads=[tb_], writes=[ps_b[ci]])
            fw.op(dve, lambda: V_.tensor_copy(grow[:, c0:c0 + cn], psb[ci][0:12, 0:cn]), reads=[ps_b[ci]], writes=[tb_])
        fw.dma(sp, g_d, grow, reads=[tb_], writes=[gd_b])
        for l in range(L):
            lam_init = 0.8 - 0.6 * math.exp(-0.3 * l)
            dl = vf(sa + 12, 256)
            pr = vf(sa + 13, 128)
            s12 = vf(sa + 14, 2)
            e12 = vf(sa + 14.5, 2)
            fw.dma(sp, dl, dlam_d[l].partition_broadcast(128), writes=[tb_])
            fw.op(dve, lambda: V_.tensor_tensor(pr[:, 0:64], dl[:, 0:64], dl[:, 64:128], op=ALU.mult), reads=[tb_], writes=[tb_])
            fw.op(dve, lambda: V_.tensor_tensor(pr[:, 64:128], dl[:, 128:192], dl[:, 192:256], op=ALU.mult), reads=[tb_], writes=[tb_])
            fw.op(dve, lambda: V_.tensor_reduce(s12, pr.rearrange("p (a b) -> p a b", a=2), axis=AX.X, op=ALU.add), reads=[tb_], writes=[tb_])
            fw.op(act, lambda: Sc.activation(e12, s12, AF.Exp), reads=[tb_], writes=[tb_])
            fw.op(dve, lambda: V_.tensor_tensor(s12[:, 0:1], e12[:, 1:2], e12[:, 0:1], op=ALU.subtract), reads=[tb_], writes=[tb_])
            fw.op(dve, lambda: V_.tensor_scalar(neglam[l], s12[:, 0:1], -lam_init, None, op0=ALU.add), reads=[tb_], writes=[tb_])
            fw.op(dve, lambda: V_.tensor_scalar(subg[l], subg[l], 1.0 - lam_init, None, op0=ALU.mult), reads=[tb_], writes=[tb_])
        cT = vf(sa + 16, 8 * nseq).rearrange("p (k b) -> p k b", k=8)
        modT = vf(sa + 17, 48 * nseq).rearrange("p (f b) -> p f b", f=48)
        adab = vf(sa + 18, 48)
        nmx = vf(sa + 19, 8)
        nml = vf(sa + 19.5, 8)
        fw.dma(sp, cT, c_d, writes=[tb_])
        fw.op(act, lambda: Sc.activation(cT, cT, AF.Silu), reads=[tb_], writes=[tb_])
        slabs = [vf(136 + 16 * i, 8 * 512).rearrange("p (k n) -> p k n", k=8) for i in range(4)]
        slab_b = [Buf("adaslab%d" % i) for i in range(4)]
        si = 0
        for l in range(L):
            fw.dma(sp, adab, adab_d[l], writes=[tb_])
            fw.dma(sp, nmx, nmix_d[l], writes=[tb_])
            fw.dma(sp, nml, nmlp_d[l], writes=[tb_])
            for sl in range(12):
                sb_, sbb = slabs[si % 4], slab_b[si % 4]
                si += 1
                fw.dma(sp, sb_, adaw_d[l][:, sl * 512:(sl + 1) * 512].rearrange("(k p) n -> p k n", p=128), writes=[sbb])
                bank = 4 + (sl % 2)
                for fc4 in range(4):
                    for k in range(8):
                        fw.op(pe, lambda: T.matmul(psb[bank][:, fc4 * nseq:(fc4 + 1) * nseq], sb_[:, k, fc4 * 128:(fc4 + 1) * 128], cT[:, k, :],
                                                   start=(k == 0), stop=(k == 7)),
                              reads=[sbb, tb_], writes=[ps_b[bank]], inc=(k == 7 and fc4 == 3))
                for fc4 in range(4):
                    fc = sl * 4 + fc4
                    fw.op(dve, lambda: V_.tensor_scalar(modT[:, fc, :], psb[bank][:, fc4 * nseq:(fc4 + 1) * nseq], adab[:, fc:fc + 1], None, op0=ALU.add),
                          reads=[ps_b[bank], tb_], writes=[tb_])
            for b in range(nseq):
                P = PRM[(l, b)]
                fw.op(dve, lambda: V_.scalar_tensor_tensor(P[:, 0, :], modT[:, 8:16, b], 1.0, nmx, op0=ALU.add, op1=ALU.mult), reads=[tb_], writes=[tb_])
                fw.op(dve, lambda: V_.tensor_copy(P[:, 1, :], modT[:, 0:8, b]), reads=[tb_], writes=[tb_])
                fw.op(dve, lambda: V_.tensor_copy(P[:, 2, :], modT[:, 16:24, b]), reads=[tb_], writes=[tb_])
                fw.op(dve, lambda: V_.scalar_tensor_tensor(P[:, 3, :], modT[:, 32:40, b], 1.0, nml, op0=ALU.add, op1=ALU.mult), reads=[tb_], writes=[tb_])
                fw.op(dve, lambda: V_.tensor_copy(P[:, 4, :], modT[:, 24:32, b]), reads=[tb_], writes=[tb_])
                fw.op(dve, lambda: V_.tensor_copy(P[:, 5, :], modT[:, 40:48, b]), reads=[tb_], writes=[tb_])
        fw.barrier()

        def rstd_from_ss(ss_bank, inv_n):
            t1, t1b = next_tmp()
            fw.op(act, lambda: Sc.activation(t1, psb[ss_bank][:, :], AF.Ln, bias=epsT, scale=inv_n), reads=[ps_b[ss_bank]], writes=[t1b])
            t2, t2b = next_tmp()
            fw.op(act, lambda: Sc.activation(t2, t1, AF.Exp, scale=-0.5), reads=[t1b], writes=[t2b])
            return t2, t2b

        def norm_mod(Aap, Bap):
            for tc in range(4):
                ts = slice(tc * 512, (tc + 1) * 512)
                bank = 6 + (tc % 2)
                for k in range(8):
                    sq, sqb = next_pt()
                    fw.op(act, lambda: Sc.activation(sq, xT[:, k, ts], AF.Square), reads=[xT_b[tc]], writes=[sqb])
                    fw.op(pe, lambda: T.matmul(psb[bank][:, :], onesB, sq, start=(k == 0), stop=(k == 7)),
                          reads=[sqb], writes=[ps_b[bank]], inc=True)
                rstd, rb = rstd_from_ss(bank, 1.0 / D)
                for k in range(8):
                    t1, t1b = next_tmp(avoid=(rb,))
                    fw.op(dve, lambda: V_.tensor_tensor(t1, xT[:, k, ts], rstd, op=ALU.mult), reads=[xT_b[tc], rb], writes=[t1b])
                    if Bap is not None:
                        fw.op(act, lambda: Sc.activation(uT[:, k, ts], t1, AF.Identity, bias=Bap[:, k:k + 1], scale=Aap[:, k:k + 1]),
                              reads=[t1b], writes=[uT_b[tc]])
                    else:
                        fw.op(act, lambda: Sc.activation(xT[:, k, ts], t1, AF.Identity, bias=zeroT, scale=Aap[:, k:k + 1]),
                              reads=[t1b], writes=[xT_b[tc]])

        def proj_fm(slab, sb, col0, ncols, dst_fn, dst_bufs, scale=None, banks=(6, 7), post=None, dst2_fn=None):
            for tc in range(4):
                bank = banks[tc % len(banks)]
                for k in range(8):
                    fw.op(pe, lambda: T.matmul(psb[bank][0:ncols, :], slab[:, k, col0:col0 + ncols], uT[:, k, tc * 512:(tc + 1) * 512],
                                               start=(k == 0), stop=(k == 7)),
                          reads=[sb, uT_b[tc]], writes=[ps_b[bank]], inc=(k == 7))
                xr = []
                if post is not None:
                    xr = post(tc, bank)
                if dst2_fn is not None:
                    fw.op(act, lambda: Sc.copy(dst_fn(tc), psb[bank][0:64, :]), reads=[ps_b[bank]] + xr, writes=dst_bufs)
                    fw.op(act, lambda: Sc.copy(dst2_fn(tc), psb[bank][64:128, :]), reads=[ps_b[bank]] + xr, writes=dst_bufs)
                elif scale is None:
                    fw.op(act, lambda: Sc.copy(dst_fn(tc), psb[bank][0:ncols, :]), reads=[ps_b[bank]] + xr, writes=dst_bufs)
                else:
                    fw.op(act, lambda: Sc.activation(dst_fn(tc), psb[bank][0:ncols, :], AF.Copy, scale=scale), reads=[ps_b[bank]] + xr, writes=dst_bufs)

        def proj_tm(slab, sb, col0, ncols, dst_fn, dst_bufs, banks=(6, 7), dtype_copy=True):
            for tb in range(16):
                bank = banks[tb % len(banks)]
                for k in range(8):
                    fw.op(pe, lambda: T.matmul(psb[bank][:, 0:ncols], uT[:, k, tb * 128:(tb + 1) * 128], slab[:, k, col0:col0 + ncols],
                                               start=(k == 0), stop=(k == 7)),
                          reads=[sb, uT_b[tb // 4]], writes=[ps_b[bank]], inc=(k == 7))
                e = dve if tb % 2 else act
                if e is dve:
                    fw.op(dve, lambda: V_.tensor_copy(dst_fn(tb), psb[bank][:, 0:ncols]), reads=[ps_b[bank]], writes=dst_bufs)
                else:
                    fw.op(act, lambda: Sc.copy(dst_fn(tb), psb[bank][:, 0:ncols]), reads=[ps_b[bank]], writes=dst_bufs)

        def attn_core(tc, k_fn, q_ap, strip_h, v_fn, o_ap, sum_ap, o_bank, s_bank, ones_ap, reads, extra=None,
                      first=True, last_grp=True):
            nkb = 4 * tc + 4
            lbanks = (0, 1)

            def qk(kb):
                bank = lbanks[kb % 2]
                dl_ = min(4 * tc - kb, 2)
                c0 = (dl_ + 3) * 128
                cl = max(kb - 4 * tc, 0) * 128
                far = dl_ >= 2
                has_extra = extra is not None and extra(kb, bank, True, cl)
                only = far and not has_extra
                fw.op(pe, lambda: T.matmul(psb[bank][:, cl:512], k_fn(kb), q_ap[:, cl:512], start=True, stop=only),
                      reads=reads, writes=[ps_b[bank]], inc=only)
                if not far:
                    fw.op(pe, lambda: T.matmul(psb[bank][:, cl:512], antiB, STR[:, strip_h, c0 + cl:c0 + 512], start=False, stop=not has_extra),
                          reads=[], writes=[ps_b[bank]], inc=not has_extra)
                if has_extra:
                    extra(kb, bank, False, cl)
                return bank, far, cl

            pend = qk(0)
            for kb in range(nkb):
                bank, far, cl = pend
                if kb + 1 < nkb:
                    pend = qk(kb + 1)
                p, pb_ = next_pt()
                if far:
                    fw.op(act, lambda: Sc.activation(p[:, cl:512], psb[bank][:, cl:512], AF.Exp, bias=c31[:, strip_h:strip_h + 1]), reads=[ps_b[bank]], writes=[pb_])
                else:
                    fw.op(act, lambda: Sc.activation(p[:, cl:512], psb[bank][:, cl:512], AF.Exp), reads=[ps_b[bank]], writes=[pb_])
                st_ = first and kb == 0
                sp_ = last_grp and kb == nkb - 1
                fw.op(pe, lambda: T.matmul(o_ap[:, cl:512], v_fn(kb), p[:, cl:512], start=st_, stop=sp_),
                      reads=reads + [pb_], writes=[ps_b[o_bank]], inc=sp_)
                fw.op(pe, lambda: T.matmul(sum_ap[:, cl:512], ones_ap, p[:, cl:512], start=st_, stop=sp_),
                      reads=[pb_], writes=[ps_b[s_bank]], inc=True)

        def recip_sum(s_bank, sum_ap, parts=128, p0=0):
            t1, t1b = next_tmp()
            t1v = t1[p0:p0 + parts, :]
            fw.op(act, lambda: Sc.activation(t1v, sum_ap, AF.Ln), reads=[ps_b[s_bank]], writes=[t1b])
            t2, t2b = next_tmp()
            t2v = t2[p0:p0 + parts, :]
            fw.op(act, lambda: Sc.activation(t2v, t1v, AF.Exp, scale=-1.0), reads=[t1b], writes=[t2b])
            return t2v, t2b

        def load_strips(heads):
            sb_ = Buf("strips")
            for h in heads:
                src = bass.AP(tensor=g_d.tensor, offset=h * 1280, ap=[[1, 128], [1, 1152]])
                fw.dma(sp, STR[:, h, :], src, reads=[gd_b], writes=[sb_])
            return sb_

        def phase_A(l):
            ak = ARENA_K
            qT = vb(ak, 4 * S).rearrange("p (h t) -> p h t", h=4)
            kTz = vb(ak + 16, 8 * S).rearrange("p (h m t) -> p h m t", h=4, m=2)
            Vt = vb(ak + 48, 16 * 512).rearrange("p (b e) -> p b e", b=16)
            q_b, k_b, v_b = Buf("qT"), Buf("kT"), Buf("Vt")
            slab, sb = load_w_slab(win_d[l][:, O_AQ:O_AQ + 512], 512)
            for h in range(4):
                proj_fm(slab, sb, h * 128, 128, lambda tc: qT[:, h, tc * 512:(tc + 1) * 512], [q_b], scale=0.125)
            fw.op(pool, lambda: G.memset(kTz[64:128, :, 0, :], 0.0), writes=[k_b])
            fw.op(pool, lambda: G.memset(kTz[0:64, :, 1, :], 0.0), writes=[k_b])
            slab, sb = load_w_slab(win_d[l][:, O_AK:O_AK + 512], 512)
            for h in range(4):
                proj_fm(slab, sb, h * 128, 128, lambda tc: kTz[0:64, h, 0, tc * 512:(tc + 1) * 512], [k_b],
                        dst2_fn=lambda tc: kTz[64:128, h, 1, tc * 512:(tc + 1) * 512])
            slab, sb = load_w_slab(win_d[l][:, O_AV:O_AV + 512], 512)
            proj_tm(slab, sb, 0, 512, lambda tb: Vt[:, tb, :], [v_b])
            sqd = [vb(ak + 64 + i, 512) for i in range(2)]
            sqd_b = [Buf("sqd0"), Buf("sqd1")]
            deferred = []
            gi = 0
            for h in range(4):
                for tc in range(4):
                    ts = slice(tc * 512, (tc + 1) * 512)
                    R = []
                    for m in range(2):
                        ob, sbk = 2 + m, 4 + m
                        attn_core(tc, lambda kb: kTz[:, h, m, kb * 128:(kb + 1) * 128], qT[:, h, ts], h,
                                  lambda kb: Vt[:, kb, h * 128:(h + 1) * 128], psb[ob][:, :], psb[sbk][:, :], ob, sbk, onesB,
                                  reads=[q_b, k_b, v_b])
                        rs, rsb = recip_sum(sbk, psb[sbk][:, :])
                        r, rb = next_tmp()
                        fw.op(dve, lambda: V_.tensor_tensor(r, psb[ob][:, :], rs, op=ALU.mult), reads=[ps_b[ob], rsb], writes=[rb])
                        R.append((r, rb))
                        if m == 0 and deferred:
                            deferred.pop()()
                    dd, ddb = next_tmp()
                    fw.op(dve, lambda: V_.scalar_tensor_tensor(dd, R[1][0], neglam[l], R[0][0], op0=ALU.mult, op1=ALU.add),
                          reads=[R[0][1], R[1][1]], writes=[ddb])
                    sq, sqb = sqd[gi % 2], sqd_b[gi % 2]
                    gi += 1
                    fw.op(act, lambda: Sc.activation(sq, dd, AF.Square), reads=[ddb], writes=[sqb])

                    def tail(dd=dd, ddb=ddb, sq=sq, sqb=sqb, h=h, ts=ts):
                        fw.op(pe, lambda: T.matmul(psb[6][:, :], onesB, sq, start=True, stop=True), reads=[sqb], writes=[ps_b[6]])
                        rstd, rb2 = rstd_from_ss(6, 1.0 / 128)
                        fw.op(dve, lambda: V_.scalar_tensor_tensor(oA[:, h, ts], dd, subg[l], rstd, op0=ALU.mult, op1=ALU.mult),
                              reads=[ddb, rb2], writes=[oA_b])
                    deferred.append(tail)
            while deferred:
                deferred.pop()()

        def phase_B(l):
            ak = ARENA_K
            ak = 90
            kvT = vb(72, S)
            kvt = vb(76, 16 * 128).rearrange("p (b r) -> p b r", b=16)
            bqT = vb(ak, 4 * S).rearrange("p (h t) -> p h t", h=4)
            iqT = vb(ak + 16, 3 * S, parts=96).rearrange("p (c t) -> p c t", c=3)
            ikT = vb(ak + 28, S, parts=96)
            scores = vf(ak + 32, S)
            MnegAll = vb(ak + 40, 8 * S).rearrange("p (u j s) -> p u j s", u=2, j=4)
            iw = vf(ak + 72, 16 * 8).rearrange("p (b h) -> p b h", b=16)
            wuv2 = vb(ak + 72.5, 4 * 128).rearrange("p (h e) -> p h e", h=4)
            caus = vf(ak + 73.5, 128)
            bis = vf(ak + 74, 16)
            ikw = vb(ak + 74.25, 8 * 96).rearrange("p (k n) -> p k n", k=8)
            bq_b, kv_b, kvt_b, iq_b, ik_b, sc_b, iw_b, wuv_b, ca_b, bis_b, ikw_b = [Buf(n) for n in
                ("bq", "kv", "kvt", "iq", "ik", "sc", "iw", "wuv", "ca", "bis", "ikw")]
            mn_bs = [[Buf("mn%d%d" % (u, j)) for j in range(4)] for u in range(2)]
            fw.dma(sp, caus, caus_d, writes=[ca_b])
            fw.op(pool, lambda: G.memset(wuv2, 0.0), writes=[wuv_b])
            for h in range(4):
                fw.dma(pool, wuv2[:, h, (h % 2) * 64:(h % 2) * 64 + 64], wuv_d[l][h], writes=[wuv_b])
            slab, sb = load_w_slab(win_d[l][:, O_BQ:O_BQ + 512], 512)
            for h in range(4):
                proj_fm(slab, sb, h * 128, 128, lambda tc: bqT[:, h, tc * 512:(tc + 1) * 512], [bq_b], scale=128 ** -0.5)
            slab, sb = load_w_slab(win_d[l][:, O_BKV:O_BKV + 424], 424)

            def kv_post(tc, bank):
                ts = slice(tc * 512, (tc + 1) * 512)
                sq, sqb = next_pt()
                fw.op(act, lambda: Sc.activation(sq, psb[bank][:, :], AF.Square), reads=[ps_b[bank]], writes=[sqb])
                fw.op(pe, lambda: T.matmul(psb[5][:, :], onesB, sq, start=True, stop=True), reads=[sqb], writes=[ps_b[5]])
                rstd, rb = rstd_from_ss(5, 1.0 / 128)
                fw.op(dve, lambda: V_.scalar_tensor_tensor(kvT[:, ts], psb[bank][:, :], kvg[l], rstd, op0=ALU.mult, op1=ALU.mult),
                      reads=[ps_b[bank], rb], writes=[kv_b])

            for tc in range(4):
                bank = 6 + (tc % 2)
                for k in range(8):
                    fw.op(pe, lambda: T.matmul(psb[bank][:, :], slab[:, k, 0:128], uT[:, k, tc * 512:(tc + 1) * 512], start=(k == 0), stop=(k == 7)),
                          reads=[sb, uT_b[tc]], writes=[ps_b[bank]], inc=(k == 7))
                kv_post(tc, bank)
            for g4 in range(4):
                bank = 6 + (g4 % 2)
                pbf = psb[bank][:, :].bitcast(BF16)
                for j in range(4):
                    tb = g4 * 4 + j
                    fw.op(pe, lambda: T.transpose(pbf[:, j * 128:(j + 1) * 128], kvT[:, tb * 128:(tb + 1) * 128], identB),
                          reads=[kv_b], writes=[ps_b[bank]], inc=(j == 3))
                fw.op(dve, lambda: V_.tensor_copy(kvt[:, g4 * 4:(g4 + 1) * 4, :], pbf[:, 0:512].rearrange("p (a b) -> p a b", a=4)),
                      reads=[ps_b[bank]], writes=[kvt_b])
            for c in range(3):
                nh = 3 if c < 2 else 2
                proj_fm(slab, sb, 128 + c * 96, nh * 32, lambda tc: iqT[0:nh * 32, c, tc * 512:(tc + 1) * 512], [iq_b])
            for j in range(3):
                fw.op(dve, lambda: V_.tensor_copy(ikw[:, :, j * 32:(j + 1) * 32], slab[:, :, 384:416]), reads=[sb], writes=[ikw_b])
            proj_fm(ikw, ikw_b, 0, 96, lambda tc: ikT[:, tc * 512:(tc + 1) * 512], [ik_b])
            proj_tm(slab, sb, 416, 8, lambda tb: iw[:, tb, :], [iw_b])

            def indexer(qb, j):
                u = (qb // 4) % 2
                Mneg = MnegAll[:, u]
                mn_b = mn_bs[u][j]
                Wd = (qb + 1) * 128
                nsc = (Wd + 511) // 512
                for sc in range(nsc):
                    cols = min(512, Wd - sc * 512)
                    cs = slice(sc * 512, sc * 512 + cols)
                    for hh in range(8):
                        c, pb = hh // 3, (hh % 3) * 32
                        bank = 6 + (hh % 2)
                        fw.op(pe, lambda: T.matmul(psb[bank][:, 0:cols], iqT[pb:pb + 32, c, qb * 128:(qb + 1) * 128], ikT[pb:pb + 32, cs],
                                                   start=True, stop=True),
                              reads=[iq_b, ik_b], writes=[ps_b[bank]])
                        fw.op(act, lambda: Sc.activation(psb[bank][:, 0:cols], psb[bank][:, 0:cols], AF.Relu), reads=[], writes=[ps_b[bank]])
                        if hh == 0:
                            fw.op(dve, lambda: V_.tensor_scalar(scores[:, cs], psb[bank][:, 0:cols], iw[:, qb, 0:1], None, op0=ALU.mult),
                                  reads=[ps_b[bank], iw_b], writes=[sc_b])
                        else:
                            fw.op(dve, lambda: V_.scalar_tensor_tensor(scores[:, cs], psb[bank][:, 0:cols], iw[:, qb, hh:hh + 1], scores[:, cs],
                                                                       op0=ALU.mult, op1=ALU.add),
                                  reads=[ps_b[bank], iw_b, sc_b], writes=[sc_b])
                dsl = slice(qb * 128, (qb + 1) * 128)
                fw.op(dve, lambda: V_.tensor_tensor(scores[:, dsl], scores[:, dsl], caus, op=ALU.add), reads=[sc_b, ca_b], writes=[sc_b])
                lo, hi, mid, cnt, w0 = (bis[:, i:i + 1] for i in range(5))
                geu = bis[:, 6:7].bitcast(U32)
                fw.op(dve, lambda: V_.tensor_reduce(hi, scores[:, 0:Wd], axis=AX.X, op=ALU.max), reads=[sc_b], writes=[bis_b])
                fw.op(dve, lambda: V_.tensor_reduce(lo, scores[:, 0:256], axis=AX.X, op=ALU.min), reads=[sc_b], writes=[bis_b])
                fw.op(dve, lambda: V_.tensor_tensor(w0, hi, lo, op=ALU.subtract), reads=[bis_b], writes=[bis_b])
                fw.op(dve, lambda: V_.tensor_scalar(w0, w0, 1.0 + 1e-5, 1e-6, op0=ALU.mult, op1=ALU.add), reads=[bis_b], writes=[bis_b])
                junk = Mneg[:, j, 0:Wd]
                for it in range(BIS_ITERS):
                    ck = 2.0 ** -(it + 1)
                    fw.op(dve, lambda: V_.scalar_tensor_tensor(mid, w0, ck, lo, op0=ALU.mult, op1=ALU.add), reads=[bis_b], writes=[bis_b])
                    fw.op(dve, lambda: V_.tensor_scalar(junk, scores[:, 0:Wd], mid, 0.0, op0=ALU.is_ge, op1=ALU.add, accum_out=cnt),
                          reads=[sc_b, bis_b], writes=[mn_b, bis_b])
                    fw.op(dve, lambda: V_.tensor_scalar(geu, cnt, 255.5, None, op0=ALU.is_ge), reads=[bis_b], writes=[bis_b])
                    fw.op(dve, lambda: V_.copy_predicated(lo, geu, mid), reads=[bis_b], writes=[bis_b])
                fw.op(dve, lambda: V_.tensor_scalar(Mneg[:, j, 0:Wd], scores[:, 0:Wd], lo, NEG, op0=ALU.is_lt, op1=ALU.mult),
                      reads=[sc_b, bis_b], writes=[mn_b])

            ohb_b = [Buf("oh%d" % i) for i in range(4)]
            for j in range(2, 4):
                indexer(j, j)
            for tc in range(4):
                ts = slice(tc * 512, (tc + 1) * 512)
                u = tc % 2
                Mneg = MnegAll[:, u]
                oh_list = []
                for h in range(4):
                    def extra(kb, bank, query_only, cl=0):
                        js = [j for j in range(4) if (4 * tc + j) >= 2 and kb <= 4 * tc + j]
                        if query_only:
                            return len(js) > 0
                        for idx, j in enumerate(js):
                            lastj = idx == len(js) - 1
                            fw.op(pe, lambda: T.matmul(psb[bank][:, j * 128:(j + 1) * 128], Mneg[:, j, kb * 128:(kb + 1) * 128], identB,
                                                       start=False, stop=lastj),
                                  reads=[mn_bs[u][j]], writes=[ps_b[bank]], inc=lastj)
                        return True
                    ob, sbk = (2, 4) if h % 2 == 0 else (3, 5)
                    attn_core(tc, lambda kb: kvT[:, kb * 128:(kb + 1) * 128], bqT[:, h, ts], 4 + h,
                              lambda kb: kvt[:, kb, :], psb[ob][:, :], psb[sbk][:, :], ob, sbk, onesB,
                              reads=[bq_b, kv_b, kvt_b], extra=extra)
                    rs, rsb = recip_sum(sbk, psb[sbk][:, :])
                    o, ob_ = vb(36 + h, 512), ohb_b[h]
                    fw.op(dve, lambda: V_.tensor_tensor(o, psb[ob][:, :], rs, op=ALU.mult), reads=[ps_b[ob], rsb], writes=[ob_, WST_b[0]])
                    oh_list.append((o, ob_))
                    if tc + 1 < 4:
                        indexer(4 * (tc + 1) + h, h)
                for pr in range(2):
                    wb_ = 6 + pr
                    for hh in range(2):
                        h = 2 * pr + hh
                        fw.op(pe, lambda: T.matmul(psb[wb_][:, :], wuv2[:, h, :], oh_list[h][0], start=(hh == 0), stop=(hh == 1)),
                              reads=[wuv_b, oh_list[h][1], WST_b[0]], writes=[ps_b[wb_]], inc=(hh == 1))
                    fw.op(act, lambda: Sc.copy(oB[:, pr, ts], psb[wb_][:, :]), reads=[ps_b[wb_]], writes=[oB_b])

        def phase_C(l):
            ak = ARENA_K
            cqT = vb(ak, 2 * S).rearrange("p (c t) -> p c t", c=2)
            ckTz = vb(ak + 8, 4 * S).rearrange("p (c h t) -> p c h t", c=2, h=2)
            cvz = vb(ak + 24, 16 * 4 * 128).rearrange("p (b c h e) -> p b c h e", b=16, c=2, h=2)
            MT = vb(ak + 40, S, parts=32)
            selc = vb(ak + 44, 32 * 128, parts=32).rearrange("p (r s) -> p r s", r=32)
            ksum = vf(ak + 52, 16).rearrange("p (c n) -> p c n", c=2)
            bm = vf(ak + 52.5, 256).rearrange("p (j n) -> p j n", j=8)
            own = vf(ak + 53.5, 256).rearrange("p (j n) -> p j n", j=8)
            gm = vf(ak + 54.5, 32)
            m8 = vf(ak + 54.75, 8)
            selo = vf(ak + 55, 32)
            Mt = vb(ak + 55.25, 32)
            kmZ = vb(ak + 55.5, 32).rearrange("p (c h n) -> p c h n", c=2, h=2)
            onesz = vb(ak + 56, 256).rearrange("p (h e) -> p h e", h=2)
            cq_b, ck_b, cv_b, mt_b, sel_b, ks_b, km_b, bm_b, gm_b, oz_b = [Buf(n) for n in ("cq", "ck", "cv", "MT", "sel", "ks", "km", "bm", "gm", "oz")]
            fw.dma(pool, selc, sel_d.rearrange("p (r s) -> p r s", r=32), writes=[sel_b], max_dma_last_dim=2048)
            fw.dma(sp, bm, bm_d.partition_broadcast(128).rearrange("p (j n) -> p j n", j=8), writes=[bm_b])
            fw.dma(sp, own, own_d.partition_broadcast(128).rearrange("p (j n) -> p j n", j=8), writes=[bm_b])
            fw.op(pool, lambda: G.memset(ckTz[64:128, :, 0, :], 0.0), writes=[ck_b])
            fw.op(pool, lambda: G.memset(ckTz[0:64, :, 1, :], 0.0), writes=[ck_b])
            fw.op(pool, lambda: G.memset(cvz, 0.0), writes=[cv_b])
            fw.op(pool, lambda: G.memset(onesz, 0.0), writes=[oz_b])
            fw.op(pool, lambda: G.memset(onesz[:, 0, 0:64], 1.0), writes=[oz_b])
            fw.op(pool, lambda: G.memset(onesz[:, 1, 64:128], 1.0), writes=[oz_b])
            slab, sb = load_w_slab(win_d[l][:, O_CQ:O_CQ + 512], 512)
            for c in range(2):
                proj_fm(slab, sb, c * 128, 128, lambda tc: cqT[:, c, tc * 512:(tc + 1) * 512], [cq_b], scale=0.125)

            for c in range(2):
                def post(tc, bank):
                    fw.op(dve, lambda: V_.tensor_reduce(ksum[:, c, 2 * tc:2 * tc + 2], psb[bank][:, :].rearrange("p (b s) -> p b s", b=2),
                                                        axis=AX.X, op=ALU.add),
                          reads=[ps_b[bank]], writes=[ks_b])
                    return [ks_b]
                proj_fm(slab, sb, 256 + c * 128, 128, lambda tc: ckTz[0:64, c, 0, tc * 512:(tc + 1) * 512], [ck_b], post=post,
                        dst2_fn=lambda tc: ckTz[64:128, c, 1, tc * 512:(tc + 1) * 512])
            fw.op(dve, lambda: V_.memset(kmZ, 0.0), writes=[km_b])
            for hh in range(2):
                pr_ = slice(hh * 64, hh * 64 + 64)
                fw.op(dve, lambda: V_.tensor_scalar(kmZ[pr_, :, hh, :], ksum[pr_, :, :], 1.0 / 256, None, op0=ALU.mult), reads=[ks_b], writes=[km_b])
            slab, sb = load_w_slab(win_d[l][:, O_CV:O_CV + 256], 256)
            for tb in range(16):
                bank = 6 + (tb % 2)
                for k in range(8):
                    fw.op(pe, lambda: T.matmul(psb[bank][:, 0:256], uT[:, k, tb * 128:(tb + 1) * 128], slab[:, k, 0:256], start=(k == 0), stop=(k == 7)),
                          reads=[sb, uT_b[tb // 4]], writes=[ps_b[bank]], inc=(k == 7))
                src = psb[bank][:, 0:256].rearrange("p (c h e) -> p c h e", c=2, h=2)
                if tb % 2:
                    fw.op(dve, lambda: V_.tensor_copy(cvz[:, tb, :, 0, 0:64], src[:, :, 0, :]), reads=[ps_b[bank]], writes=[cv_b])
                    fw.op(dve, lambda: V_.tensor_copy(cvz[:, tb, :, 1, 64:128], src[:, :, 1, :]), reads=[ps_b[bank]], writes=[cv_b])
                else:
                    fw.op(act, lambda: Sc.copy(cvz[:, tb, :, 0, 0:64], src[:, :, 0, :]), reads=[ps_b[bank]], writes=[cv_b])
                    fw.op(act, lambda: Sc.copy(cvz[:, tb, :, 1, 64:128], src[:, :, 1, :]), reads=[ps_b[bank]], writes=[cv_b])
            for qb in range(16):
                jb = qb // 2
                bank = 6 + (qb % 2)
                for h in range(4):
                    c = h // 2
                    fw.op(pe, lambda: T.matmul(psb[bank][:, h * 8:(h + 1) * 8], cqT[:, c, qb * 128:(qb + 1) * 128], kmZ[:, c, h % 2, :],
                                               start=True, stop=True),
                          reads=[cq_b, km_b], writes=[ps_b[bank]], inc=(h == 3))
                fw.op(dve, lambda: V_.tensor_tensor(gm, psb[bank][:, 0:32], bm[:, jb, :], op=ALU.add), reads=[ps_b[bank], bm_b], writes=[gm_b])
                for h in range(4):
                    fw.op(dve, lambda: V_.max(m8, gm[:, h * 8:(h + 1) * 8]), reads=[gm_b], writes=[gm_b])
                    fw.op(dve, lambda: V_.tensor_scalar(selo[:, h * 8:(h + 1) * 8], gm[:, h * 8:(h + 1) * 8], m8[:, 2:3], None, op0=ALU.is_ge),
                          reads=[gm_b], writes=[gm_b])
                fw.op(dve, lambda: V_.tensor_tensor(selo, selo, own[:, jb, :], op=ALU.max), reads=[gm_b, bm_b], writes=[gm_b])
                fw.op(dve, lambda: V_.tensor_scalar(Mt, selo, -NEG, NEG, op0=ALU.mult, op1=ALU.add), reads=[gm_b], writes=[gm_b])
                pbf = psb[bank][:, :].bitcast(BF16)
                fw.op(pe, lambda: T.transpose(pbf[0:32, 0:128], Mt, identB), reads=[gm_b], writes=[ps_b[bank]])
                fw.op(act, lambda: Sc.copy(MT[:, qb * 128:(qb + 1) * 128], pbf[0:32, 0:128]), reads=[ps_b[bank]], writes=[mt_b])
            for c in range(2):
                for tc in range(4):
                    ts = slice(tc * 512, (tc + 1) * 512)
                    for hh in range(2):
                        h = 2 * c + hh

                        def extra(kb, bank, query_only, cl=0):
                            if query_only:
                                return True
                            fw.op(pe, lambda: T.matmul(psb[bank][:, cl:512], selc[:, h * 8 + kb // 2, :], MT[:, tc * 512 + cl:(tc + 1) * 512], start=False, stop=True),
                                  reads=[sel_b, mt_b], writes=[ps_b[bank]], inc=True)
                            return True
                        ob, sbk = (2, 4) if tc % 2 == 0 else (3, 5)
                        attn_core(tc, lambda kb: ckTz[:, c, hh, kb * 128:(kb + 1) * 128], cqT[:, c, ts], 8 + h,
                                  lambda kb: cvz[:, kb, c, hh, :], psb[ob][:, :], psb[sbk][:, :], ob, sbk, onesz[:, hh, :],
                                  reads=[cq_b, ck_b, cv_b, oz_b], extra=extra, first=(hh == 0), last_grp=(hh == 1))
                    rs, rsb = recip_sum(sbk, psb[sbk][:, :])
                    fw.op(dve, lambda: V_.tensor_tensor(oC[:, c, ts], psb[ob][:, :], rs, op=ALU.mult), reads=[ps_b[ob], rsb], writes=[oC_b])

        def phase_M(l, b):
            P = PRM[(l, b)]
            wg = [vb(36 + 8 * i, 8 * 384).rearrange("p (k n) -> p k n", k=8) for i in range(2)]
            wbr = [vb(36 + 8 * i + 6, 8 * 128).rearrange("p (k n) -> p k n", k=8) for i in range(2)]
            srcs = [(oA, oA_b, 4, wbra_d, 0), (oB, oB_b, 2, wbrb_d, 4), (oC, oC_b, 2, wbrc_d, 6)]
            for mf in range(8):
                i = mf % 2
                wgi, wbi, wb_ = wg[i], wbr[i], WST_b[i]
                for j in range(3):
                    c0 = O_G + j * 1024 + mf * 128
                    fw.dma(pool, wgi[:, :, j * 128:(j + 1) * 128], win_d[l][:, c0:c0 + 128].rearrange("(k p) n -> p k n", p=128), writes=[wb_])
                for (o_, ob, nk, wd, k0) in srcs:
                    fw.dma(pool, wbi[:, k0:k0 + nk, :], wd[l][:, mf * 128:(mf + 1) * 128].rearrange("(k p) n -> p k n", p=128), writes=[wb_])
                for tc in range(4):
                    ts = slice(tc * 512, (tc + 1) * 512)
                    sig = []
                    for bi in range(3):
                        gbank = (0, 1, 6)[bi]
                        for k in range(8):
                            fw.op(pe, lambda: T.matmul(psb[gbank][:, :], wgi[:, k, bi * 128:(bi + 1) * 128], uT[:, k, ts], start=(k == 0), stop=(k == 7)),
                                  reads=[wb_, uT_b[tc]], writes=[ps_b[gbank]], inc=(k == 7))
                        sg, sgb = next_pt()
                        fw.op(act, lambda: Sc.activation(sg, psb[gbank][:, :], AF.Sigmoid, bias=gateb[l][:, bi * 8 + mf:bi * 8 + mf + 1]),
                              reads=[ps_b[gbank]], writes=[sgb])
                        sig.append((sg, sgb))
                    terms = []
                    for bi, (o_, ob, nk, wd, k0) in enumerate(srcs):
                        ybank = (2, 3, 4)[bi]
                        for k in range(nk):
                            fw.op(pe, lambda: T.matmul(psb[ybank][:, :], wbi[:, k0 + k, :], o_[:, k, ts], start=(k == 0), stop=(k == nk - 1)),
                                  reads=[wb_, ob], writes=[ps_b[ybank]], inc=(k == nk - 1))
                        tm, tmb = next_tmp()
                        fw.op(dve, lambda: V_.tensor_tensor(tm, psb[ybank][:, :], sig[bi][0], op=ALU.mult), reads=[ps_b[ybank], sig[bi][1]], writes=[tmb])
                        terms.append((tm, tmb))
                    fw.op(pool, lambda: G.tensor_tensor(terms[0][0], terms[0][0], terms[1][0], op=ALU.add), reads=[terms[1][1]], writes=[terms[0][1]])
                    fw.op(pool, lambda: G.tensor_tensor(merged[:, mf, ts], terms[0][0], terms[2][0], op=ALU.add),
                          reads=[terms[0][1], terms[2][1]], writes=[merged_b[tc]])
            for half in range(2):
                slab, sb = load_w_slab(wo_d[l][:, half * 512:(half + 1) * 512], 512)
                for f4 in range(4):
                    f = half * 4 + f4
                    for tc in range(4):
                        ts = slice(tc * 512, (tc + 1) * 512)
                        bank = (f4 * 4 + tc) % 8
                        for k in range(8):
                            fw.op(pe, lambda: T.matmul(psb[bank][:, :], slab[:, k, f4 * 128:(f4 + 1) * 128], merged[:, k, ts], start=(k == 0), stop=(k == 7)),
                                  reads=[sb, merged_b[tc]], writes=[ps_b[bank]], inc=(k == 7))
                        fw.op(dve, lambda: V_.scalar_tensor_tensor(xT[:, f, ts], psb[bank][:, :], P[:, 2, f:f + 1], xT[:, f, ts], op0=ALU.mult, op1=ALU.add),
                              reads=[ps_b[bank], xT_b[tc]], writes=[xT_b[tc]])

        def phase_F(l, b):
            P = PRM[(l, b)]
            w1 = [vb(136 + 32 * i, 8 * 1024).rearrange("p (k n) -> p k n", k=8) for i in range(2)]
            w2 = [vb(152 + 32 * i, 8 * 1024).rearrange("p (k n) -> p k n", k=8) for i in range(2)]
            w_b = [Buf("ffw0"), Buf("ffw1")]
            hT = [vb(56 + i, 512) for i in range(8)]
            h_b = Buf("hT")
            rT = [TMP[4 + i] for i in range(4)]
            r_b = [TMP_b[4 + i] for i in range(4)]
            for cg in range(4):
                i = cg % 2
                for hf in range(2):
                    fw.dma(pool, w1[i][:, :, hf * 512:(hf + 1) * 512],
                           wff1_d[l][:, cg * 1024 + hf * 512:cg * 1024 + (hf + 1) * 512].rearrange("(k p) n -> p k n", p=128), writes=[w_b[i]])
                for hf in range(2):
                    fw.dma(pool, w2[i][:, :, hf * 512:(hf + 1) * 512],
                           wff2_d[l][cg * 1024:(cg + 1) * 1024, hf * 512:(hf + 1) * 512].rearrange("(k p) n -> p k n", p=128), writes=[w_b[i]])
                for tc in range(4):
                    ts = slice(tc * 512, (tc + 1) * 512)
                    for c in range(8):
                        bank = c % 2
                        for k in range(8):
                            fw.op(pe, lambda: T.matmul(psb[bank][:, :], w1[i][:, k, c * 128:(c + 1) * 128], uT[:, k, ts], start=(k == 0), stop=(k == 7)),
                                  reads=[w_b[i], uT_b[tc]], writes=[ps_b[bank]], inc=(k == 7))
                        r, rb = rT[c % 4], r_b[c % 4]
                        fw.op(act, lambda: Sc.activation(r, psb[bank][:, :], AF.Relu), reads=[ps_b[bank]], writes=[rb])
                        fw.op(pool, lambda: G.tensor_tensor(hT[c], r, r, op=ALU.mult), reads=[rb], writes=[h_b])
                    for f in range(8):
                        bank = 2 + (f % 6)
                        for c in range(8):
                            fw.op(pe, lambda: T.matmul(psb[bank][:, :], w2[i][:, c, f * 128:(f + 1) * 128], hT[c], start=(c == 0), stop=(c == 7)),
                                  reads=[w_b[i], h_b], writes=[ps_b[bank]], inc=(c == 7))
                        fw.op(dve, lambda: V_.scalar_tensor_tensor(xT[:, f, ts], psb[bank][:, :], P[:, 5, f:f + 1], xT[:, f, ts], op0=ALU.mult, op1=ALU.add),
                              reads=[ps_b[bank], xT_b[tc]], writes=[xT_b[tc]])

        def load_x(b):
            xin = [vf(56 + 4 * i, 1024) for i in range(2)]
            xin_b = [Buf("xin0"), Buf("xin1")]
            for tb in range(16):
                i = tb % 2
                fw.dma(sp, xin[i], x_d[b, tb * 128:(tb + 1) * 128, :], writes=[xin_b[i]])
                for half in range(2):
                    bank = 2 * i + half
                    for j in range(4):
                        kc = half * 4 + j
                        fw.op(pe, lambda: T.transpose(psb[bank][:, j * 128:(j + 1) * 128], xin[i][:, kc * 128:(kc + 1) * 128], identF),
                              reads=[xin_b[i]], writes=[ps_b[bank]], inc=(j == 3))
                    dst = xT[:, half * 4:(half + 1) * 4, tb * 128:(tb + 1) * 128]
                    src = psb[bank][:, :].rearrange("p (a c) -> p a c", a=4)
                    if half == 0:
                        fw.op(act, lambda: Sc.copy(dst, src), reads=[ps_b[bank]], writes=[xT_b[tb // 4]])
                    else:
                        fw.op(dve, lambda: V_.tensor_copy(dst, src), reads=[ps_b[bank]], writes=[xT_b[tb // 4]])

        def store_out(b):
            ot = [vf(56 + 4 * i, 1024) for i in range(2)]
            ot_b = [Buf("ot0"), Buf("ot1")]
            toks = []
            for tb in range(16):
                i = tb % 2
                for half in range(2):
                    bank = 2 * i + half
                    for j in range(4):
                        kc = half * 4 + j
                        fw.op(pe, lambda: T.transpose(psb[bank][:, j * 128:(j + 1) * 128], xT[:, kc, tb * 128:(tb + 1) * 128], identF),
                              reads=[xT_b[tb // 4]], writes=[ps_b[bank]], inc=(j == 3))
                    dst = ot[i][:, half * 512:(half + 1) * 512]
                    if half == 0:
                        fw.op(act, lambda: Sc.copy(dst, psb[bank][:, :]), reads=[ps_b[bank]], writes=[ot_b[i]])
                    else:
                        fw.op(dve, lambda: V_.tensor_copy(dst, psb[bank][:, :]), reads=[ps_b[bank]], writes=[ot_b[i]])
                toks.append(fw.dma(sp, out_d[b, tb * 128:(tb + 1) * 128, :], ot[i], reads=[ot_b[i]]))
            return toks

        def spill_x():
            for k in range(8):
                fw.dma(sp, xsp_d[:, k, :], xT[:, k, :], reads=xT_b, writes=[xsp_b])

        def reload_x():
            for k in range(8):
                fw.dma(sp, xT[:, k, :], xsp_d[:, k, :], reads=[xsp_b], writes=xT_b)

        out_toks = []
        for b in range(nseq):
            load_x(b)
            fw.barrier()
            P0 = PRM[(0, b)]
            norm_mod(P0[:, 0, :], P0[:, 1, :])
            if b == 0:
                dump("u0", uT, [128, 8, S], BF16, uT_b)
            spill_x()
            fw.barrier()
            for l in range(L):
                if stop_after != "pre" and "A" not in SKIP:
                    load_strips(range(0, 4))
                    fw.barrier()
                    phase_A(l)
                    fw.barrier()
                    if b == 0 and l == 0:
                        dump("oA", oA, [128, 4, S], BF16, [oA_b])
                if stop_after not in ("pre", "A") and "B" not in SKIP:
                    load_strips(range(4, 8))
                    fw.barrier()
                    phase_B(l)
                    fw.barrier()
                    if b == 0 and l == 0:
                        dump("oB", oB, [128, 2, S], BF16, [oB_b])
                if stop_after not in ("pre", "A", "B"):
                    load_strips(range(8, 12))
                    fw.barrier()
                    phase_C(l)
                    fw.barrier()
                    if b == 0 and l == 0:
                        dump("oC", oC, [128, 2, S], BF16, [oC_b])
                reload_x()
                fw.barrier()
                if stop_after not in ("pre", "A", "B", "C"):
                    phase_M(l, b)
                    fw.barrier()
                    if b == 0 and l == 0:
                        dump("x1", xT, [128, 8, S], F32, xT_b)
                    P = PRM[(l, b)]
                    norm_mod(P[:, 3, :], P[:, 4, :])
                    fw.barrier()
                    phase_F(l, b)
                    fw.barrier()
                    if b == 0 and l == 0:
                        dump("x2", xT, [128, 8, S], F32, xT_b)
                if l + 1 < L:
                    Pn = PRM[(l + 1, b)]
                    norm_mod(Pn[:, 0, :], Pn[:, 1, :])
                    spill_x()
                    fw.barrier()
            norm_mod(nfin, None)
            fw.barrier()
            out_toks += store_out(b)
            fw.barrier()
        fw._wait(sp, out_toks)
        fw.barrier()
        stats = {e.name: e.ninst for e in fw.engs}
    return nc, dbg_out, stats


_CONSTS = None


def _consts():
    global _CONSTS
    if _CONSTS is None:
        ident = np.eye(128, dtype=np.float32)
        anti = np.ascontiguousarray(ident[::-1])
        caus = np.where(np.arange(128)[None, :] <= np.arange(128)[:, None], 0.0, -1e30).astype(np.float32)
        sel = np.zeros((32, 32, 128), np.float32)
        for r in range(32):
            sel[r, r, :] = 1.0
        bm = np.zeros((8, 4, 8), np.float32)
        own = np.zeros((8, 4, 8), np.float32)
        for j in range(8):
            bm[j, :, j:] = -1e30
            own[j, :, j] = 1.0
        _CONSTS = dict(k_ident=ident, k_anti=anti, k_onehot=_t5_onehot(), k_caus=caus,
                       k_sel=sel.reshape(32, 32 * 128), k_bm=bm.reshape(-1), k_own=own.reshape(-1))
    return _CONSTS


def make_in_maps(inputs, n_cores, nseq, nlayer=2):
    f = lambda a: np.ascontiguousarray(np.asarray(a, dtype=np.float32))
    L = nlayer
    x = f(inputs["x"])
    c = f(inputs["c"])
    shared = dict(
        rel_bias=f(inputs["rel_bias"]),
        ada_w=f(inputs["ada_w"])[:L],
        ada_bT=f(f(inputs["ada_b"])[:L].reshape(L, 48, 128).transpose(0, 2, 1)),
        norm_mixT=f(f(inputs["norm_mix"])[:L].reshape(L, 8, 128).transpose(0, 2, 1)),
        w_in=f(inputs["w_in"])[:L],
        gate_bT=f(f(inputs["gate_b"])[:L].reshape(L, 24, 128).transpose(0, 2, 1)),
        diff_lambda=f(inputs["diff_lambda"])[:L].reshape(L, 256),
        diff_subln=f(inputs["diff_subln"])[:L].reshape(L, 128, 1),
        dsa_kv_norm=f(inputs["dsa_kv_norm"])[:L].reshape(L, 128, 1),
        dsa_w_uv=f(inputs["dsa_w_uv"])[:L],
        w_br_a=f(inputs["w_br_a"])[:L], w_br_b=f(inputs["w_br_b"])[:L], w_br_c=f(inputs["w_br_c"])[:L],
        w_o=f(inputs["w_o"])[:L],
        norm_mlpT=f(f(inputs["norm_mlp"])[:L].reshape(L, 8, 128).transpose(0, 2, 1)),
        w_ff1=f(inputs["w_ff1"])[:L], w_ff2=f(inputs["w_ff2"])[:L],
        norm_finalT=f(f(inputs["norm_final"]).reshape(8, 128).T),
    )
    shared.update(_consts())
    maps = []
    for i in range(n_cores):
        m = dict(shared)
        m["x"] = f(x[i * nseq:(i + 1) * nseq])
        m["c_lay"] = f(c[i * nseq:(i + 1) * nseq].reshape(nseq, 8, 128).transpose(2, 1, 0))
        maps.append(m)
    return maps


def kernel(**inputs):
    n_cores, nseq = 8, 2
    nc, _, _ = build(nseq=nseq, nlayer=2)
    maps = make_in_maps(inputs, n_cores, nseq)
    res = run_bass_kernel_spmd(nc, maps, core_ids=list(range(n_cores)))
    out = np.concatenate([np.asarray(r["out"]) for r in res.results], axis=0)
    return out.astype(np.float32)
```

```python
import contextlib
import math
import numpy as np
import concourse.bass as bass
import concourse.mybir as mybir
from concourse.bass_utils import run_bass_kernel_spmd

F32 = mybir.dt.float32
BF16 = mybir.dt.bfloat16
U32 = mybir.dt.uint32
AF = mybir.ActivationFunctionType
ALU = mybir.AluOpType
AX = mybir.AxisListType

S = 2048
D = 1024
NEG = -30000.0
EPS = 1e-6
O_AQ, O_AK, O_AV, O_BQ, O_BKV, O_BIQ, O_BIK, O_BIW, O_CQ, O_CK, O_CV, O_G = (
    0, 512, 1024, 1536, 2048, 2176, 2432, 2464, 2472, 2728, 2984, 3240)
IN_COLS = 6312
BIS_ITERS = 12
SAME_DIST = 1 << 30
CSTOP = 0
SKIP = ()


class Buf:
    __slots__ = ("name", "w", "r")

    def __init__(self, name=""):
        self.name = name
        self.w = None
        self.r = {}


class Eng:
    def __init__(self, name, h, same_sync):
        self.name = name
        self.h = h
        self.sem = None
        self.count = 0
        self.seen = {}
        self.same_sync = same_sync
        self.ninst = 0
        self.pos = 0


class Fw:
    def __init__(self, nc, stack, n_dma_sems=8, same_sync=True):
        self.nc = nc
        self.pe = Eng("pe", nc.tensor, False)
        self.act = Eng("act", nc.scalar, same_sync)
        self.dve = Eng("dve", nc.vector, same_sync)
        self.pool = Eng("pool", nc.gpsimd, same_sync)
        self.sp = Eng("sp", nc.sync, False)
        self.engs = [self.pe, self.act, self.dve, self.pool, self.sp]
        for e in self.engs:
            e.sem = stack.enter_context(nc.semaphore("s_" + e.name))
        self.dma_pools = {}
        for q in ("sp", "pool"):
            sems = [stack.enter_context(nc.semaphore("d_%s%d" % (q, i))) for i in range(n_dma_sems)]
            self.dma_pools[q] = dict(sems=sems, cnt=[0] * n_dma_sems, nxt=0)

    def _need(self, eng, tok):
        sem, val, owner = tok[0], tok[1], tok[2]
        if owner is eng:
            if not eng.same_sync:
                return False
            if eng.pos - tok[3] >= SAME_DIST:
                return False
        return eng.seen.get(id(sem), 0) < val

    def _wait(self, eng, toks):
        best = {}
        for t in toks:
            if t is None or not self._need(eng, t):
                continue
            k = id(t[0])
            if k not in best or best[k][1] < t[1]:
                best[k] = t
        for k, t in best.items():
            eng.h.wait_ge(t[0], t[1])
            eng.seen[k] = t[1]
            eng.ninst += 1

    @staticmethod
    def _deps(reads, writes):
        toks = []
        for b in reads:
            toks.append(b.w)
        for b in writes:
            toks.append(b.w)
            toks.extend(b.r.values())
        return toks

    @staticmethod
    def _record(tok, reads, writes):
        k = id(tok[0])
        for b in reads:
            o = b.r.get(k)
            if o is None or o[1] < tok[1]:
                b.r[k] = tok
        for b in writes:
            b.w = tok
            b.r = {}

    def op(self, eng, fn, reads=(), writes=(), inc=True, nosync=False):
        toks = self._deps(reads, writes)
        if nosync:
            toks = [t for t in toks if t is not None and t[2] is not eng]
        self._wait(eng, toks)
        ins = fn()
        eng.ninst += 1
        eng.pos += 1
        if inc:
            ins.then_inc(eng.sem, 1)
            eng.count += 1
            tok = (eng.sem, eng.count, eng, eng.pos)
        else:
            tok = (eng.sem, eng.count + 1, eng, eng.pos + 1)
        self._record(tok, reads, writes)
        return tok

    def dma(self, eng, out, in_, reads=(), writes=(), **kw):
        pool = self.dma_pools[eng.name]
        self._wait(eng, self._deps(reads, writes))
        j = pool["nxt"]
        pool["nxt"] = (j + 1) % len(pool["sems"])
        sem = pool["sems"][j]
        if pool["cnt"][j] > 0 and eng.seen.get(id(sem), 0) < pool["cnt"][j]:
            eng.h.wait_ge(sem, pool["cnt"][j])
            eng.seen[id(sem)] = pool["cnt"][j]
        ins = eng.h.dma_start(out=out, in_=in_, **kw)
        ins.then_inc(sem, 16)
        eng.ninst += 1
        pool["cnt"][j] += 16
        tok = (sem, pool["cnt"][j], None, 0)
        self._record(tok, reads, writes)
        return tok

    def barrier(self):
        sp = self.sp
        toks = []
        for e in self.engs:
            if e is not sp and e.count > 0:
                toks.append((e.sem, e.count, e, e.pos))
        for q, pool in self.dma_pools.items():
            for j, sem in enumerate(pool["sems"]):
                if pool["cnt"][j] > 0:
                    toks.append((sem, pool["cnt"][j], None, 0))
        self._wait(sp, toks)
        ins = sp.h.nop()
        ins.then_inc(sp.sem, 1)
        sp.count += 1
        tok = (sp.sem, sp.count, sp, sp.pos)
        for e in self.engs:
            if e is sp:
                continue
            e.h.wait_ge(sp.sem, sp.count)
            e.seen[id(sp.sem)] = sp.count
            for t in toks:
                k = id(t[0])
                if e.seen.get(k, 0) < t[1]:
                    e.seen[k] = t[1]
        return tok


def _t5_onehot():
    dd = np.arange(1280, dtype=np.int64) - 511
    n = np.maximum(dd, 0)
    nf = np.maximum(n, 1).astype(np.float32)
    large = 16 + (np.log(nf / np.float32(16)) / np.float32(math.log(128 / 16)) * np.float32(16)).astype(np.int32)
    large = np.minimum(large, 31)
    bucket = np.where(n < 16, n, large)
    oh = np.zeros((33, 1280), np.float32)
    for j in range(1280):
        if dd[j] < 0:
            oh[32, j] = 1.0
        else:
            oh[bucket[j], j] = 1.0
    return oh


def build(nseq=2, nlayer=2, dbg=(), stop_after=None, same_sync=True):
    nc = bass.Bass("TRN2", target_bir_lowering=False)
    L = nlayer

    def din(name, shape, dt=F32):
        return nc.dram_tensor(name, list(shape), dt, kind="ExternalInput").ap()

    x_d = din("x", [nseq, S, D])
    c_d = din("c_lay", [128, 8, nseq])
    relb_d = din("rel_bias", [32, 12])
    adaw_d = din("ada_w", [L, D, 6 * D])
    adab_d = din("ada_bT", [L, 128, 48])
    nmix_d = din("norm_mixT", [L, 128, 8])
    win_d = din("w_in", [L, D, IN_COLS])
    gateb_d = din("gate_bT", [L, 128, 24])
    dlam_d = din("diff_lambda", [L, 256])
    subln_d = din("diff_subln", [L, 128, 1])
    kvn_d = din("dsa_kv_norm", [L, 128, 1])
    wuv_d = din("dsa_w_uv", [L, 4, 128, 64])
    wbra_d = din("w_br_a", [L, 512, D])
    wbrb_d = din("w_br_b", [L, 256, D])
    wbrc_d = din("w_br_c", [L, 256, D])
    wo_d = din("w_o", [L, D, D])
    nmlp_d = din("norm_mlpT", [L, 128, 8])
    wff1_d = din("w_ff1", [L, D, 4 * D])
    wff2_d = din("w_ff2", [L, 4 * D, D])
    nfin_d = din("norm_finalT", [128, 8])
    ident_d = din("k_ident", [128, 128])
    anti_d = din("k_anti", [128, 128])
    oh_d = din("k_onehot", [33, 1280])
    caus_d = din("k_caus", [128, 128])
    sel_d = din("k_sel", [32, 32 * 128])
    bm_d = din("k_bm", [8 * 32])
    own_d = din("k_own", [8 * 32])
    out_d = nc.dram_tensor("out", [nseq, S, D], F32, kind="ExternalOutput").ap()
    g_d = nc.dram_tensor("g_scr", [12, 1280], BF16, kind="Internal").ap()
    xsp_d = nc.dram_tensor("x_spill", [128, 8, S], F32, kind="Internal").ap()
    dbg_out = {}

    with contextlib.ExitStack() as st:
        fw = Fw(nc, st, same_sync=same_sync)
        pe, act, dve, pool, sp = fw.pe, fw.act, fw.dve, fw.pool, fw.sp
        T, V_, Sc, G = nc.tensor, nc.vector, nc.scalar, nc.gpsimd
        arena = st.enter_context(nc.sbuf_tensor("arena", [128, 51200], F32))
        psb = [st.enter_context(nc.psum_tensor("ps%d" % i, [128, 512], F32)) for i in range(8)]
        ps_b = [Buf("ps%d" % i) for i in range(8)]

        KW = 256

        def vf(off_k, nwords, parts=128, p0=0):
            o = int(round(off_k * KW))
            return arena[p0:p0 + parts, o:o + nwords]

        def vb(off_k, nelem, parts=128, p0=0):
            o = int(round(off_k * KW))
            return arena[p0:p0 + parts, o:o + nelem // 2].bitcast(BF16)

        identF = vf(0, 128)
        identB = vb(0.5, 128)
        antiB = vb(0.75, 128)
        onesB = vb(1.0, 128)
        epsT = vf(1.25, 1)
        halfT = vf(1.25, 1)
        neglam = [vf(1.26 + 0.01 * l, 1) for l in range(L)]
        def wv(word, n, parts=128):
            return arena[0:parts, word:word + n]
        W0 = 330
        epsT = wv(W0, 1)
        neglam = [wv(W0 + 1 + l, 1) for l in range(L)]
        subg = [wv(W0 + 4 + l, 1) for l in range(L)]
        kvg = [wv(W0 + 8 + l, 1) for l in range(L)]
        nfin = wv(W0 + 12, 8)
        zeroT = wv(W0 + 20, 1)
        c31 = wv(1000, 12)
        gateb = [wv(W0 + 24 + 24 * l, 24) for l in range(L)]
        PRM = {}
        w = W0 + 80
        for l in range(L):
            for b in range(nseq):
                PRM[(l, b)] = wv(w, 48).rearrange("p (j k) -> p j k", j=6)
                w += 48
        assert w <= 1024
        uT = vb(4, 8 * S).rearrange("p (k t) -> p k t", k=8)
        WST = [vb(36 + 8 * i, 8 * 512).rearrange("p (k n) -> p k n", k=8) for i in range(2)]
        PT = [vb(52 + i, 512) for i in range(4)]
        TMP = [vf(56 + 2 * i, 512) for i in range(8)]
        uT_b = [Buf("uT%d" % i) for i in range(4)]
        WST_b = [Buf("wst%d" % i) for i in range(2)]
        PT_b = [Buf("pt%d" % i) for i in range(4)]
        TMP_b = [Buf("tmp%d" % i) for i in range(8)]
        xT = vf(72, 8 * S).rearrange("p (k t) -> p k t", k=8)
        xT_b = [Buf("xT%d" % i) for i in range(4)]
        STR = vb(72, 12 * 1152).rearrange("p (h n) -> p h n", h=12)
        ARENA_K = 99
        merged = vb(136, 8 * S).rearrange("p (k t) -> p k t", k=8)
        merged_b = [Buf("mg%d" % i) for i in range(4)]
        oA = vb(168, 4 * S).rearrange("p (k t) -> p k t", k=4)
        oB = vb(184, 2 * S).rearrange("p (k t) -> p k t", k=2)
        oC = vb(192, 2 * S).rearrange("p (k t) -> p k t", k=2)
        oA_b, oB_b, oC_b = Buf("oA"), Buf("oB"), Buf("oC")
        xsp_b = Buf("xsp")
        gd_b = Buf("gd")

        state = {"pt": 0, "tmp": 0, "wst": 0, "lg": 0}

        def next_pt():
            i = state["pt"]
            state["pt"] = (i + 1) % 4
            return PT[i], PT_b[i]

        def next_tmp(avoid=()):
            i = state["tmp"]
            while any(TMP_b[i] is a for a in avoid):
                i = (i + 1) % 8
            state["tmp"] = (i + 1) % 8
            return TMP[i], TMP_b[i]

        def next_wst():
            i = state["wst"]
            state["wst"] = (i + 1) % 2
            return WST[i], WST_b[i]

        def dump(name, ap, shape, dt, reads):
            if name not in dbg:
                return
            d = nc.dram_tensor("dbg_" + name, list(shape), dt, kind="ExternalOutput").ap()
            dbg_out[name] = d
            fw.barrier()
            t = fw.dma(sp, d, ap, reads=reads)
            fw._wait(sp, [t])

        def load_w_slab(src_ap, ncols, eng=None):
            slab, sb = next_wst()
            fw.dma(pool, slab[:, :, 0:ncols], src_ap.rearrange("(k p) n -> p k n", p=128), writes=[sb])
            return slab, sb

        cb = Buf("consts")
        fw.dma(sp, identF, ident_d, writes=[cb])
        fw.dma(pool, identB, ident_d, writes=[cb])
        fw.dma(pool, antiB, anti_d, writes=[cb])
        fw.op(dve, lambda: V_.memset(onesB, 1.0), writes=[cb])
        fw.op(dve, lambda: V_.memset(epsT, EPS), writes=[cb])
        fw.op(dve, lambda: V_.memset(zeroT, 0.0), writes=[cb])
        fw.dma(sp, nfin, nfin_d, writes=[cb])
        fw.dma(sp, c31, relb_d[31].partition_broadcast(128), writes=[cb])
        for l in range(L):
            fw.dma(sp, subg[l], subln_d[l], writes=[cb])
            fw.dma(sp, kvg[l], kvn_d[l], writes=[cb])
            fw.dma(sp, gateb[l], gateb_d[l], writes=[cb])
        fw.barrier()
        sa = 72
        relb = vf(sa, 12, parts=33)
        ohT = vf(sa + 1, 1280, parts=33)
        grow = vb(sa + 7, 1280, parts=12)
        tb_ = Buf("setup")
        fw.op(dve, lambda: V_.memset(vf(sa, 12, parts=64)[32:64, :], NEG), writes=[tb_])
        fw.dma(sp, relb[0:32, :], relb_d, writes=[tb_])
        fw.dma(sp, ohT, oh_d, writes=[tb_])
        for ci, (c0, cn) in enumerate(((0, 512), (512, 512), (1024, 256))):
            fw.op(pe, lambda: T.matmul(psb[ci][0:12, 0:cn], relb, ohT[:, c0:c0 + cn], start=True, stop=True),
                  reads=[tb_], writes=[ps_b[ci]])
            fw.op(dve, lambda: V_.tensor_copy(grow[:, c0:c0 + cn], psb[ci][0:12, 0:cn]), reads=[ps_b[ci]], writes=[tb_])
        fw.dma(sp, g_d, grow, reads=[tb_], writes=[gd_b])
        for l in range(L):
            lam_init = 0.8 - 0.6 * math.exp(-0.3 * l)
            dl = vf(sa + 12, 256)
            pr = vf(sa + 13, 128)
            s12 = vf(sa + 14, 2)
            e12 = vf(sa + 14.5, 2)
            fw.dma(sp, dl, dlam_d[l].partition_broadcast(128), writes=[tb_])
            fw.op(dve, lambda: V_.tensor_tensor(pr[:, 0:64], dl[:, 0:64], dl[:, 64:128], op=ALU.mult), reads=[tb_], writes=[tb_])
            fw.op(dve, lambda: V_.tensor_tensor(pr[:, 64:128], dl[:, 128:192], dl[:, 192:256], op=ALU.mult), reads=[tb_], writes=[tb_])
            fw.op(dve, lambda: V_.tensor_reduce(s12, pr.rearrange("p (a b) -> p a b", a=2), axis=AX.X, op=ALU.add), reads=[tb_], writes=[tb_])
            fw.op(act, lambda: Sc.activation(e12, s12, AF.Exp), reads=[tb_], writes=[tb_])
            fw.op(dve, lambda: V_.tensor_tensor(s12[:, 0:1], e12[:, 1:2], e12[:, 0:1], op=ALU.subtract), reads=[tb_], writes=[tb_])
            fw.op(dve, lambda: V_.tensor_scalar(neglam[l], s12[:, 0:1], -lam_init, None, op0=ALU.add), reads=[tb_], writes=[tb_])
            fw.op(dve, lambda: V_.tensor_scalar(subg[l], subg[l], 1.0 - lam_init, None, op0=ALU.mult), reads=[tb_], writes=[tb_])
        cT = vf(sa + 16, 8 * nseq).rearrange("p (k b) -> p k b", k=8)
        modT = vf(sa + 17, 48 * nseq).rearrange("p (f b) -> p f b", f=48)
        adab = vf(sa + 18, 48)
        nmx = vf(sa + 19, 8)
        nml = vf(sa + 19.5, 8)
        fw.dma(sp, cT, c_d, writes=[tb_])
        fw.op(act, lambda: Sc.activation(cT, cT, AF.Silu), reads=[tb_], writes=[tb_])
        cTb = vb(sa + 16.25, 8 * nseq).rearrange("p (k b) -> p k b", k=8)
        fw.op(dve, lambda: V_.tensor_copy(cTb, cT), reads=[tb_], writes=[tb_])
        slabs = [vb(136 + 8 * i, 8 * 512).rearrange("p (k n) -> p k n", k=8) for i in range(4)]
        slab_b = [Buf("adaslab%d" % i) for i in range(4)]
        si = 0
        for l in range(L):
            fw.dma(sp, adab, adab_d[l], writes=[tb_])
            fw.dma(sp, nmx, nmix_d[l], writes=[tb_])
            fw.dma(sp, nml, nmlp_d[l], writes=[tb_])
            for sl in range(12):
                sb_, sbb = slabs[si % 4], slab_b[si % 4]
                si += 1
                fw.dma(pool, sb_, adaw_d[l][:, sl * 512:(sl + 1) * 512].rearrange("(k p) n -> p k n", p=128), writes=[sbb])
                bank = 4 + (sl % 2)
                for fc4 in range(4):
                    for k in range(8):
                        fw.op(pe, lambda: T.matmul(psb[bank][:, fc4 * nseq:(fc4 + 1) * nseq], sb_[:, k, fc4 * 128:(fc4 + 1) * 128], cTb[:, k, :],
                                                   start=(k == 0), stop=(k == 7)),
                              reads=[sbb, tb_], writes=[ps_b[bank]], inc=(k == 7 and fc4 == 3))
                for fc4 in range(4):
                    fc = sl * 4 + fc4
                    fw.op(dve, lambda: V_.tensor_scalar(modT[:, fc, :], psb[bank][:, fc4 * nseq:(fc4 + 1) * nseq], adab[:, fc:fc + 1], None, op0=ALU.add),
                          reads=[ps_b[bank], tb_], writes=[tb_])
            for b in range(nseq):
                P = PRM[(l, b)]
                fw.op(dve, lambda: V_.scalar_tensor_tensor(P[:, 0, :], modT[:, 8:16, b], 1.0, nmx, op0=ALU.add, op1=ALU.mult), reads=[tb_], writes=[tb_])
                fw.op(dve, lambda: V_.tensor_copy(P[:, 1, :], modT[:, 0:8, b]), reads=[tb_], writes=[tb_])
                fw.op(dve, lambda: V_.tensor_copy(P[:, 2, :], modT[:, 16:24, b]), reads=[tb_], writes=[tb_])
                fw.op(dve, lambda: V_.scalar_tensor_tensor(P[:, 3, :], modT[:, 32:40, b], 1.0, nml, op0=ALU.add, op1=ALU.mult), reads=[tb_], writes=[tb_])
                fw.op(dve, lambda: V_.tensor_copy(P[:, 4, :], modT[:, 24:32, b]), reads=[tb_], writes=[tb_])
                fw.op(dve, lambda: V_.tensor_copy(P[:, 5, :], modT[:, 40:48, b]), reads=[tb_], writes=[tb_])
        fw.barrier()

        def rstd_from_ss(ss_bank, inv_n):
            t1, t1b = next_tmp()
            fw.op(act, lambda: Sc.activation(t1, psb[ss_bank][:, :], AF.Ln, bias=epsT, scale=inv_n), reads=[ps_b[ss_bank]], writes=[t1b])
            t2, t2b = next_tmp()
            fw.op(act, lambda: Sc.activation(t2, t1, AF.Exp, scale=-0.5), reads=[t1b], writes=[t2b])
            return t2, t2b

        def norm_mod(Aap, Bap):
            for tc in range(4):
                ts = slice(tc * 512, (tc + 1) * 512)
                bank = 6 + (tc % 2)
                for k in range(8):
                    sq, sqb = next_pt()
                    fw.op(act, lambda: Sc.activation(sq, xT[:, k, ts], AF.Square), reads=[xT_b[tc]], writes=[sqb])
                    fw.op(pe, lambda: T.matmul(psb[bank][:, :], onesB, sq, start=(k == 0), stop=(k == 7)),
                          reads=[sqb], writes=[ps_b[bank]], inc=True)
                rstd, rb = rstd_from_ss(bank, 1.0 / D)
                for k in range(8):
                    t1, t1b = next_tmp(avoid=(rb,))
                    fw.op(dve, lambda: V_.tensor_tensor(t1, xT[:, k, ts], rstd, op=ALU.mult), reads=[xT_b[tc], rb], writes=[t1b])
                    if Bap is not None:
                        fw.op(act, lambda: Sc.activation(uT[:, k, ts], t1, AF.Identity, bias=Bap[:, k:k + 1], scale=Aap[:, k:k + 1]),
                              reads=[t1b], writes=[uT_b[tc]])
                    else:
                        fw.op(act, lambda: Sc.activation(xT[:, k, ts], t1, AF.Identity, bias=zeroT, scale=Aap[:, k:k + 1]),
                              reads=[t1b], writes=[xT_b[tc]])

        def proj_fm(slab, sb, col0, ncols, dst_fn, dst_bufs, scale=None, banks=(6, 7), post=None, dst2_fn=None):
            for tc in range(4):
                bank = banks[tc % len(banks)]
                for k in range(8):
                    fw.op(pe, lambda: T.matmul(psb[bank][0:ncols, :], slab[:, k, col0:col0 + ncols], uT[:, k, tc * 512:(tc + 1) * 512],
                                               start=(k == 0), stop=(k == 7)),
                          reads=[sb, uT_b[tc]], writes=[ps_b[bank]], inc=(k == 7))
                xr = []
                if post is not None:
                    xr = post(tc, bank)
                if dst2_fn is not None:
                    fw.op(act, lambda: Sc.copy(dst_fn(tc), psb[bank][0:64, :]), reads=[ps_b[bank]] + xr, writes=dst_bufs)
                    fw.op(act, lambda: Sc.copy(dst2_fn(tc), psb[bank][64:128, :]), reads=[ps_b[bank]] + xr, writes=dst_bufs)
                elif scale is None:
                    fw.op(act, lambda: Sc.copy(dst_fn(tc), psb[bank][0:ncols, :]), reads=[ps_b[bank]] + xr, writes=dst_bufs)
                else:
                    fw.op(act, lambda: Sc.activation(dst_fn(tc), psb[bank][0:ncols, :], AF.Copy, scale=scale), reads=[ps_b[bank]] + xr, writes=dst_bufs)

        def proj_tm(slab, sb, col0, ncols, dst_fn, dst_bufs, banks=(6, 7), dtype_copy=True):
            for tb in range(16):
                bank = banks[tb % len(banks)]
                for k in range(8):
                    fw.op(pe, lambda: T.matmul(psb[bank][:, 0:ncols], uT[:, k, tb * 128:(tb + 1) * 128], slab[:, k, col0:col0 + ncols],
                                               start=(k == 0), stop=(k == 7)),
                          reads=[sb, uT_b[tb // 4]], writes=[ps_b[bank]], inc=(k == 7))
                e = dve if tb % 2 else act
                if e is dve:
                    fw.op(dve, lambda: V_.tensor_copy(dst_fn(tb), psb[bank][:, 0:ncols]), reads=[ps_b[bank]], writes=dst_bufs)
                else:
                    fw.op(act, lambda: Sc.copy(dst_fn(tb), psb[bank][:, 0:ncols]), reads=[ps_b[bank]], writes=dst_bufs)

        def attn_core(tc, k_fn, q_ap, strip_h, v_fn, o_ap, sum_ap, o_bank, s_bank, ones_ap, reads, extra=None,
                      first=True, last_grp=True):
            nkb = 4 * tc + 4
            lbanks = (0, 1)

            def qk(kb):
                bank = lbanks[kb % 2]
                dl_ = min(4 * tc - kb, 2)
                c0 = (dl_ + 3) * 128
                cl = max(kb - 4 * tc, 0) * 128
                far = dl_ >= 2
                has_extra = extra is not None and extra(kb, bank, True, cl)
                only = far and not has_extra
                fw.op(pe, lambda: T.matmul(psb[bank][:, cl:512], k_fn(kb), q_ap[:, cl:512], start=True, stop=only),
                      reads=reads, writes=[ps_b[bank]], inc=only)
                if not far:
                    fw.op(pe, lambda: T.matmul(psb[bank][:, cl:512], antiB, STR[:, strip_h, c0 + cl:c0 + 512], start=False, stop=not has_extra),
                          reads=[], writes=[ps_b[bank]], inc=not has_extra)
                if has_extra:
                    extra(kb, bank, False, cl)
                return bank, far, cl

            pend = qk(0)
            for kb in range(nkb):
                bank, far, cl = pend
                if kb + 1 < nkb:
                    pend = qk(kb + 1)
                p, pb_ = next_pt()
                if far:
                    fw.op(act, lambda: Sc.activation(p[:, cl:512], psb[bank][:, cl:512], AF.Exp, bias=c31[:, strip_h:strip_h + 1]), reads=[ps_b[bank]], writes=[pb_])
                else:
                    fw.op(act, lambda: Sc.activation(p[:, cl:512], psb[bank][:, cl:512], AF.Exp), reads=[ps_b[bank]], writes=[pb_])
                st_ = first and kb == 0
                sp_ = last_grp and kb == nkb - 1
                fw.op(pe, lambda: T.matmul(o_ap[:, cl:512], v_fn(kb), p[:, cl:512], start=st_, stop=sp_),
                      reads=reads + [pb_], writes=[ps_b[o_bank]], inc=sp_)
                fw.op(pe, lambda: T.matmul(sum_ap[:, cl:512], ones_ap, p[:, cl:512], start=st_, stop=sp_),
                      reads=[pb_], writes=[ps_b[s_bank]], inc=True)

        def recip_sum(s_bank, sum_ap, parts=128, p0=0):
            t1, t1b = next_tmp()
            t1v = t1[p0:p0 + parts, :]
            fw.op(act, lambda: Sc.activation(t1v, sum_ap, AF.Ln), reads=[ps_b[s_bank]], writes=[t1b])
            t2, t2b = next_tmp()
            t2v = t2[p0:p0 + parts, :]
            fw.op(act, lambda: Sc.activation(t2v, t1v, AF.Exp, scale=-1.0), reads=[t1b], writes=[t2b])
            return t2v, t2b

        def load_strips(heads):
            sb_ = Buf("strips")
            for h in heads:
                src = bass.AP(tensor=g_d.tensor, offset=h * 1280, ap=[[1, 128], [1, 1152]])
                fw.dma(sp, STR[:, h, :], src, reads=[gd_b], writes=[sb_])
            return sb_

        def phase_A(l):
            ak = ARENA_K
            qT = vb(ak, 4 * S).rearrange("p (h t) -> p h t", h=4)
            kTz = vb(ak + 16, 8 * S).rearrange("p (h m t) -> p h m t", h=4, m=2)
            Vt = vb(ak + 48, 16 * 512).rearrange("p (b e) -> p b e", b=16)
            q_b, k_b, v_b = Buf("qT"), Buf("kT"), Buf("Vt")
            slab, sb = load_w_slab(win_d[l][:, O_AQ:O_AQ + 512], 512)
            for h in range(4):
                proj_fm(slab, sb, h * 128, 128, lambda tc: qT[:, h, tc * 512:(tc + 1) * 512], [q_b], scale=0.125)
            fw.op(pool, lambda: G.memset(kTz[64:128, :, 0, :], 0.0), writes=[k_b])
            fw.op(pool, lambda: G.memset(kTz[0:64, :, 1, :], 0.0), writes=[k_b])
            slab, sb = load_w_slab(win_d[l][:, O_AK:O_AK + 512], 512)
            for h in range(4):
                proj_fm(slab, sb, h * 128, 128, lambda tc: kTz[0:64, h, 0, tc * 512:(tc + 1) * 512], [k_b],
                        dst2_fn=lambda tc: kTz[64:128, h, 1, tc * 512:(tc + 1) * 512])
            slab, sb = load_w_slab(win_d[l][:, O_AV:O_AV + 512], 512)
            proj_tm(slab, sb, 0, 512, lambda tb: Vt[:, tb, :], [v_b])
            sqd = [vb(ak + 64 + i, 512) for i in range(2)]
            sqd_b = [Buf("sqd0"), Buf("sqd1")]
            deferred = []
            gi = 0
            for h in range(4):
                for tc in range(4):
                    ts = slice(tc * 512, (tc + 1) * 512)
                    R = []
                    for m in range(2):
                        ob, sbk = 2 + m, 4 + m
                        attn_core(tc, lambda kb: kTz[:, h, m, kb * 128:(kb + 1) * 128], qT[:, h, ts], h,
                                  lambda kb: Vt[:, kb, h * 128:(h + 1) * 128], psb[ob][:, :], psb[sbk][:, :], ob, sbk, onesB,
                                  reads=[q_b, k_b, v_b])
                        rs, rsb = recip_sum(sbk, psb[sbk][:, :])
                        r, rb = next_tmp()
                        fw.op(dve, lambda: V_.tensor_tensor(r, psb[ob][:, :], rs, op=ALU.mult), reads=[ps_b[ob], rsb], writes=[rb])
                        R.append((r, rb))
                        if m == 0 and deferred:
                            deferred.pop()()
                    dd, ddb = next_tmp()
                    fw.op(dve, lambda: V_.scalar_tensor_tensor(dd, R[1][0], neglam[l], R[0][0], op0=ALU.mult, op1=ALU.add),
                          reads=[R[0][1], R[1][1]], writes=[ddb])
                    sq, sqb = sqd[gi % 2], sqd_b[gi % 2]
                    gi += 1
                    fw.op(act, lambda: Sc.activation(sq, dd, AF.Square), reads=[ddb], writes=[sqb])

                    def tail(dd=dd, ddb=ddb, sq=sq, sqb=sqb, h=h, ts=ts):
                        fw.op(pe, lambda: T.matmul(psb[6][:, :], onesB, sq, start=True, stop=True), reads=[sqb], writes=[ps_b[6]])
                        rstd, rb2 = rstd_from_ss(6, 1.0 / 128)
                        fw.op(dve, lambda: V_.scalar_tensor_tensor(oA[:, h, ts], dd, subg[l], rstd, op0=ALU.mult, op1=ALU.mult),
                              reads=[ddb, rb2], writes=[oA_b])
                    deferred.append(tail)
            while deferred:
                deferred.pop()()

        def phase_B(l):
            ak = ARENA_K
            ak = 90
            kvT = vb(72, S)
            kvt = vb(76, 16 * 128).rearrange("p (b r) -> p b r", b=16)
            bqT = vb(ak, 4 * S).rearrange("p (h t) -> p h t", h=4)
            iqT = vb(ak + 16, 3 * S, parts=96).rearrange("p (c t) -> p c t", c=3)
            ikT = vb(ak + 28, S, parts=96)
            scores = vf(ak + 32, S)
            MnegAll = vb(ak + 40, 8 * S).rearrange("p (u j s) -> p u j s", u=2, j=4)
            iw = vf(ak + 72, 16 * 8).rearrange("p (b h) -> p b h", b=16)
            wuv2 = vb(ak + 72.5, 4 * 128).rearrange("p (h e) -> p h e", h=4)
            caus = vf(ak + 73.5, 128)
            bis = vf(ak + 74, 16)
            ikw = vb(ak + 74.25, 8 * 96).rearrange("p (k n) -> p k n", k=8)
            bq_b, kv_b, kvt_b, iq_b, ik_b, sc_b, iw_b, wuv_b, ca_b, bis_b, ikw_b = [Buf(n) for n in
                ("bq", "kv", "kvt", "iq", "ik", "sc", "iw", "wuv", "ca", "bis", "ikw")]
            mn_bs = [[Buf("mn%d%d" % (u, j)) for j in range(4)] for u in range(2)]
            fw.dma(sp, caus, caus_d, writes=[ca_b])
            fw.op(pool, lambda: G.memset(wuv2, 0.0), writes=[wuv_b])
            for h in range(4):
                fw.dma(pool, wuv2[:, h, (h % 2) * 64:(h % 2) * 64 + 64], wuv_d[l][h], writes=[wuv_b])
            slab, sb = load_w_slab(win_d[l][:, O_BQ:O_BQ + 512], 512)
            for h in range(4):
                proj_fm(slab, sb, h * 128, 128, lambda tc: bqT[:, h, tc * 512:(tc + 1) * 512], [bq_b], scale=128 ** -0.5)
            slab, sb = load_w_slab(win_d[l][:, O_BKV:O_BKV + 424], 424)

            def kv_post(tc, bank):
                ts = slice(tc * 512, (tc + 1) * 512)
                sq, sqb = next_pt()
                fw.op(act, lambda: Sc.activation(sq, psb[bank][:, :], AF.Square), reads=[ps_b[bank]], writes=[sqb])
                fw.op(pe, lambda: T.matmul(psb[5][:, :], onesB, sq, start=True, stop=True), reads=[sqb], writes=[ps_b[5]])
                rstd, rb = rstd_from_ss(5, 1.0 / 128)
                fw.op(dve, lambda: V_.scalar_tensor_tensor(kvT[:, ts], psb[bank][:, :], kvg[l], rstd, op0=ALU.mult, op1=ALU.mult),
                      reads=[ps_b[bank], rb], writes=[kv_b])

            for tc in range(4):
                bank = 6 + (tc % 2)
                for k in range(8):
                    fw.op(pe, lambda: T.matmul(psb[bank][:, :], slab[:, k, 0:128], uT[:, k, tc * 512:(tc + 1) * 512], start=(k == 0), stop=(k == 7)),
                          reads=[sb, uT_b[tc]], writes=[ps_b[bank]], inc=(k == 7))
                kv_post(tc, bank)
            for g4 in range(4):
                bank = 6 + (g4 % 2)
                pbf = psb[bank][:, :].bitcast(BF16)
                for j in range(4):
                    tb = g4 * 4 + j
                    fw.op(pe, lambda: T.transpose(pbf[:, j * 128:(j + 1) * 128], kvT[:, tb * 128:(tb + 1) * 128], identB),
                          reads=[kv_b], writes=[ps_b[bank]], inc=(j == 3))
                fw.op(dve, lambda: V_.tensor_copy(kvt[:, g4 * 4:(g4 + 1) * 4, :], pbf[:, 0:512].rearrange("p (a b) -> p a b", a=4)),
                      reads=[ps_b[bank]], writes=[kvt_b])
            for c in range(3):
                nh = 3 if c < 2 else 2
                proj_fm(slab, sb, 128 + c * 96, nh * 32, lambda tc: iqT[0:nh * 32, c, tc * 512:(tc + 1) * 512], [iq_b])
            for j in range(3):
                fw.op(dve, lambda: V_.tensor_copy(ikw[:, :, j * 32:(j + 1) * 32], slab[:, :, 384:416]), reads=[sb], writes=[ikw_b])
            proj_fm(ikw, ikw_b, 0, 96, lambda tc: ikT[:, tc * 512:(tc + 1) * 512], [ik_b])
            proj_tm(slab, sb, 416, 8, lambda tb: iw[:, tb, :], [iw_b])

            def indexer(qb, j):
                u = (qb // 4) % 2
                Mneg = MnegAll[:, u]
                mn_b = mn_bs[u][j]
                Wd = (qb + 1) * 128
                nsc = (Wd + 511) // 512
                for sc in range(nsc):
                    cols = min(512, Wd - sc * 512)
                    cs = slice(sc * 512, sc * 512 + cols)
                    for hh in range(8):
                        c, pb = hh // 3, (hh % 3) * 32
                        bank = 6 + (hh % 2)
                        fw.op(pe, lambda: T.matmul(psb[bank][:, 0:cols], iqT[pb:pb + 32, c, qb * 128:(qb + 1) * 128], ikT[pb:pb + 32, cs],
                                                   start=True, stop=True),
                              reads=[iq_b, ik_b], writes=[ps_b[bank]])
                        fw.op(act, lambda: Sc.activation(psb[bank][:, 0:cols], psb[bank][:, 0:cols], AF.Relu), reads=[], writes=[ps_b[bank]])
                        if hh == 0:
                            fw.op(dve, lambda: V_.tensor_scalar(scores[:, cs], psb[bank][:, 0:cols], iw[:, qb, 0:1], None, op0=ALU.mult),
                                  reads=[ps_b[bank], iw_b], writes=[sc_b])
                        else:
                            fw.op(dve, lambda: V_.scalar_tensor_tensor(scores[:, cs], psb[bank][:, 0:cols], iw[:, qb, hh:hh + 1], scores[:, cs],
                                                                       op0=ALU.mult, op1=ALU.add),
                                  reads=[ps_b[bank], iw_b, sc_b], writes=[sc_b])
                dsl = slice(qb * 128, (qb + 1) * 128)
                fw.op(dve, lambda: V_.tensor_tensor(scores[:, dsl], scores[:, dsl], caus, op=ALU.add), reads=[sc_b, ca_b], writes=[sc_b])
                lo, hi, mid, cnt, w0 = (bis[:, i:i + 1] for i in range(5))
                geu = bis[:, 6:7].bitcast(U32)
                fw.op(dve, lambda: V_.tensor_reduce(hi, scores[:, 0:Wd], axis=AX.X, op=ALU.max), reads=[sc_b], writes=[bis_b])
                fw.op(dve, lambda: V_.tensor_reduce(lo, scores[:, 0:256], axis=AX.X, op=ALU.min), reads=[sc_b], writes=[bis_b])
                fw.op(dve, lambda: V_.tensor_tensor(w0, hi, lo, op=ALU.subtract), reads=[bis_b], writes=[bis_b])
                fw.op(dve, lambda: V_.tensor_scalar(w0, w0, 1.0 + 1e-5, 1e-6, op0=ALU.mult, op1=ALU.add), reads=[bis_b], writes=[bis_b])
                junk = Mneg[:, j, 0:Wd]
                for it in range(BIS_ITERS):
                    ck = 2.0 ** -(it + 1)
                    fw.op(dve, lambda: V_.scalar_tensor_tensor(mid, w0, ck, lo, op0=ALU.mult, op1=ALU.add), reads=[bis_b], writes=[bis_b])
                    fw.op(dve, lambda: V_.tensor_scalar(junk, scores[:, 0:Wd], mid, 0.0, op0=ALU.is_ge, op1=ALU.add, accum_out=cnt),
                          reads=[sc_b, bis_b], writes=[mn_b, bis_b])
                    fw.op(dve, lambda: V_.tensor_scalar(geu, cnt, 255.5, None, op0=ALU.is_ge), reads=[bis_b], writes=[bis_b])
                    fw.op(dve, lambda: V_.copy_predicated(lo, geu, mid), reads=[bis_b], writes=[bis_b])
                fw.op(dve, lambda: V_.tensor_scalar(Mneg[:, j, 0:Wd], scores[:, 0:Wd], lo, NEG, op0=ALU.is_lt, op1=ALU.mult),
                      reads=[sc_b, bis_b], writes=[mn_b])

            ohb_b = [Buf("oh%d" % i) for i in range(4)]
            for j in range(2, 4):
                indexer(j, j)
            for tc in range(4):
                ts = slice(tc * 512, (tc + 1) * 512)
                u = tc % 2
                Mneg = MnegAll[:, u]
                oh_list = []
                for h in range(4):
                    def extra(kb, bank, query_only, cl=0):
                        js = [j for j in range(4) if (4 * tc + j) >= 2 and kb <= 4 * tc + j]
                        if query_only:
                            return len(js) > 0
                        for idx, j in enumerate(js):
                            lastj = idx == len(js) - 1
                            fw.op(pe, lambda: T.matmul(psb[bank][:, j * 128:(j + 1) * 128], Mneg[:, j, kb * 128:(kb + 1) * 128], identB,
                                                       start=False, stop=lastj),
                                  reads=[mn_bs[u][j]], writes=[ps_b[bank]], inc=lastj)
                        return True
                    ob, sbk = (2, 4) if h % 2 == 0 else (3, 5)
                    attn_core(tc, lambda kb: kvT[:, kb * 128:(kb + 1) * 128], bqT[:, h, ts], 4 + h,
                              lambda kb: kvt[:, kb, :], psb[ob][:, :], psb[sbk][:, :], ob, sbk, onesB,
                              reads=[bq_b, kv_b, kvt_b], extra=extra)
                    rs, rsb = recip_sum(sbk, psb[sbk][:, :])
                    o, ob_ = vb(36 + h, 512), ohb_b[h]
                    fw.op(dve, lambda: V_.tensor_tensor(o, psb[ob][:, :], rs, op=ALU.mult), reads=[ps_b[ob], rsb], writes=[ob_, WST_b[0]])
                    oh_list.append((o, ob_))
                    if tc + 1 < 4:
                        indexer(4 * (tc + 1) + h, h)
                for pr in range(2):
                    wb_ = 6 + pr
                    for hh in range(2):
                        h = 2 * pr + hh
                        fw.op(pe, lambda: T.matmul(psb[wb_][:, :], wuv2[:, h, :], oh_list[h][0], start=(hh == 0), stop=(hh == 1)),
                              reads=[wuv_b, oh_list[h][1], WST_b[0]], writes=[ps_b[wb_]], inc=(hh == 1))
                    fw.op(act, lambda: Sc.copy(oB[:, pr, ts], psb[wb_][:, :]), reads=[ps_b[wb_]], writes=[oB_b])

        def phase_C(l):
            ak = ARENA_K
            cqT = vb(ak, 2 * S).rearrange("p (c t) -> p c t", c=2)
            ckTz = vb(ak + 8, 4 * S).rearrange("p (c h t) -> p c h t", c=2, h=2)
            cvz = vb(ak + 24, 16 * 4 * 128).rearrange("p (b c h e) -> p b c h e", b=16, c=2, h=2)
            MT = vb(ak + 40, S, parts=32)
            selc = vb(ak + 44, 32 * 128, parts=32).rearrange("p (r s) -> p r s", r=32)
            ksum = vf(ak + 52, 16).rearrange("p (c n) -> p c n", c=2)
            bm = vf(ak + 52.5, 256).rearrange("p (j n) -> p j n", j=8)
            own = vf(ak + 53.5, 256).rearrange("p (j n) -> p j n", j=8)
            gm = vf(ak + 54.5, 32)
            m8 = vf(ak + 54.75, 8)
            selo = vf(ak + 55, 32)
            Mt = vb(ak + 55.25, 32)
            kmZ = vb(ak + 55.5, 32).rearrange("p (c h n) -> p c h n", c=2, h=2)
            onesz = vb(ak + 56, 256).rearrange("p (h e) -> p h e", h=2)
            cq_b, ck_b, cv_b, mt_b, sel_b, ks_b, km_b, bm_b, gm_b, oz_b = [Buf(n) for n in ("cq", "ck", "cv", "MT", "sel", "ks", "km", "bm", "gm", "oz")]
            fw.dma(pool, selc, sel_d.rearrange("p (r s) -> p r s", r=32), writes=[sel_b], max_dma_last_dim=2048)
            fw.dma(sp, bm, bm_d.partition_broadcast(128).rearrange("p (j n) -> p j n", j=8), writes=[bm_b])
            fw.dma(sp, own, own_d.partition_broadcast(128).rearrange("p (j n) -> p j n", j=8), writes=[bm_b])
            fw.op(pool, lambda: G.memset(ckTz[64:128, :, 0, :], 0.0), writes=[ck_b])
            fw.op(pool, lambda: G.memset(ckTz[0:64, :, 1, :], 0.0), writes=[ck_b])
            fw.op(pool, lambda: G.memset(cvz, 0.0), writes=[cv_b])
            fw.op(pool, lambda: G.memset(onesz, 0.0), writes=[oz_b])
            fw.op(pool, lambda: G.memset(onesz[:, 0, 0:64], 1.0), writes=[oz_b])
            fw.op(pool, lambda: G.memset(onesz[:, 1, 64:128], 1.0), writes=[oz_b])
            slab, sb = load_w_slab(win_d[l][:, O_CQ:O_CQ + 512], 512)
            for c in range(2):
                proj_fm(slab, sb, c * 128, 128, lambda tc: cqT[:, c, tc * 512:(tc + 1) * 512], [cq_b], scale=0.125)

            for c in range(2):
                def post(tc, bank):
                    fw.op(dve, lambda: V_.tensor_reduce(ksum[:, c, 2 * tc:2 * tc + 2], psb[bank][:, :].rearrange("p (b s) -> p b s", b=2),
                                                        axis=AX.X, op=ALU.add),
                          reads=[ps_b[bank]], writes=[ks_b])
                    return [ks_b]
                proj_fm(slab, sb, 256 + c * 128, 128, lambda tc: ckTz[0:64, c, 0, tc * 512:(tc + 1) * 512], [ck_b], post=post,
                        dst2_fn=lambda tc: ckTz[64:128, c, 1, tc * 512:(tc + 1) * 512])
            fw.op(dve, lambda: V_.memset(kmZ, 0.0), writes=[km_b])
            for hh in range(2):
                pr_ = slice(hh * 64, hh * 64 + 64)
                fw.op(dve, lambda: V_.tensor_scalar(kmZ[pr_, :, hh, :], ksum[pr_, :, :], 1.0 / 256, None, op0=ALU.mult), reads=[ks_b], writes=[km_b])
            slab, sb = load_w_slab(win_d[l][:, O_CV:O_CV + 256], 256)
            for tb in range(16):
                bank = 6 + (tb % 2)
                for k in range(8):
                    fw.op(pe, lambda: T.matmul(psb[bank][:, 0:256], uT[:, k, tb * 128:(tb + 1) * 128], slab[:, k, 0:256], start=(k == 0), stop=(k == 7)),
                          reads=[sb, uT_b[tb // 4]], writes=[ps_b[bank]], inc=(k == 7))
                src = psb[bank][:, 0:256].rearrange("p (c h e) -> p c h e", c=2, h=2)
                if tb % 2:
                    fw.op(dve, lambda: V_.tensor_copy(cvz[:, tb, :, 0, 0:64], src[:, :, 0, :]), reads=[ps_b[bank]], writes=[cv_b])
                    fw.op(dve, lambda: V_.tensor_copy(cvz[:, tb, :, 1, 64:128], src[:, :, 1, :]), reads=[ps_b[bank]], writes=[cv_b])
                else:
                    fw.op(act, lambda: Sc.copy(cvz[:, tb, :, 0, 0:64], src[:, :, 0, :]), reads=[ps_b[bank]], writes=[cv_b])
                    fw.op(act, lambda: Sc.copy(cvz[:, tb, :, 1, 64:128], src[:, :, 1, :]), reads=[ps_b[bank]], writes=[cv_b])
            for qb in range(16):
                jb = qb // 2
                bank = 6 + (qb % 2)
                for h in range(4):
                    c = h // 2
                    fw.op(pe, lambda: T.matmul(psb[bank][:, h * 8:(h + 1) * 8], cqT[:, c, qb * 128:(qb + 1) * 128], kmZ[:, c, h % 2, :],
                                               start=True, stop=True),
                          reads=[cq_b, km_b], writes=[ps_b[bank]], inc=(h == 3))
                fw.op(dve, lambda: V_.tensor_tensor(gm, psb[bank][:, 0:32], bm[:, jb, :], op=ALU.add), reads=[ps_b[bank], bm_b], writes=[gm_b])
                for h in range(4):
                    fw.op(dve, lambda: V_.max(m8, gm[:, h * 8:(h + 1) * 8]), reads=[gm_b], writes=[gm_b])
                    fw.op(dve, lambda: V_.tensor_scalar(selo[:, h * 8:(h + 1) * 8], gm[:, h * 8:(h + 1) * 8], m8[:, 2:3], None, op0=ALU.is_ge),
                          reads=[gm_b], writes=[gm_b])
                fw.op(dve, lambda: V_.tensor_tensor(selo, selo, own[:, jb, :], op=ALU.max), reads=[gm_b, bm_b], writes=[gm_b])
                fw.op(dve, lambda: V_.tensor_scalar(Mt, selo, -NEG, NEG, op0=ALU.mult, op1=ALU.add), reads=[gm_b], writes=[gm_b])
                pbf = psb[bank][:, :].bitcast(BF16)
                fw.op(pe, lambda: T.transpose(pbf[0:32, 0:128], Mt, identB), reads=[gm_b], writes=[ps_b[bank]])
                fw.op(act, lambda: Sc.copy(MT[:, qb * 128:(qb + 1) * 128], pbf[0:32, 0:128]), reads=[ps_b[bank]], writes=[mt_b])
            for c in range(2):
                for tc in range(4):
                    ts = slice(tc * 512, (tc + 1) * 512)
                    for hh in range(2):
                        h = 2 * c + hh

                        def extra(kb, bank, query_only, cl=0):
                            if query_only:
                                return True
                            fw.op(pe, lambda: T.matmul(psb[bank][:, cl:512], selc[:, h * 8 + kb // 2, :], MT[:, tc * 512 + cl:(tc + 1) * 512], start=False, stop=True),
                                  reads=[sel_b, mt_b], writes=[ps_b[bank]], inc=True)
                            return True
                        ob, sbk = (2, 4) if tc % 2 == 0 else (3, 5)
                        attn_core(tc, lambda kb: ckTz[:, c, hh, kb * 128:(kb + 1) * 128], cqT[:, c, ts], 8 + h,
                                  lambda kb: cvz[:, kb, c, hh, :], psb[ob][:, :], psb[sbk][:, :], ob, sbk, onesz[:, hh, :],
                                  reads=[cq_b, ck_b, cv_b, oz_b], extra=extra, first=(hh == 0), last_grp=(hh == 1))
                    rs, rsb = recip_sum(sbk, psb[sbk][:, :])
                    fw.op(dve, lambda: V_.tensor_tensor(oC[:, c, ts], psb[ob][:, :], rs, op=ALU.mult), reads=[ps_b[ob], rsb], writes=[oC_b])

        def phase_M(l, b):
            P = PRM[(l, b)]
            wg = [vb(36 + 8 * i, 8 * 384).rearrange("p (k n) -> p k n", k=8) for i in range(2)]
            wbr = [vb(36 + 8 * i + 6, 8 * 128).rearrange("p (k n) -> p k n", k=8) for i in range(2)]
            srcs = [(oA, oA_b, 4, wbra_d, 0), (oB, oB_b, 2, wbrb_d, 4), (oC, oC_b, 2, wbrc_d, 6)]
            for mf in range(8):
                i = mf % 2
                wgi, wbi, wb_ = wg[i], wbr[i], WST_b[i]
                for j in range(3):
                    c0 = O_G + j * 1024 + mf * 128
                    fw.dma(pool, wgi[:, :, j * 128:(j + 1) * 128], win_d[l][:, c0:c0 + 128].rearrange("(k p) n -> p k n", p=128), writes=[wb_])
                for (o_, ob, nk, wd, k0) in srcs:
                    fw.dma(pool, wbi[:, k0:k0 + nk, :], wd[l][:, mf * 128:(mf + 1) * 128].rearrange("(k p) n -> p k n", p=128), writes=[wb_])
                for tc in range(4):
                    ts = slice(tc * 512, (tc + 1) * 512)
                    sig = []
                    for bi in range(3):
                        gbank = (0, 1, 6)[bi]
                        for k in range(8):
                            fw.op(pe, lambda: T.matmul(psb[gbank][:, :], wgi[:, k, bi * 128:(bi + 1) * 128], uT[:, k, ts], start=(k == 0), stop=(k == 7)),
                                  reads=[wb_, uT_b[tc]], writes=[ps_b[gbank]], inc=(k == 7))
                        sg, sgb = next_pt()
                        fw.op(act, lambda: Sc.activation(sg, psb[gbank][:, :], AF.Sigmoid, bias=gateb[l][:, bi * 8 + mf:bi * 8 + mf + 1]),
                              reads=[ps_b[gbank]], writes=[sgb])
                        sig.append((sg, sgb))
                    terms = []
                    for bi, (o_, ob, nk, wd, k0) in enumerate(srcs):
                        ybank = (2, 3, 4)[bi]
                        for k in range(nk):
                            fw.op(pe, lambda: T.matmul(psb[ybank][:, :], wbi[:, k0 + k, :], o_[:, k, ts], start=(k == 0), stop=(k == nk - 1)),
                                  reads=[wb_, ob], writes=[ps_b[ybank]], inc=(k == nk - 1))
                        tm, tmb = next_tmp()
                        fw.op(dve, lambda: V_.tensor_tensor(tm, psb[ybank][:, :], sig[bi][0], op=ALU.mult), reads=[ps_b[ybank], sig[bi][1]], writes=[tmb])
                        terms.append((tm, tmb))
                    fw.op(pool, lambda: G.tensor_tensor(terms[0][0], terms[0][0], terms[1][0], op=ALU.add), reads=[terms[1][1]], writes=[terms[0][1]])
                    fw.op(pool, lambda: G.tensor_tensor(merged[:, mf, ts], terms[0][0], terms[2][0], op=ALU.add),
                          reads=[terms[0][1], terms[2][1]], writes=[merged_b[tc]])
            for half in range(2):
                slab, sb = load_w_slab(wo_d[l][:, half * 512:(half + 1) * 512], 512)
                for f4 in range(4):
                    f = half * 4 + f4
                    for tc in range(4):
                        ts = slice(tc * 512, (tc + 1) * 512)
                        bank = (f4 * 4 + tc) % 8
                        for k in range(8):
                            fw.op(pe, lambda: T.matmul(psb[bank][:, :], slab[:, k, f4 * 128:(f4 + 1) * 128], merged[:, k, ts], start=(k == 0), stop=(k == 7)),
                                  reads=[sb, merged_b[tc]], writes=[ps_b[bank]], inc=(k == 7))
                        fw.op(dve, lambda: V_.scalar_tensor_tensor(xT[:, f, ts], psb[bank][:, :], P[:, 2, f:f + 1], xT[:, f, ts], op0=ALU.mult, op1=ALU.add),
                              reads=[ps_b[bank], xT_b[tc]], writes=[xT_b[tc]])

        def phase_F(l, b):
            P = PRM[(l, b)]
            w1 = [vb(136 + 32 * i, 8 * 1024).rearrange("p (k n) -> p k n", k=8) for i in range(2)]
            w2 = [vb(152 + 32 * i, 8 * 1024).rearrange("p (k n) -> p k n", k=8) for i in range(2)]
            w_b = [Buf("ffw0"), Buf("ffw1")]
            hT = [vb(56 + i, 512) for i in range(8)]
            h_b = Buf("hT")
            rT = [TMP[4 + i] for i in range(4)]
            r_b = [TMP_b[4 + i] for i in range(4)]
            for cg in range(4):
                i = cg % 2
                for hf in range(2):
                    fw.dma(pool, w1[i][:, :, hf * 512:(hf + 1) * 512],
                           wff1_d[l][:, cg * 1024 + hf * 512:cg * 1024 + (hf + 1) * 512].rearrange("(k p) n -> p k n", p=128), writes=[w_b[i]])
                for hf in range(2):
                    fw.dma(pool, w2[i][:, :, hf * 512:(hf + 1) * 512],
                           wff2_d[l][cg * 1024:(cg + 1) * 1024, hf * 512:(hf + 1) * 512].rearrange("(k p) n -> p k n", p=128), writes=[w_b[i]])
                for tc in range(4):
                    ts = slice(tc * 512, (tc + 1) * 512)
                    for c in range(8):
                        bank = c % 2
                        for k in range(8):
                            fw.op(pe, lambda: T.matmul(psb[bank][:, :], w1[i][:, k, c * 128:(c + 1) * 128], uT[:, k, ts], start=(k == 0), stop=(k == 7)),
                                  reads=[w_b[i], uT_b[tc]], writes=[ps_b[bank]], inc=(k == 7))
                        r, rb = rT[c % 4], r_b[c % 4]
                        fw.op(act, lambda: Sc.activation(r, psb[bank][:, :], AF.Relu), reads=[ps_b[bank]], writes=[rb])
                        fw.op(pool, lambda: G.tensor_tensor(hT[c], r, r, op=ALU.mult), reads=[rb], writes=[h_b])
                    for f in range(8):
                        bank = 2 + (f % 6)
                        for c in range(8):
                            fw.op(pe, lambda: T.matmul(psb[bank][:, :], w2[i][:, c, f * 128:(f + 1) * 128], hT[c], start=(c == 0), stop=(c == 7)),
                                  reads=[w_b[i], h_b], writes=[ps_b[bank]], inc=(c == 7))
                        fw.op(dve, lambda: V_.scalar_tensor_tensor(xT[:, f, ts], psb[bank][:, :], P[:, 5, f:f + 1], xT[:, f, ts], op0=ALU.mult, op1=ALU.add),
                              reads=[ps_b[bank], xT_b[tc]], writes=[xT_b[tc]])

        def load_x(b):
            xin = [vf(56 + 4 * i, 1024) for i in range(2)]
            xin_b = [Buf("xin0"), Buf("xin1")]
            for tb in range(16):
                i = tb % 2
                fw.dma(sp, xin[i], x_d[b, tb * 128:(tb + 1) * 128, :], writes=[xin_b[i]])
                for half in range(2):
                    bank = 2 * i + half
                    for j in range(4):
                        kc = half * 4 + j
                        fw.op(pe, lambda: T.transpose(psb[bank][:, j * 128:(j + 1) * 128], xin[i][:, kc * 128:(kc + 1) * 128], identF),
                              reads=[xin_b[i]], writes=[ps_b[bank]], inc=(j == 3))
                    dst = xT[:, half * 4:(half + 1) * 4, tb * 128:(tb + 1) * 128]
                    src = psb[bank][:, :].rearrange("p (a c) -> p a c", a=4)
                    if half == 0:
                        fw.op(act, lambda: Sc.copy(dst, src), reads=[ps_b[bank]], writes=[xT_b[tb // 4]])
                    else:
                        fw.op(dve, lambda: V_.tensor_copy(dst, src), reads=[ps_b[bank]], writes=[xT_b[tb // 4]])

        def store_out(b):
            ot = [vf(56 + 4 * i, 1024) for i in range(2)]
            ot_b = [Buf("ot0"), Buf("ot1")]
            toks = []
            for tb in range(16):
                i = tb % 2
                for half in range(2):
                    bank = 2 * i + half
                    for j in range(4):
                        kc = half * 4 + j
                        fw.op(pe, lambda: T.transpose(psb[bank][:, j * 128:(j + 1) * 128], xT[:, kc, tb * 128:(tb + 1) * 128], identF),
                              reads=[xT_b[tb // 4]], writes=[ps_b[bank]], inc=(j == 3))
                    dst = ot[i][:, half * 512:(half + 1) * 512]
                    if half == 0:
                        fw.op(act, lambda: Sc.copy(dst, psb[bank][:, :]), reads=[ps_b[bank]], writes=[ot_b[i]])
                    else:
                        fw.op(dve, lambda: V_.tensor_copy(dst, psb[bank][:, :]), reads=[ps_b[bank]], writes=[ot_b[i]])
                toks.append(fw.dma(sp, out_d[b, tb * 128:(tb + 1) * 128, :], ot[i], reads=[ot_b[i]]))
            return toks

        def spill_x():
            for k in range(8):
                fw.dma(sp, xsp_d[:, k, :], xT[:, k, :], reads=xT_b, writes=[xsp_b])

        def reload_x():
            for k in range(8):
                fw.dma(sp, xT[:, k, :], xsp_d[:, k, :], reads=[xsp_b], writes=xT_b)

        out_toks = []
        for b in range(nseq):
            load_x(b)
            fw.barrier()
            P0 = PRM[(0, b)]
            norm_mod(P0[:, 0, :], P0[:, 1, :])
            if b == 0:
                dump("u0", uT, [128, 8, S], BF16, uT_b)
            spill_x()
            fw.barrier()
            for l in range(L):
                if stop_after != "pre" and "A" not in SKIP:
                    load_strips(range(0, 4))
                    fw.barrier()
                    phase_A(l)
                    fw.barrier()
                    if b == 0 and l == 0:
                        dump("oA", oA, [128, 4, S], BF16, [oA_b])
                if stop_after not in ("pre", "A") and "B" not in SKIP:
                    load_strips(range(4, 8))
                    fw.barrier()
                    phase_B(l)
                    fw.barrier()
                    if b == 0 and l == 0:
                        dump("oB", oB, [128, 2, S], BF16, [oB_b])
                if stop_after not in ("pre", "A", "B"):
                    load_strips(range(8, 12))
                    fw.barrier()
                    phase_C(l)
                    fw.barrier()
                    if b == 0 and l == 0:
                        dump("oC", oC, [128, 2, S], BF16, [oC_b])
                reload_x()
                fw.barrier()
                if stop_after not in ("pre", "A", "B", "C"):
                    phase_M(l, b)
                    fw.barrier()
                    if b == 0 and l == 0:
                        dump("x1", xT, [128, 8, S], F32, xT_b)
                    P = PRM[(l, b)]
                    norm_mod(P[:, 3, :], P[:, 4, :])
                    fw.barrier()
                    phase_F(l, b)
                    fw.barrier()
                    if b == 0 and l == 0:
                        dump("x2", xT, [128, 8, S], F32, xT_b)
                if l + 1 < L:
                    Pn = PRM[(l + 1, b)]
                    norm_mod(Pn[:, 0, :], Pn[:, 1, :])
                    spill_x()
                    fw.barrier()
            norm_mod(nfin, None)
            fw.barrier()
            out_toks += store_out(b)
            fw.barrier()
        fw._wait(sp, out_toks)
        fw.barrier()
        stats = {e.name: e.ninst for e in fw.engs}
    return nc, dbg_out, stats


_CONSTS = None


def _consts():
    global _CONSTS
    if _CONSTS is None:
        ident = np.eye(128, dtype=np.float32)
        anti = np.ascontiguousarray(ident[::-1])
        caus = np.where(np.arange(128)[None, :] <= np.arange(128)[:, None], 0.0, -1e30).astype(np.float32)
        sel = np.zeros((32, 32, 128), np.float32)
        for r in range(32):
            sel[r, r, :] = 1.0
        bm = np.zeros((8, 4, 8), np.float32)
        own = np.zeros((8, 4, 8), np.float32)
        for j in range(8):
            bm[j, :, j:] = -1e30
            own[j, :, j] = 1.0
        _CONSTS = dict(k_ident=ident, k_anti=anti, k_onehot=_t5_onehot(), k_caus=caus,
                       k_sel=sel.reshape(32, 32 * 128), k_bm=bm.reshape(-1), k_own=own.reshape(-1))
    return _CONSTS


def make_in_maps(inputs, n_cores, nseq, nlayer=2):
    f = lambda a: np.ascontiguousarray(np.asarray(a, dtype=np.float32))
    L = nlayer
    x = f(inputs["x"])
    c = f(inputs["c"])
    shared = dict(
        rel_bias=f(inputs["rel_bias"]),
        ada_w=f(inputs["ada_w"])[:L],
        ada_bT=f(f(inputs["ada_b"])[:L].reshape(L, 48, 128).transpose(0, 2, 1)),
        norm_mixT=f(f(inputs["norm_mix"])[:L].reshape(L, 8, 128).transpose(0, 2, 1)),
        w_in=f(inputs["w_in"])[:L],
        gate_bT=f(f(inputs["gate_b"])[:L].reshape(L, 24, 128).transpose(0, 2, 1)),
        diff_lambda=f(inputs["diff_lambda"])[:L].reshape(L, 256),
        diff_subln=f(inputs["diff_subln"])[:L].reshape(L, 128, 1),
        dsa_kv_norm=f(inputs["dsa_kv_norm"])[:L].reshape(L, 128, 1),
        dsa_w_uv=f(inputs["dsa_w_uv"])[:L],
        w_br_a=f(inputs["w_br_a"])[:L], w_br_b=f(inputs["w_br_b"])[:L], w_br_c=f(inputs["w_br_c"])[:L],
        w_o=f(inputs["w_o"])[:L],
        norm_mlpT=f(f(inputs["norm_mlp"])[:L].reshape(L, 8, 128).transpose(0, 2, 1)),
        w_ff1=f(inputs["w_ff1"])[:L], w_ff2=f(inputs["w_ff2"])[:L],
        norm_finalT=f(f(inputs["norm_final"]).reshape(8, 128).T),
    )
    shared.update(_consts())
    maps = []
    for i in range(n_cores):
        m = dict(shared)
        m["x"] = f(x[i * nseq:(i + 1) * nseq])
        m["c_lay"] = f(c[i * nseq:(i + 1) * nseq].reshape(nseq, 8, 128).transpose(2, 1, 0))
        maps.append(m)
    return maps


def kernel(**inputs):
    n_cores, nseq = 8, 2
    nc, _, _ = build(nseq=nseq, nlayer=2)
    maps = make_in_maps(inputs, n_cores, nseq)
    res = run_bass_kernel_spmd(nc, maps, core_ids=list(range(n_cores)))
    out = np.concatenate([np.asarray(r["out"]) for r in res.results], axis=0)
    return out.astype(np.float32)
```

```python
import contextlib
import math
import numpy as np
import concourse.bass as bass
import concourse.mybir as mybir
from concourse.bass_utils import run_bass_kernel_spmd

F32 = mybir.dt.float32
BF16 = mybir.dt.bfloat16
U32 = mybir.dt.uint32
AF = mybir.ActivationFunctionType
ALU = mybir.AluOpType
AX = mybir.AxisListType

S = 2048
D = 1024
NEG = -30000.0
EPS = 1e-6
O_AQ, O_AK, O_AV, O_BQ, O_BKV, O_BIQ, O_BIK, O_BIW, O_CQ, O_CK, O_CV, O_G = (
    0, 512, 1024, 1536, 2048, 2176, 2432, 2464, 2472, 2728, 2984, 3240)
IN_COLS = 6312
BIS_ITERS = 12
SAME_DIST = 1 << 30
CSTOP = 0
SKIP = ()


class Buf:
    __slots__ = ("name", "w", "r")

    def __init__(self, name=""):
        self.name = name
        self.w = None
        self.r = {}


class Eng:
    def __init__(self, name, h, same_sync):
        self.name = name
        self.h = h
        self.sem = None
        self.count = 0
        self.seen = {}
        self.same_sync = same_sync
        self.ninst = 0
        self.pos = 0


class Fw:
    def __init__(self, nc, stack, n_dma_sems=8, same_sync=True):
        self.nc = nc
        self.pe = Eng("pe", nc.tensor, False)
        self.act = Eng("act", nc.scalar, same_sync)
        self.dve = Eng("dve", nc.vector, same_sync)
        self.pool = Eng("pool", nc.gpsimd, same_sync)
        self.sp = Eng("sp", nc.sync, False)
        self.engs = [self.pe, self.act, self.dve, self.pool, self.sp]
        for e in self.engs:
            e.sem = stack.enter_context(nc.semaphore("s_" + e.name))
        self.dma_pools = {}
        for q in ("sp", "pool"):
            sems = [stack.enter_context(nc.semaphore("d_%s%d" % (q, i))) for i in range(n_dma_sems)]
            self.dma_pools[q] = dict(sems=sems, cnt=[0] * n_dma_sems, nxt=0)

    def _need(self, eng, tok):
        sem, val, owner = tok[0], tok[1], tok[2]
        if owner is eng:
            if not eng.same_sync:
                return False
            if eng.pos - tok[3] >= SAME_DIST:
                return False
        return eng.seen.get(id(sem), 0) < val

    def _wait(self, eng, toks):
        best = {}
        for t in toks:
            if t is None or not self._need(eng, t):
                continue
            k = id(t[0])
            if k not in best or best[k][1] < t[1]:
                best[k] = t
        for k, t in best.items():
            eng.h.wait_ge(t[0], t[1])
            eng.seen[k] = t[1]
            eng.ninst += 1

    @staticmethod
    def _deps(reads, writes):
        toks = []
        for b in reads:
            toks.append(b.w)
        for b in writes:
            toks.append(b.w)
            toks.extend(b.r.values())
        return toks

    @staticmethod
    def _record(tok, reads, writes):
        k = id(tok[0])
        for b in reads:
            o = b.r.get(k)
            if o is None or o[1] < tok[1]:
                b.r[k] = tok
        for b in writes:
            b.w = tok
            b.r = {}

    def op(self, eng, fn, reads=(), writes=(), inc=True, nosync=False):
        toks = []
        for b in reads:
            toks.append(b.w)
        for b in writes:
            rd = list(b.r.values())
            if not (b.w is not None and b.w[2] is eng and any(t[2] is not eng for t in rd)):
                toks.append(b.w)
            toks.extend(rd)
        self._wait(eng, toks)
        ins = fn()
        eng.ninst += 1
        eng.pos += 1
        if inc:
            ins.then_inc(eng.sem, 1)
            eng.count += 1
            tok = (eng.sem, eng.count, eng, eng.pos)
        else:
            tok = (eng.sem, eng.count + 1, eng, eng.pos + 1)
        self._record(tok, reads, writes)
        return tok

    def dma(self, eng, out, in_, reads=(), writes=(), **kw):
        pool = self.dma_pools[eng.name]
        self._wait(eng, self._deps(reads, writes))
        j = pool["nxt"]
        pool["nxt"] = (j + 1) % len(pool["sems"])
        sem = pool["sems"][j]
        if pool["cnt"][j] > 0 and eng.seen.get(id(sem), 0) < pool["cnt"][j]:
            eng.h.wait_ge(sem, pool["cnt"][j])
            eng.seen[id(sem)] = pool["cnt"][j]
        ins = eng.h.dma_start(out=out, in_=in_, **kw)
        ins.then_inc(sem, 16)
        eng.ninst += 1
        pool["cnt"][j] += 16
        tok = (sem, pool["cnt"][j], None, 0)
        self._record(tok, reads, writes)
        return tok

    def barrier(self):
        sp = self.sp
        toks = []
        for e in self.engs:
            if e is not sp and e.count > 0:
                toks.append((e.sem, e.count, e, e.pos))
        for q, pool in self.dma_pools.items():
            for j, sem in enumerate(pool["sems"]):
                if pool["cnt"][j] > 0:
                    toks.append((sem, pool["cnt"][j], None, 0))
        self._wait(sp, toks)
        ins = sp.h.nop()
        ins.then_inc(sp.sem, 1)
        sp.count += 1
        tok = (sp.sem, sp.count, sp, sp.pos)
        for e in self.engs:
            if e is sp:
                continue
            e.h.wait_ge(sp.sem, sp.count)
            e.seen[id(sp.sem)] = sp.count
            for t in toks:
                k = id(t[0])
                if e.seen.get(k, 0) < t[1]:
                    e.seen[k] = t[1]
        return tok


def _t5_onehot():
    dd = np.arange(1280, dtype=np.int64) - 511
    n = np.maximum(dd, 0)
    nf = np.maximum(n, 1).astype(np.float32)
    large = 16 + (np.log(nf / np.float32(16)) / np.float32(math.log(128 / 16)) * np.float32(16)).astype(np.int32)
    large = np.minimum(large, 31)
    bucket = np.where(n < 16, n, large)
    oh = np.zeros((33, 1280), np.float32)
    for j in range(1280):
        if dd[j] < 0:
            oh[32, j] = 1.0
        else:
            oh[bucket[j], j] = 1.0
    return oh


def build(nseq=2, nlayer=2, dbg=(), stop_after=None, same_sync=True):
    nc = bass.Bass("TRN2", target_bir_lowering=False)
    L = nlayer

    def din(name, shape, dt=F32):
        return nc.dram_tensor(name, list(shape), dt, kind="ExternalInput").ap()

    x_d = din("x", [nseq, S, D])
    c_d = din("c_lay", [128, 8, nseq])
    relb_d = din("rel_bias", [32, 12])
    adaw_d = din("ada_w", [L, D, 6 * D])
    adab_d = din("ada_bT", [L, 128, 48])
    nmix_d = din("norm_mixT", [L, 128, 8])
    win_d = din("w_in", [L, D, IN_COLS])
    gateb_d = din("gate_bT", [L, 128, 24])
    dlam_d = din("diff_lambda", [L, 256])
    subln_d = din("diff_subln", [L, 128, 1])
    kvn_d = din("dsa_kv_norm", [L, 128, 1])
    wuv_d = din("dsa_w_uv", [L, 4, 128, 64])
    wbra_d = din("w_br_a", [L, 512, D])
    wbrb_d = din("w_br_b", [L, 256, D])
    wbrc_d = din("w_br_c", [L, 256, D])
    wo_d = din("w_o", [L, D, D])
    nmlp_d = din("norm_mlpT", [L, 128, 8])
    wff1_d = din("w_ff1", [L, D, 4 * D])
    wff2_d = din("w_ff2", [L, 4 * D, D])
    nfin_d = din("norm_finalT", [128, 8])
    ident_d = din("k_ident", [128, 128])
    anti_d = din("k_anti", [128, 128])
    oh_d = din("k_onehot", [33, 1280])
    caus_d = din("k_caus", [128, 128])
    sel_d = din("k_sel", [32, 32 * 128])
    bm_d = din("k_bm", [8 * 32])
    own_d = din("k_own", [8 * 32])
    out_d = nc.dram_tensor("out", [nseq, S, D], F32, kind="ExternalOutput").ap()
    g_d = nc.dram_tensor("g_scr", [12, 1280], BF16, kind="Internal").ap()
    xsp_d = nc.dram_tensor("x_spill", [128, 8, S], F32, kind="Internal").ap()
    dbg_out = {}

    with contextlib.ExitStack() as st:
        fw = Fw(nc, st, same_sync=same_sync)
        pe, act, dve, pool, sp = fw.pe, fw.act, fw.dve, fw.pool, fw.sp
        T, V_, Sc, G = nc.tensor, nc.vector, nc.scalar, nc.gpsimd
        arena = st.enter_context(nc.sbuf_tensor("arena", [128, 51200], F32))
        psb = [st.enter_context(nc.psum_tensor("ps%d" % i, [128, 512], F32)) for i in range(8)]
        ps_b = [Buf("ps%d" % i) for i in range(8)]

        KW = 256

        def vf(off_k, nwords, parts=128, p0=0):
            o = int(round(off_k * KW))
            return arena[p0:p0 + parts, o:o + nwords]

        def vb(off_k, nelem, parts=128, p0=0):
            o = int(round(off_k * KW))
            return arena[p0:p0 + parts, o:o + nelem // 2].bitcast(BF16)

        identF = vf(0, 128)
        identB = vb(0.5, 128)
        antiB = vb(0.75, 128)
        onesB = vb(1.0, 128)
        epsT = vf(1.25, 1)
        halfT = vf(1.25, 1)
        neglam = [vf(1.26 + 0.01 * l, 1) for l in range(L)]
        def wv(word, n, parts=128):
            return arena[0:parts, word:word + n]
        W0 = 330
        epsT = wv(W0, 1)
        neglam = [wv(W0 + 1 + l, 1) for l in range(L)]
        subg = [wv(W0 + 4 + l, 1) for l in range(L)]
        kvg = [wv(W0 + 8 + l, 1) for l in range(L)]
        nfin = wv(W0 + 12, 8)
        zeroT = wv(W0 + 20, 1)
        c31 = wv(1000, 12)
        gateb = [wv(W0 + 24 + 24 * l, 24) for l in range(L)]
        PRM = {}
        w = W0 + 80
        for l in range(L):
            for b in range(nseq):
                PRM[(l, b)] = wv(w, 48).rearrange("p (j k) -> p j k", j=6)
                w += 48
        assert w <= 1024
        uT = vb(4, 8 * S).rearrange("p (k t) -> p k t", k=8)
        WST = [vb(36 + 8 * i, 8 * 512).rearrange("p (k n) -> p k n", k=8) for i in range(2)]
        PT = [vb(52 + i, 512) for i in range(4)]
        TMP = [vf(56 + 2 * i, 512) for i in range(8)]
        uT_b = [Buf("uT%d" % i) for i in range(4)]
        WST_b = [Buf("wst%d" % i) for i in range(2)]
        PT_b = [Buf("pt%d" % i) for i in range(4)]
        TMP_b = [Buf("tmp%d" % i) for i in range(8)]
        xT = vf(72, 8 * S).rearrange("p (k t) -> p k t", k=8)
        xT_b = [Buf("xT%d" % i) for i in range(4)]
        STR = vb(72, 12 * 1152).rearrange("p (h n) -> p h n", h=12)
        ARENA_K = 99
        merged = vb(136, 8 * S).rearrange("p (k t) -> p k t", k=8)
        merged_b = [Buf("mg%d" % i) for i in range(4)]
        oA = vb(168, 4 * S).rearrange("p (k t) -> p k t", k=4)
        oB = vb(184, 2 * S).rearrange("p (k t) -> p k t", k=2)
        oC = vb(192, 2 * S).rearrange("p (k t) -> p k t", k=2)
        oA_b, oB_b, oC_b = Buf("oA"), Buf("oB"), Buf("oC")
        xsp_b = Buf("xsp")
        gd_b = Buf("gd")

        state = {"pt": 0, "tmp": 0, "wst": 0, "lg": 0}

        def next_pt():
            i = state["pt"]
            state["pt"] = (i + 1) % 4
            return PT[i], PT_b[i]

        def next_tmp(avoid=()):
            i = state["tmp"]
            while any(TMP_b[i] is a for a in avoid):
                i = (i + 1) % 8
            state["tmp"] = (i + 1) % 8
            return TMP[i], TMP_b[i]

        def next_wst():
            i = state["wst"]
            state["wst"] = (i + 1) % 2
            return WST[i], WST_b[i]

        def dump(name, ap, shape, dt, reads):
            if name not in dbg:
                return
            d = nc.dram_tensor("dbg_" + name, list(shape), dt, kind="ExternalOutput").ap()
            dbg_out[name] = d
            fw.barrier()
            t = fw.dma(sp, d, ap, reads=reads)
            fw._wait(sp, [t])

        def load_w_slab(src_ap, ncols, eng=None):
            slab, sb = next_wst()
            fw.dma(pool, slab[:, :, 0:ncols], src_ap.rearrange("(k p) n -> p k n", p=128), writes=[sb])
            return slab, sb

        cb = Buf("consts")
        fw.dma(sp, identF, ident_d, writes=[cb])
        fw.dma(pool, identB, ident_d, writes=[cb])
        fw.dma(pool, antiB, anti_d, writes=[cb])
        fw.op(dve, lambda: V_.memset(onesB, 1.0), writes=[cb])
        fw.op(dve, lambda: V_.memset(epsT, EPS), writes=[cb])
        fw.op(dve, lambda: V_.memset(zeroT, 0.0), writes=[cb])
        fw.dma(sp, nfin, nfin_d, writes=[cb])
        fw.dma(sp, c31, relb_d[31].partition_broadcast(128), writes=[cb])
        for l in range(L):
            fw.dma(sp, subg[l], subln_d[l], writes=[cb])
            fw.dma(sp, kvg[l], kvn_d[l], writes=[cb])
            fw.dma(sp, gateb[l], gateb_d[l], writes=[cb])
        fw.barrier()
        sa = 72
        relb = vf(sa, 12, parts=33)
        ohT = vf(sa + 1, 1280, parts=33)
        grow = vb(sa + 7, 1280, parts=12)
        tb_ = Buf("setup")
        fw.op(dve, lambda: V_.memset(vf(sa, 12, parts=64)[32:64, :], NEG), writes=[tb_])
        fw.dma(sp, relb[0:32, :], relb_d, writes=[tb_])
        fw.dma(sp, ohT, oh_d, writes=[tb_])
        for ci, (c0, cn) in enumerate(((0, 512), (512, 512), (1024, 256))):
            fw.op(pe, lambda: T.matmul(psb[ci][0:12, 0:cn], relb, ohT[:, c0:c0 + cn], start=True, stop=True),
                  reads=[tb_], writes=[ps_b[ci]])
            fw.op(dve, lambda: V_.tensor_copy(grow[:, c0:c0 + cn], psb[ci][0:12, 0:cn]), reads=[ps_b[ci]], writes=[tb_])
        fw.dma(sp, g_d, grow, reads=[tb_], writes=[gd_b])
        for l in range(L):
            lam_init = 0.8 - 0.6 * math.exp(-0.3 * l)
            dl = vf(sa + 12, 256)
            pr = vf(sa + 13, 128)
            s12 = vf(sa + 14, 2)
            e12 = vf(sa + 14.5, 2)
            fw.dma(sp, dl, dlam_d[l].partition_broadcast(128), writes=[tb_])
            fw.op(dve, lambda: V_.tensor_tensor(pr[:, 0:64], dl[:, 0:64], dl[:, 64:128], op=ALU.mult), reads=[tb_], writes=[tb_])
            fw.op(dve, lambda: V_.tensor_tensor(pr[:, 64:128], dl[:, 128:192], dl[:, 192:256], op=ALU.mult), reads=[tb_], writes=[tb_])
            fw.op(dve, lambda: V_.tensor_reduce(s12, pr.rearrange("p (a b) -> p a b", a=2), axis=AX.X, op=ALU.add), reads=[tb_], writes=[tb_])
            fw.op(act, lambda: Sc.activation(e12, s12, AF.Exp), reads=[tb_], writes=[tb_])
            fw.op(dve, lambda: V_.tensor_tensor(s12[:, 0:1], e12[:, 1:2], e12[:, 0:1], op=ALU.subtract), reads=[tb_], writes=[tb_])
            fw.op(dve, lambda: V_.tensor_scalar(neglam[l], s12[:, 0:1], -lam_init, None, op0=ALU.add), reads=[tb_], writes=[tb_])
            fw.op(dve, lambda: V_.tensor_scalar(subg[l], subg[l], 1.0 - lam_init, None, op0=ALU.mult), reads=[tb_], writes=[tb_])
        cT = vf(sa + 16, 8 * nseq).rearrange("p (k b) -> p k b", k=8)
        modT = vf(sa + 17, 48 * nseq).rearrange("p (f b) -> p f b", f=48)
        adab = vf(sa + 18, 48)
        nmx = vf(sa + 19, 8)
        nml = vf(sa + 19.5, 8)
        fw.dma(sp, cT, c_d, writes=[tb_])
        fw.op(act, lambda: Sc.activation(cT, cT, AF.Silu), reads=[tb_], writes=[tb_])
        cTb = vb(sa + 16.25, 8 * nseq).rearrange("p (k b) -> p k b", k=8)
        fw.op(dve, lambda: V_.tensor_copy(cTb, cT), reads=[tb_], writes=[tb_])
        slabs = [vb(136 + 8 * i, 8 * 512).rearrange("p (k n) -> p k n", k=8) for i in range(4)]
        slab_b = [Buf("adaslab%d" % i) for i in range(4)]
        si = 0
        for l in range(L):
            fw.dma(sp, adab, adab_d[l], writes=[tb_])
            fw.dma(sp, nmx, nmix_d[l], writes=[tb_])
            fw.dma(sp, nml, nmlp_d[l], writes=[tb_])
            for sl in range(12):
                sb_, sbb = slabs[si % 4], slab_b[si % 4]
                si += 1
                fw.dma(pool, sb_, adaw_d[l][:, sl * 512:(sl + 1) * 512].rearrange("(k p) n -> p k n", p=128), writes=[sbb])
                bank = 4 + (sl % 2)
                for fc4 in range(4):
                    for k in range(8):
                        fw.op(pe, lambda: T.matmul(psb[bank][:, fc4 * nseq:(fc4 + 1) * nseq], sb_[:, k, fc4 * 128:(fc4 + 1) * 128], cTb[:, k, :],
                                                   start=(k == 0), stop=(k == 7)),
                              reads=[sbb, tb_], writes=[ps_b[bank]], inc=(k == 7 and fc4 == 3))
                for fc4 in range(4):
                    fc = sl * 4 + fc4
                    fw.op(dve, lambda: V_.tensor_scalar(modT[:, fc, :], psb[bank][:, fc4 * nseq:(fc4 + 1) * nseq], adab[:, fc:fc + 1], None, op0=ALU.add),
                          reads=[ps_b[bank], tb_], writes=[tb_])
            for b in range(nseq):
                P = PRM[(l, b)]
                fw.op(dve, lambda: V_.scalar_tensor_tensor(P[:, 0, :], modT[:, 8:16, b], 1.0, nmx, op0=ALU.add, op1=ALU.mult), reads=[tb_], writes=[tb_])
                fw.op(dve, lambda: V_.tensor_copy(P[:, 1, :], modT[:, 0:8, b]), reads=[tb_], writes=[tb_])
                fw.op(dve, lambda: V_.tensor_copy(P[:, 2, :], modT[:, 16:24, b]), reads=[tb_], writes=[tb_])
                fw.op(dve, lambda: V_.scalar_tensor_tensor(P[:, 3, :], modT[:, 32:40, b], 1.0, nml, op0=ALU.add, op1=ALU.mult), reads=[tb_], writes=[tb_])
                fw.op(dve, lambda: V_.tensor_copy(P[:, 4, :], modT[:, 24:32, b]), reads=[tb_], writes=[tb_])
                fw.op(dve, lambda: V_.tensor_copy(P[:, 5, :], modT[:, 40:48, b]), reads=[tb_], writes=[tb_])
        fw.barrier()

        def rstd_from_ss(ss_bank, inv_n):
            t1, t1b = next_tmp()
            fw.op(act, lambda: Sc.activation(t1, psb[ss_bank][:, :], AF.Ln, bias=epsT, scale=inv_n), reads=[ps_b[ss_bank]], writes=[t1b])
            t2, t2b = next_tmp()
            fw.op(act, lambda: Sc.activation(t2, t1, AF.Exp, scale=-0.5), reads=[t1b], writes=[t2b])
            return t2, t2b

        def norm_mod(Aap, Bap):
            for tc in range(4):
                ts = slice(tc * 512, (tc + 1) * 512)
                bank = 6 + (tc % 2)
                for k in range(8):
                    sq, sqb = next_pt()
                    fw.op(act, lambda: Sc.activation(sq, xT[:, k, ts], AF.Square), reads=[xT_b[tc]], writes=[sqb])
                    fw.op(pe, lambda: T.matmul(psb[bank][:, :], onesB, sq, start=(k == 0), stop=(k == 7)),
                          reads=[sqb], writes=[ps_b[bank]], inc=True)
                rstd, rb = rstd_from_ss(bank, 1.0 / D)
                for k in range(8):
                    t1, t1b = next_tmp(avoid=(rb,))
                    fw.op(dve, lambda: V_.tensor_tensor(t1, xT[:, k, ts], rstd, op=ALU.mult), reads=[xT_b[tc], rb], writes=[t1b])
                    if Bap is not None:
                        fw.op(act, lambda: Sc.activation(uT[:, k, ts], t1, AF.Identity, bias=Bap[:, k:k + 1], scale=Aap[:, k:k + 1]),
                              reads=[t1b], writes=[uT_b[tc]])
                    else:
                        fw.op(act, lambda: Sc.activation(xT[:, k, ts], t1, AF.Identity, bias=zeroT, scale=Aap[:, k:k + 1]),
                              reads=[t1b], writes=[xT_b[tc]])

        def proj_fm(slab, sb, col0, ncols, dst_fn, dst_bufs, scale=None, banks=(6, 7), post=None, dst2_fn=None):
            for tc in range(4):
                bank = banks[tc % len(banks)]
                for k in range(8):
                    fw.op(pe, lambda: T.matmul(psb[bank][0:ncols, :], slab[:, k, col0:col0 + ncols], uT[:, k, tc * 512:(tc + 1) * 512],
                                               start=(k == 0), stop=(k == 7)),
                          reads=[sb, uT_b[tc]], writes=[ps_b[bank]], inc=(k == 7))
                xr = []
                if post is not None:
                    xr = post(tc, bank)
                if dst2_fn is not None:
                    fw.op(act, lambda: Sc.copy(dst_fn(tc), psb[bank][0:64, :]), reads=[ps_b[bank]] + xr, writes=dst_bufs)
                    fw.op(act, lambda: Sc.copy(dst2_fn(tc), psb[bank][64:128, :]), reads=[ps_b[bank]] + xr, writes=dst_bufs)
                elif scale is None:
                    fw.op(act, lambda: Sc.copy(dst_fn(tc), psb[bank][0:ncols, :]), reads=[ps_b[bank]] + xr, writes=dst_bufs)
                else:
                    fw.op(act, lambda: Sc.activation(dst_fn(tc), psb[bank][0:ncols, :], AF.Copy, scale=scale), reads=[ps_b[bank]] + xr, writes=dst_bufs)

        def proj_tm(slab, sb, col0, ncols, dst_fn, dst_bufs, banks=(6, 7), dtype_copy=True):
            for tb in range(16):
                bank = banks[tb % len(banks)]
                for k in range(8):
                    fw.op(pe, lambda: T.matmul(psb[bank][:, 0:ncols], uT[:, k, tb * 128:(tb + 1) * 128], slab[:, k, col0:col0 + ncols],
                                               start=(k == 0), stop=(k == 7)),
                          reads=[sb, uT_b[tb // 4]], writes=[ps_b[bank]], inc=(k == 7))
                e = dve if tb % 2 else act
                if e is dve:
                    fw.op(dve, lambda: V_.tensor_copy(dst_fn(tb), psb[bank][:, 0:ncols]), reads=[ps_b[bank]], writes=dst_bufs)
                else:
                    fw.op(act, lambda: Sc.copy(dst_fn(tb), psb[bank][:, 0:ncols]), reads=[ps_b[bank]], writes=dst_bufs)

        def attn_core(tc, k_fn, q_ap, strip_h, v_fn, o_ap, sum_ap, o_bank, s_bank, ones_ap, reads, extra=None,
                      first=True, last_grp=True):
            nkb = 4 * tc + 4
            lbanks = (0, 1)

            def qk(kb):
                bank = lbanks[kb % 2]
                dl_ = min(4 * tc - kb, 2)
                c0 = (dl_ + 3) * 128
                cl = max(kb - 4 * tc, 0) * 128
                far = dl_ >= 2
                has_extra = extra is not None and extra(kb, bank, True, cl)
                only = far and not has_extra
                fw.op(pe, lambda: T.matmul(psb[bank][:, cl:512], k_fn(kb), q_ap[:, cl:512], start=True, stop=only),
                      reads=reads, writes=[ps_b[bank]], inc=only)
                if not far:
                    fw.op(pe, lambda: T.matmul(psb[bank][:, cl:512], antiB, STR[:, strip_h, c0 + cl:c0 + 512], start=False, stop=not has_extra),
                          reads=[], writes=[ps_b[bank]], inc=not has_extra)
                if has_extra:
                    extra(kb, bank, False, cl)
                return bank, far, cl

            pend = qk(0)
            for kb in range(nkb):
                bank, far, cl = pend
                if kb + 1 < nkb:
                    pend = qk(kb + 1)
                p, pb_ = next_pt()
                if far:
                    fw.op(act, lambda: Sc.activation(p[:, cl:512], psb[bank][:, cl:512], AF.Exp, bias=c31[:, strip_h:strip_h + 1]), reads=[ps_b[bank]], writes=[pb_])
                else:
                    fw.op(act, lambda: Sc.activation(p[:, cl:512], psb[bank][:, cl:512], AF.Exp), reads=[ps_b[bank]], writes=[pb_])
                st_ = first and kb == 0
                sp_ = last_grp and kb == nkb - 1
                fw.op(pe, lambda: T.matmul(o_ap[:, cl:512], v_fn(kb), p[:, cl:512], start=st_, stop=sp_),
                      reads=reads + [pb_], writes=[ps_b[o_bank]], inc=sp_)
                fw.op(pe, lambda: T.matmul(sum_ap[:, cl:512], ones_ap, p[:, cl:512], start=st_, stop=sp_),
                      reads=[pb_], writes=[ps_b[s_bank]], inc=True)

        def recip_sum(s_bank, sum_ap, parts=128, p0=0):
            t1, t1b = next_tmp()
            t1v = t1[p0:p0 + parts, :]
            fw.op(act, lambda: Sc.activation(t1v, sum_ap, AF.Ln), reads=[ps_b[s_bank]], writes=[t1b])
            t2, t2b = next_tmp()
            t2v = t2[p0:p0 + parts, :]
            fw.op(act, lambda: Sc.activation(t2v, t1v, AF.Exp, scale=-1.0), reads=[t1b], writes=[t2b])
            return t2v, t2b

        def load_strips(heads):
            sb_ = Buf("strips")
            for h in heads:
                src = bass.AP(tensor=g_d.tensor, offset=h * 1280, ap=[[1, 128], [1, 1152]])
                fw.dma(sp, STR[:, h, :], src, reads=[gd_b], writes=[sb_])
            return sb_

        def phase_A(l):
            ak = ARENA_K
            qT = vb(ak, 4 * S).rearrange("p (h t) -> p h t", h=4)
            kTz = vb(ak + 16, 8 * S).rearrange("p (h m t) -> p h m t", h=4, m=2)
            Vt = vb(ak + 48, 16 * 512).rearrange("p (b e) -> p b e", b=16)
            q_b, k_b, v_b = Buf("qT"), Buf("kT"), Buf("Vt")
            slab, sb = load_w_slab(win_d[l][:, O_AQ:O_AQ + 512], 512)
            for h in range(4):
                proj_fm(slab, sb, h * 128, 128, lambda tc: qT[:, h, tc * 512:(tc + 1) * 512], [q_b], scale=0.125)
            fw.op(pool, lambda: G.memset(kTz[64:128, :, 0, :], 0.0), writes=[k_b])
            fw.op(pool, lambda: G.memset(kTz[0:64, :, 1, :], 0.0), writes=[k_b])
            slab, sb = load_w_slab(win_d[l][:, O_AK:O_AK + 512], 512)
            for h in range(4):
                proj_fm(slab, sb, h * 128, 128, lambda tc: kTz[0:64, h, 0, tc * 512:(tc + 1) * 512], [k_b],
                        dst2_fn=lambda tc: kTz[64:128, h, 1, tc * 512:(tc + 1) * 512])
            slab, sb = load_w_slab(win_d[l][:, O_AV:O_AV + 512], 512)
            proj_tm(slab, sb, 0, 512, lambda tb: Vt[:, tb, :], [v_b])
            sqd = [vb(ak + 64 + i, 512) for i in range(2)]
            sqd_b = [Buf("sqd0"), Buf("sqd1")]
            deferred = []
            gi = 0
            for h in range(4):
                for tc in range(4):
                    ts = slice(tc * 512, (tc + 1) * 512)
                    R = []
                    for m in range(2):
                        ob, sbk = 2 + m, 4 + m
                        attn_core(tc, lambda kb: kTz[:, h, m, kb * 128:(kb + 1) * 128], qT[:, h, ts], h,
                                  lambda kb: Vt[:, kb, h * 128:(h + 1) * 128], psb[ob][:, :], psb[sbk][:, :], ob, sbk, onesB,
                                  reads=[q_b, k_b, v_b])
                        rs, rsb = recip_sum(sbk, psb[sbk][:, :])
                        r, rb = next_tmp()
                        fw.op(dve, lambda: V_.tensor_tensor(r, psb[ob][:, :], rs, op=ALU.mult), reads=[ps_b[ob], rsb], writes=[rb])
                        R.append((r, rb))
                        if m == 0 and deferred:
                            deferred.pop()()
                    dd, ddb = next_tmp()
                    fw.op(dve, lambda: V_.scalar_tensor_tensor(dd, R[1][0], neglam[l], R[0][0], op0=ALU.mult, op1=ALU.add),
                          reads=[R[0][1], R[1][1]], writes=[ddb])
                    sq, sqb = sqd[gi % 2], sqd_b[gi % 2]
                    gi += 1
                    fw.op(act, lambda: Sc.activation(sq, dd, AF.Square), reads=[ddb], writes=[sqb])

                    def tail(dd=dd, ddb=ddb, sq=sq, sqb=sqb, h=h, ts=ts):
                        fw.op(pe, lambda: T.matmul(psb[6][:, :], onesB, sq, start=True, stop=True), reads=[sqb], writes=[ps_b[6]])
                        rstd, rb2 = rstd_from_ss(6, 1.0 / 128)
                        fw.op(dve, lambda: V_.scalar_tensor_tensor(oA[:, h, ts], dd, subg[l], rstd, op0=ALU.mult, op1=ALU.mult),
                              reads=[ddb, rb2], writes=[oA_b])
                    deferred.append(tail)
            while deferred:
                deferred.pop()()

        def phase_B(l):
            ak = ARENA_K
            ak = 90
            kvT = vb(72, S)
            kvt = vb(76, 16 * 128).rearrange("p (b r) -> p b r", b=16)
            bqT = vb(ak, 4 * S).rearrange("p (h t) -> p h t", h=4)
            iqT = vb(ak + 16, 3 * S, parts=96).rearrange("p (c t) -> p c t", c=3)
            ikT = vb(ak + 28, S, parts=96)
            scores = vf(ak + 32, S)
            MnegAll = vb(ak + 40, 8 * S).rearrange("p (u j s) -> p u j s", u=2, j=4)
            iw = vf(ak + 72, 16 * 8).rearrange("p (b h) -> p b h", b=16)
            wuv2 = vb(ak + 72.5, 4 * 128).rearrange("p (h e) -> p h e", h=4)
            caus = vf(ak + 73.5, 128)
            bis = vf(ak + 74, 16)
            ikw = vb(ak + 74.25, 8 * 96).rearrange("p (k n) -> p k n", k=8)
            bq_b, kv_b, kvt_b, iq_b, ik_b, sc_b, iw_b, wuv_b, ca_b, bis_b, ikw_b = [Buf(n) for n in
                ("bq", "kv", "kvt", "iq", "ik", "sc", "iw", "wuv", "ca", "bis", "ikw")]
            mn_bs = [[Buf("mn%d%d" % (u, j)) for j in range(4)] for u in range(2)]
            fw.dma(sp, caus, caus_d, writes=[ca_b])
            fw.op(pool, lambda: G.memset(wuv2, 0.0), writes=[wuv_b])
            for h in range(4):
                fw.dma(pool, wuv2[:, h, (h % 2) * 64:(h % 2) * 64 + 64], wuv_d[l][h], writes=[wuv_b])
            slab, sb = load_w_slab(win_d[l][:, O_BQ:O_BQ + 512], 512)
            for h in range(4):
                proj_fm(slab, sb, h * 128, 128, lambda tc: bqT[:, h, tc * 512:(tc + 1) * 512], [bq_b], scale=128 ** -0.5)
            slab, sb = load_w_slab(win_d[l][:, O_BKV:O_BKV + 424], 424)

            def kv_post(tc, bank):
                ts = slice(tc * 512, (tc + 1) * 512)
                sq, sqb = next_pt()
                fw.op(act, lambda: Sc.activation(sq, psb[bank][:, :], AF.Square), reads=[ps_b[bank]], writes=[sqb])
                fw.op(pe, lambda: T.matmul(psb[5][:, :], onesB, sq, start=True, stop=True), reads=[sqb], writes=[ps_b[5]])
                rstd, rb = rstd_from_ss(5, 1.0 / 128)
                fw.op(dve, lambda: V_.scalar_tensor_tensor(kvT[:, ts], psb[bank][:, :], kvg[l], rstd, op0=ALU.mult, op1=ALU.mult),
                      reads=[ps_b[bank], rb], writes=[kv_b])

            for tc in range(4):
                bank = 6 + (tc % 2)
                for k in range(8):
                    fw.op(pe, lambda: T.matmul(psb[bank][:, :], slab[:, k, 0:128], uT[:, k, tc * 512:(tc + 1) * 512], start=(k == 0), stop=(k == 7)),
                          reads=[sb, uT_b[tc]], writes=[ps_b[bank]], inc=(k == 7))
                kv_post(tc, bank)
            for g4 in range(4):
                bank = 6 + (g4 % 2)
                pbf = psb[bank][:, :].bitcast(BF16)
                for j in range(4):
                    tb = g4 * 4 + j
                    fw.op(pe, lambda: T.transpose(pbf[:, j * 128:(j + 1) * 128], kvT[:, tb * 128:(tb + 1) * 128], identB),
                          reads=[kv_b], writes=[ps_b[bank]], inc=(j == 3))
                fw.op(dve, lambda: V_.tensor_copy(kvt[:, g4 * 4:(g4 + 1) * 4, :], pbf[:, 0:512].rearrange("p (a b) -> p a b", a=4)),
                      reads=[ps_b[bank]], writes=[kvt_b])
            for c in range(3):
                nh = 3 if c < 2 else 2
                proj_fm(slab, sb, 128 + c * 96, nh * 32, lambda tc: iqT[0:nh * 32, c, tc * 512:(tc + 1) * 512], [iq_b])
            for j in range(3):
                fw.op(dve, lambda: V_.tensor_copy(ikw[:, :, j * 32:(j + 1) * 32], slab[:, :, 384:416]), reads=[sb], writes=[ikw_b])
            proj_fm(ikw, ikw_b, 0, 96, lambda tc: ikT[:, tc * 512:(tc + 1) * 512], [ik_b])
            proj_tm(slab, sb, 416, 8, lambda tb: iw[:, tb, :], [iw_b])

            def indexer(qb, j):
                u = (qb // 4) % 2
                Mneg = MnegAll[:, u]
                mn_b = mn_bs[u][j]
                Wd = (qb + 1) * 128
                nsc = (Wd + 511) // 512
                for sc in range(nsc):
                    cols = min(512, Wd - sc * 512)
                    cs = slice(sc * 512, sc * 512 + cols)
                    for hh in range(8):
                        c, pb = hh // 3, (hh % 3) * 32
                        bank = 6 + (hh % 2)
                        fw.op(pe, lambda: T.matmul(psb[bank][:, 0:cols], iqT[pb:pb + 32, c, qb * 128:(qb + 1) * 128], ikT[pb:pb + 32, cs],
                                                   start=True, stop=True),
                              reads=[iq_b, ik_b], writes=[ps_b[bank]])
                        fw.op(act, lambda: Sc.activation(psb[bank][:, 0:cols], psb[bank][:, 0:cols], AF.Relu), reads=[], writes=[ps_b[bank]])
                        if hh == 0:
                            fw.op(dve, lambda: V_.tensor_scalar(scores[:, cs], psb[bank][:, 0:cols], iw[:, qb, 0:1], None, op0=ALU.mult),
                                  reads=[ps_b[bank], iw_b], writes=[sc_b])
                        else:
                            fw.op(dve, lambda: V_.scalar_tensor_tensor(scores[:, cs], psb[bank][:, 0:cols], iw[:, qb, hh:hh + 1], scores[:, cs],
                                                                       op0=ALU.mult, op1=ALU.add),
                                  reads=[ps_b[bank], iw_b, sc_b], writes=[sc_b])
                dsl = slice(qb * 128, (qb + 1) * 128)
                fw.op(dve, lambda: V_.tensor_tensor(scores[:, dsl], scores[:, dsl], caus, op=ALU.add), reads=[sc_b, ca_b], writes=[sc_b])
                lo, hi, mid, cnt, w0 = (bis[:, i:i + 1] for i in range(5))
                geu = bis[:, 6:7].bitcast(U32)
                fw.op(dve, lambda: V_.tensor_reduce(hi, scores[:, 0:Wd], axis=AX.X, op=ALU.max), reads=[sc_b], writes=[bis_b])
                fw.op(dve, lambda: V_.tensor_reduce(lo, scores[:, 0:256], axis=AX.X, op=ALU.min), reads=[sc_b], writes=[bis_b])
                fw.op(dve, lambda: V_.tensor_tensor(w0, hi, lo, op=ALU.subtract), reads=[bis_b], writes=[bis_b])
                fw.op(dve, lambda: V_.tensor_scalar(w0, w0, 1.0 + 1e-5, 1e-6, op0=ALU.mult, op1=ALU.add), reads=[bis_b], writes=[bis_b])
                junk = Mneg[:, j, 0:Wd]
                for it in range(BIS_ITERS):
                    ck = 2.0 ** -(it + 1)
                    fw.op(dve, lambda: V_.scalar_tensor_tensor(mid, w0, ck, lo, op0=ALU.mult, op1=ALU.add), reads=[bis_b], writes=[bis_b])
                    fw.op(dve, lambda: V_.tensor_scalar(junk, scores[:, 0:Wd], mid, 0.0, op0=ALU.is_ge, op1=ALU.add, accum_out=cnt),
                          reads=[sc_b, bis_b], writes=[mn_b, bis_b])
                    fw.op(dve, lambda: V_.tensor_scalar(geu, cnt, 255.5, None, op0=ALU.is_ge), reads=[bis_b], writes=[bis_b])
                    fw.op(dve, lambda: V_.copy_predicated(lo, geu, mid), reads=[bis_b], writes=[bis_b])
                fw.op(dve, lambda: V_.tensor_scalar(Mneg[:, j, 0:Wd], scores[:, 0:Wd], lo, NEG, op0=ALU.is_lt, op1=ALU.mult),
                      reads=[sc_b, bis_b], writes=[mn_b])

            ohb_b = [Buf("oh%d" % i) for i in range(4)]
            for j in range(2, 4):
                indexer(j, j)
            for tc in range(4):
                ts = slice(tc * 512, (tc + 1) * 512)
                u = tc % 2
                Mneg = MnegAll[:, u]
                oh_list = []
                for h in range(4):
                    def extra(kb, bank, query_only, cl=0):
                        js = [j for j in range(4) if (4 * tc + j) >= 2 and kb <= 4 * tc + j]
                        if query_only:
                            return len(js) > 0
                        for idx, j in enumerate(js):
                            lastj = idx == len(js) - 1
                            fw.op(pe, lambda: T.matmul(psb[bank][:, j * 128:(j + 1) * 128], Mneg[:, j, kb * 128:(kb + 1) * 128], identB,
                                                       start=False, stop=lastj),
                                  reads=[mn_bs[u][j]], writes=[ps_b[bank]], inc=lastj)
                        return True
                    ob, sbk = (2, 4) if h % 2 == 0 else (3, 5)
                    attn_core(tc, lambda kb: kvT[:, kb * 128:(kb + 1) * 128], bqT[:, h, ts], 4 + h,
                              lambda kb: kvt[:, kb, :], psb[ob][:, :], psb[sbk][:, :], ob, sbk, onesB,
                              reads=[bq_b, kv_b, kvt_b], extra=extra)
                    rs, rsb = recip_sum(sbk, psb[sbk][:, :])
                    o, ob_ = vb(36 + h, 512), ohb_b[h]
                    fw.op(dve, lambda: V_.tensor_tensor(o, psb[ob][:, :], rs, op=ALU.mult), reads=[ps_b[ob], rsb], writes=[ob_, WST_b[0]])
                    oh_list.append((o, ob_))
                    if tc + 1 < 4:
                        indexer(4 * (tc + 1) + h, h)
                for pr in range(2):
                    wb_ = 6 + pr
                    for hh in range(2):
                        h = 2 * pr + hh
                        fw.op(pe, lambda: T.matmul(psb[wb_][:, :], wuv2[:, h, :], oh_list[h][0], start=(hh == 0), stop=(hh == 1)),
                              reads=[wuv_b, oh_list[h][1], WST_b[0]], writes=[ps_b[wb_]], inc=(hh == 1))
                    fw.op(act, lambda: Sc.copy(oB[:, pr, ts], psb[wb_][:, :]), reads=[ps_b[wb_]], writes=[oB_b])

        def phase_C(l):
            ak = ARENA_K
            cqT = vb(ak, 2 * S).rearrange("p (c t) -> p c t", c=2)
            ckTz = vb(ak + 8, 4 * S).rearrange("p (c h t) -> p c h t", c=2, h=2)
            cvz = vb(ak + 24, 16 * 4 * 128).rearrange("p (b c h e) -> p b c h e", b=16, c=2, h=2)
            MT = vb(ak + 40, S, parts=32)
            selc = vb(ak + 44, 32 * 128, parts=32).rearrange("p (r s) -> p r s", r=32)
            ksum = vf(ak + 52, 16).rearrange("p (c n) -> p c n", c=2)
            bm = vf(ak + 52.5, 256).rearrange("p (j n) -> p j n", j=8)
            own = vf(ak + 53.5, 256).rearrange("p (j n) -> p j n", j=8)
            gm = vf(ak + 54.5, 32)
            m8 = vf(ak + 54.75, 8)
            selo = vf(ak + 55, 32)
            Mt = vb(ak + 55.25, 32)
            kmZ = vb(ak + 55.5, 32).rearrange("p (c h n) -> p c h n", c=2, h=2)
            onesz = vb(ak + 56, 256).rearrange("p (h e) -> p h e", h=2)
            cq_b, ck_b, cv_b, mt_b, sel_b, ks_b, km_b, bm_b, gm_b, oz_b = [Buf(n) for n in ("cq", "ck", "cv", "MT", "sel", "ks", "km", "bm", "gm", "oz")]
            fw.dma(pool, selc, sel_d.rearrange("p (r s) -> p r s", r=32), writes=[sel_b], max_dma_last_dim=2048)
            fw.dma(sp, bm, bm_d.partition_broadcast(128).rearrange("p (j n) -> p j n", j=8), writes=[bm_b])
            fw.dma(sp, own, own_d.partition_broadcast(128).rearrange("p (j n) -> p j n", j=8), writes=[bm_b])
            fw.op(pool, lambda: G.memset(ckTz[64:128, :, 0, :], 0.0), writes=[ck_b])
            fw.op(pool, lambda: G.memset(ckTz[0:64, :, 1, :], 0.0), writes=[ck_b])
            fw.op(pool, lambda: G.memset(cvz, 0.0), writes=[cv_b])
            fw.op(pool, lambda: G.memset(onesz, 0.0), writes=[oz_b])
            fw.op(pool, lambda: G.memset(onesz[:, 0, 0:64], 1.0), writes=[oz_b])
            fw.op(pool, lambda: G.memset(onesz[:, 1, 64:128], 1.0), writes=[oz_b])
            slab, sb = load_w_slab(win_d[l][:, O_CQ:O_CQ + 512], 512)
            for c in range(2):
                proj_fm(slab, sb, c * 128, 128, lambda tc: cqT[:, c, tc * 512:(tc + 1) * 512], [cq_b], scale=0.125)

            for c in range(2):
                def post(tc, bank):
                    fw.op(dve, lambda: V_.tensor_reduce(ksum[:, c, 2 * tc:2 * tc + 2], psb[bank][:, :].rearrange("p (b s) -> p b s", b=2),
                                                        axis=AX.X, op=ALU.add),
                          reads=[ps_b[bank]], writes=[ks_b])
                    return [ks_b]
                proj_fm(slab, sb, 256 + c * 128, 128, lambda tc: ckTz[0:64, c, 0, tc * 512:(tc + 1) * 512], [ck_b], post=post,
                        dst2_fn=lambda tc: ckTz[64:128, c, 1, tc * 512:(tc + 1) * 512])
            fw.op(dve, lambda: V_.memset(kmZ, 0.0), writes=[km_b])
            for hh in range(2):
                pr_ = slice(hh * 64, hh * 64 + 64)
                fw.op(dve, lambda: V_.tensor_scalar(kmZ[pr_, :, hh, :], ksum[pr_, :, :], 1.0 / 256, None, op0=ALU.mult), reads=[ks_b], writes=[km_b])
            slab, sb = load_w_slab(win_d[l][:, O_CV:O_CV + 256], 256)
            for tb in range(16):
                bank = 6 + (tb % 2)
                for k in range(8):
                    fw.op(pe, lambda: T.matmul(psb[bank][:, 0:256], uT[:, k, tb * 128:(tb + 1) * 128], slab[:, k, 0:256], start=(k == 0), stop=(k == 7)),
                          reads=[sb, uT_b[tb // 4]], writes=[ps_b[bank]], inc=(k == 7))
                src = psb[bank][:, 0:256].rearrange("p (c h e) -> p c h e", c=2, h=2)
                if tb % 2:
                    fw.op(dve, lambda: V_.tensor_copy(cvz[:, tb, :, 0, 0:64], src[:, :, 0, :]), reads=[ps_b[bank]], writes=[cv_b])
                    fw.op(dve, lambda: V_.tensor_copy(cvz[:, tb, :, 1, 64:128], src[:, :, 1, :]), reads=[ps_b[bank]], writes=[cv_b])
                else:
                    fw.op(act, lambda: Sc.copy(cvz[:, tb, :, 0, 0:64], src[:, :, 0, :]), reads=[ps_b[bank]], writes=[cv_b])
                    fw.op(act, lambda: Sc.copy(cvz[:, tb, :, 1, 64:128], src[:, :, 1, :]), reads=[ps_b[bank]], writes=[cv_b])
            for qb in range(16):
                jb = qb // 2
                bank = 6 + (qb % 2)
                for h in range(4):
                    c = h // 2
                    fw.op(pe, lambda: T.matmul(psb[bank][:, h * 8:(h + 1) * 8], cqT[:, c, qb * 128:(qb + 1) * 128], kmZ[:, c, h % 2, :],
                                               start=True, stop=True),
                          reads=[cq_b, km_b], writes=[ps_b[bank]], inc=(h == 3))
                fw.op(dve, lambda: V_.tensor_tensor(gm, psb[bank][:, 0:32], bm[:, jb, :], op=ALU.add), reads=[ps_b[bank], bm_b], writes=[gm_b])
                for h in range(4):
                    fw.op(dve, lambda: V_.max(m8, gm[:, h * 8:(h + 1) * 8]), reads=[gm_b], writes=[gm_b])
                    fw.op(dve, lambda: V_.tensor_scalar(selo[:, h * 8:(h + 1) * 8], gm[:, h * 8:(h + 1) * 8], m8[:, 2:3], None, op0=ALU.is_ge),
                          reads=[gm_b], writes=[gm_b])
                fw.op(dve, lambda: V_.tensor_tensor(selo, selo, own[:, jb, :], op=ALU.max), reads=[gm_b, bm_b], writes=[gm_b])
                fw.op(dve, lambda: V_.tensor_scalar(Mt, selo, -NEG, NEG, op0=ALU.mult, op1=ALU.add), reads=[gm_b], writes=[gm_b])
                pbf = psb[bank][:, :].bitcast(BF16)
                fw.op(pe, lambda: T.transpose(pbf[0:32, 0:128], Mt, identB), reads=[gm_b], writes=[ps_b[bank]])
                fw.op(act, lambda: Sc.copy(MT[:, qb * 128:(qb + 1) * 128], pbf[0:32, 0:128]), reads=[ps_b[bank]], writes=[mt_b])
            for c in range(2):
                for tc in range(4):
                    ts = slice(tc * 512, (tc + 1) * 512)
                    for hh in range(2):
                        h = 2 * c + hh

                        def extra(kb, bank, query_only, cl=0):
                            if query_only:
                                return True
                            fw.op(pe, lambda: T.matmul(psb[bank][:, cl:512], selc[:, h * 8 + kb // 2, :], MT[:, tc * 512 + cl:(tc + 1) * 512], start=False, stop=True),
                                  reads=[sel_b, mt_b], writes=[ps_b[bank]], inc=True)
                            return True
                        ob, sbk = (2, 4) if tc % 2 == 0 else (3, 5)
                        attn_core(tc, lambda kb: ckTz[:, c, hh, kb * 128:(kb + 1) * 128], cqT[:, c, ts], 8 + h,
                                  lambda kb: cvz[:, kb, c, hh, :], psb[ob][:, :], psb[sbk][:, :], ob, sbk, onesz[:, hh, :],
                                  reads=[cq_b, ck_b, cv_b, oz_b], extra=extra, first=(hh == 0), last_grp=(hh == 1))
                    rs, rsb = recip_sum(sbk, psb[sbk][:, :])
                    fw.op(dve, lambda: V_.tensor_tensor(oC[:, c, ts], psb[ob][:, :], rs, op=ALU.mult), reads=[ps_b[ob], rsb], writes=[oC_b])

        def phase_M(l, b):
            P = PRM[(l, b)]
            wg = [vb(36 + 8 * i, 8 * 384).rearrange("p (k n) -> p k n", k=8) for i in range(2)]
            wbr = [vb(36 + 8 * i + 6, 8 * 128).rearrange("p (k n) -> p k n", k=8) for i in range(2)]
            srcs = [(oA, oA_b, 4, wbra_d, 0), (oB, oB_b, 2, wbrb_d, 4), (oC, oC_b, 2, wbrc_d, 6)]
            def load_m(mf):
                i = mf % 2
                for j in range(3):
                    c0 = O_G + j * 1024 + mf * 128
                    fw.dma(pool, wg[i][:, :, j * 128:(j + 1) * 128], win_d[l][:, c0:c0 + 128].rearrange("(k p) n -> p k n", p=128), writes=[WST_b[i]])
                for (o_, ob, nk, wd, k0) in srcs:
                    fw.dma(pool, wbr[i][:, k0:k0 + nk, :], wd[l][:, mf * 128:(mf + 1) * 128].rearrange("(k p) n -> p k n", p=128), writes=[WST_b[i]])

            load_m(0)
            for mf in range(8):
                i = mf % 2
                wgi, wbi, wb_ = wg[i], wbr[i], WST_b[i]
                if mf + 1 < 8:
                    load_m(mf + 1)
                for tc in range(4):
                    ts = slice(tc * 512, (tc + 1) * 512)
                    sig = []
                    for bi in range(3):
                        gbank = (0, 1, 6)[bi]
                        for k in range(8):
                            fw.op(pe, lambda: T.matmul(psb[gbank][:, :], wgi[:, k, bi * 128:(bi + 1) * 128], uT[:, k, ts], start=(k == 0), stop=(k == 7)),
                                  reads=[wb_, uT_b[tc]], writes=[ps_b[gbank]], inc=(k == 7))
                        sg, sgb = next_pt()
                        fw.op(act, lambda: Sc.activation(sg, psb[gbank][:, :], AF.Sigmoid, bias=gateb[l][:, bi * 8 + mf:bi * 8 + mf + 1]),
                              reads=[ps_b[gbank]], writes=[sgb])
                        sig.append((sg, sgb))
                    terms = []
                    for bi, (o_, ob, nk, wd, k0) in enumerate(srcs):
                        ybank = (2, 3, 4)[bi]
                        for k in range(nk):
                            fw.op(pe, lambda: T.matmul(psb[ybank][:, :], wbi[:, k0 + k, :], o_[:, k, ts], start=(k == 0), stop=(k == nk - 1)),
                                  reads=[wb_, ob], writes=[ps_b[ybank]], inc=(k == nk - 1))
                        tm, tmb = next_tmp()
                        fw.op(dve, lambda: V_.tensor_tensor(tm, psb[ybank][:, :], sig[bi][0], op=ALU.mult), reads=[ps_b[ybank], sig[bi][1]], writes=[tmb])
                        terms.append((tm, tmb))
                    fw.op(pool, lambda: G.tensor_tensor(terms[0][0], terms[0][0], terms[1][0], op=ALU.add), reads=[terms[1][1]], writes=[terms[0][1]])
                    fw.op(pool, lambda: G.tensor_tensor(merged[:, mf, ts], terms[0][0], terms[2][0], op=ALU.add),
                          reads=[terms[0][1], terms[2][1]], writes=[merged_b[tc]])
            for half in range(2):
                slab, sb = load_w_slab(wo_d[l][:, half * 512:(half + 1) * 512], 512)
                for f4 in range(4):
                    f = half * 4 + f4
                    for tc in range(4):
                        ts = slice(tc * 512, (tc + 1) * 512)
                        bank = (f4 * 4 + tc) % 8
                        for k in range(8):
                            fw.op(pe, lambda: T.matmul(psb[bank][:, :], slab[:, k, f4 * 128:(f4 + 1) * 128], merged[:, k, ts], start=(k == 0), stop=(k == 7)),
                                  reads=[sb, merged_b[tc]], writes=[ps_b[bank]], inc=(k == 7))
                        fw.op(dve, lambda: V_.scalar_tensor_tensor(xT[:, f, ts], psb[bank][:, :], P[:, 2, f:f + 1], xT[:, f, ts], op0=ALU.mult, op1=ALU.add),
                              reads=[ps_b[bank], xT_b[tc]], writes=[xT_b[tc]])

        ffw1 = [vb(136 + 32 * i, 8 * 1024).rearrange("p (k n) -> p k n", k=8) for i in range(2)]
        ffw2 = [vb(152 + 32 * i, 8 * 1024).rearrange("p (k n) -> p k n", k=8) for i in range(2)]
        ffw_b = [Buf("ffw0"), Buf("ffw1")]

        def load_f(l, cg):
            i = cg % 2
            for hf in range(2):
                fw.dma(pool, ffw1[i][:, :, hf * 512:(hf + 1) * 512],
                       wff1_d[l][:, cg * 1024 + hf * 512:cg * 1024 + (hf + 1) * 512].rearrange("(k p) n -> p k n", p=128), writes=[ffw_b[i]])
            for hf in range(2):
                fw.dma(pool, ffw2[i][:, :, hf * 512:(hf + 1) * 512],
                       wff2_d[l][cg * 1024:(cg + 1) * 1024, hf * 512:(hf + 1) * 512].rearrange("(k p) n -> p k n", p=128), writes=[ffw_b[i]])

        def phase_F(l, b):
            P = PRM[(l, b)]
            w1, w2, w_b = ffw1, ffw2, ffw_b
            hT = [vb(56 + i, 512) for i in range(8)]
            h_b = Buf("hT")
            rT = [TMP[4 + i] for i in range(4)]
            r_b = [TMP_b[4 + i] for i in range(4)]
            for cg in range(4):
                i = cg % 2
                if 1 <= cg and cg + 1 < 4:
                    load_f(l, cg + 1)
                for tc in range(4):
                    ts = slice(tc * 512, (tc + 1) * 512)
                    for c in range(8):
                        bank = c % 2
                        for k in range(8):
                            fw.op(pe, lambda: T.matmul(psb[bank][:, :], w1[i][:, k, c * 128:(c + 1) * 128], uT[:, k, ts], start=(k == 0), stop=(k == 7)),
                                  reads=[w_b[i], uT_b[tc]], writes=[ps_b[bank]], inc=(k == 7))
                        r, rb = rT[c % 4], r_b[c % 4]
                        fw.op(act, lambda: Sc.activation(r, psb[bank][:, :], AF.Relu), reads=[ps_b[bank]], writes=[rb])
                        fw.op(pool, lambda: G.tensor_tensor(hT[c], r, r, op=ALU.mult), reads=[rb], writes=[h_b])
                    for f in range(8):
                        bank = 2 + (f % 6)
                        for c in range(8):
                            fw.op(pe, lambda: T.matmul(psb[bank][:, :], w2[i][:, c, f * 128:(f + 1) * 128], hT[c], start=(c == 0), stop=(c == 7)),
                                  reads=[w_b[i], h_b], writes=[ps_b[bank]], inc=(c == 7))
                        fw.op(dve, lambda: V_.scalar_tensor_tensor(xT[:, f, ts], psb[bank][:, :], P[:, 5, f:f + 1], xT[:, f, ts], op0=ALU.mult, op1=ALU.add),
                              reads=[ps_b[bank], xT_b[tc]], writes=[xT_b[tc]])

        def load_x(b):
            xin = [vf(56 + 4 * i, 1024) for i in range(2)]
            xin_b = [Buf("xin0"), Buf("xin1")]
            for tb in range(16):
                i = tb % 2
                fw.dma(sp, xin[i], x_d[b, tb * 128:(tb + 1) * 128, :], writes=[xin_b[i]])
                for half in range(2):
                    bank = 2 * i + half
                    for j in range(4):
                        kc = half * 4 + j
                        fw.op(pe, lambda: T.transpose(psb[bank][:, j * 128:(j + 1) * 128], xin[i][:, kc * 128:(kc + 1) * 128], identF),
                              reads=[xin_b[i]], writes=[ps_b[bank]], inc=(j == 3))
                    dst = xT[:, half * 4:(half + 1) * 4, tb * 128:(tb + 1) * 128]
                    src = psb[bank][:, :].rearrange("p (a c) -> p a c", a=4)
                    if half == 0:
                        fw.op(act, lambda: Sc.copy(dst, src), reads=[ps_b[bank]], writes=[xT_b[tb // 4]])
                    else:
                        fw.op(dve, lambda: V_.tensor_copy(dst, src), reads=[ps_b[bank]], writes=[xT_b[tb // 4]])

        def store_out(b):
            ot = [vf(56 + 4 * i, 1024) for i in range(2)]
            ot_b = [Buf("ot0"), Buf("ot1")]
            toks = []
            for tb in range(16):
                i = tb % 2
                for half in range(2):
                    bank = 2 * i + half
                    for j in range(4):
                        kc = half * 4 + j
                        fw.op(pe, lambda: T.transpose(psb[bank][:, j * 128:(j + 1) * 128], xT[:, kc, tb * 128:(tb + 1) * 128], identF),
                              reads=[xT_b[tb // 4]], writes=[ps_b[bank]], inc=(j == 3))
                    dst = ot[i][:, half * 512:(half + 1) * 512]
                    if half == 0:
                        fw.op(act, lambda: Sc.copy(dst, psb[bank][:, :]), reads=[ps_b[bank]], writes=[ot_b[i]])
                    else:
                        fw.op(dve, lambda: V_.tensor_copy(dst, psb[bank][:, :]), reads=[ps_b[bank]], writes=[ot_b[i]])
                toks.append(fw.dma(sp, out_d[b, tb * 128:(tb + 1) * 128, :], ot[i], reads=[ot_b[i]]))
            return toks

        def spill_x():
            for k in range(8):
                fw.dma(sp, xsp_d[:, k, :], xT[:, k, :], reads=xT_b, writes=[xsp_b])

        def reload_x():
            for k in range(8):
                fw.dma(sp, xT[:, k, :], xsp_d[:, k, :], reads=[xsp_b], writes=xT_b)

        out_toks = []
        for b in range(nseq):
            load_x(b)
            fw.barrier()
            P0 = PRM[(0, b)]
            norm_mod(P0[:, 0, :], P0[:, 1, :])
            if b == 0:
                dump("u0", uT, [128, 8, S], BF16, uT_b)
            spill_x()
            fw.barrier()
            for l in range(L):
                if stop_after != "pre" and "A" not in SKIP:
                    load_strips(range(0, 4))
                    fw.barrier()
                    phase_A(l)
                    fw.barrier()
                    if b == 0 and l == 0:
                        dump("oA", oA, [128, 4, S], BF16, [oA_b])
                if stop_after not in ("pre", "A") and "B" not in SKIP:
                    load_strips(range(4, 8))
                    fw.barrier()
                    phase_B(l)
                    fw.barrier()
                    if b == 0 and l == 0:
                        dump("oB", oB, [128, 2, S], BF16, [oB_b])
                if stop_after not in ("pre", "A", "B"):
                    load_strips(range(8, 12))
                    fw.barrier()
                    phase_C(l)
                    fw.barrier()
                    if b == 0 and l == 0:
                        dump("oC", oC, [128, 2, S], BF16, [oC_b])
                reload_x()
                fw.barrier()
                if stop_after not in ("pre", "A", "B", "C"):
                    phase_M(l, b)
                    fw.barrier()
                    if b == 0 and l == 0:
                        dump("x1", xT, [128, 8, S], F32, xT_b)
                    P = PRM[(l, b)]
                    load_f(l, 0)
                    load_f(l, 1)
                    norm_mod(P[:, 3, :], P[:, 4, :])
                    fw.barrier()
                    phase_F(l, b)
                    fw.barrier()
                    if b == 0 and l == 0:
                        dump("x2", xT, [128, 8, S], F32, xT_b)
                if l + 1 < L:
                    Pn = PRM[(l + 1, b)]
                    norm_mod(Pn[:, 0, :], Pn[:, 1, :])
                    spill_x()
                    fw.barrier()
            norm_mod(nfin, None)
            fw.barrier()
            out_toks += store_out(b)
            fw.barrier()
        fw._wait(sp, out_toks)
        fw.barrier()
        stats = {e.name: e.ninst for e in fw.engs}
    return nc, dbg_out, stats


_CONSTS = None


def _consts():
    global _CONSTS
    if _CONSTS is None:
        ident = np.eye(128, dtype=np.float32)
        anti = np.ascontiguousarray(ident[::-1])
        caus = np.where(np.arange(128)[None, :] <= np.arange(128)[:, None], 0.0, -1e30).astype(np.float32)
        sel = np.zeros((32, 32, 128), np.float32)
        for r in range(32):
            sel[r, r, :] = 1.0
        bm = np.zeros((8, 4, 8), np.float32)
        own = np.zeros((8, 4, 8), np.float32)
        for j in range(8):
            bm[j, :, j:] = -1e30
            own[j, :, j] = 1.0
        _CONSTS = dict(k_ident=ident, k_anti=anti, k_onehot=_t5_onehot(), k_caus=caus,
                       k_sel=sel.reshape(32, 32 * 128), k_bm=bm.reshape(-1), k_own=own.reshape(-1))
    return _CONSTS


def make_in_maps(inputs, n_cores, nseq, nlayer=2):
    f = lambda a: np.ascontiguousarray(np.asarray(a, dtype=np.float32))
    L = nlayer
    x = f(inputs["x"])
    c = f(inputs["c"])
    shared = dict(
        rel_bias=f(inputs["rel_bias"]),
        ada_w=f(inputs["ada_w"])[:L],
        ada_bT=f(f(inputs["ada_b"])[:L].reshape(L, 48, 128).transpose(0, 2, 1)),
        norm_mixT=f(f(inputs["norm_mix"])[:L].reshape(L, 8, 128).transpose(0, 2, 1)),
        w_in=f(inputs["w_in"])[:L],
        gate_bT=f(f(inputs["gate_b"])[:L].reshape(L, 24, 128).transpose(0, 2, 1)),
        diff_lambda=f(inputs["diff_lambda"])[:L].reshape(L, 256),
        diff_subln=f(inputs["diff_subln"])[:L].reshape(L, 128, 1),
        dsa_kv_norm=f(inputs["dsa_kv_norm"])[:L].reshape(L, 128, 1),
        dsa_w_uv=f(inputs["dsa_w_uv"])[:L],
        w_br_a=f(inputs["w_br_a"])[:L], w_br_b=f(inputs["w_br_b"])[:L], w_br_c=f(inputs["w_br_c"])[:L],
        w_o=f(inputs["w_o"])[:L],
        norm_mlpT=f(f(inputs["norm_mlp"])[:L].reshape(L, 8, 128).transpose(0, 2, 1)),
        w_ff1=f(inputs["w_ff1"])[:L], w_ff2=f(inputs["w_ff2"])[:L],
        norm_finalT=f(f(inputs["norm_final"]).reshape(8, 128).T),
    )
    shared.update(_consts())
    maps = []
    for i in range(n_cores):
        m = dict(shared)
        m["x"] = f(x[i * nseq:(i + 1) * nseq])
        m["c_lay"] = f(c[i * nseq:(i + 1) * nseq].reshape(nseq, 8, 128).transpose(2, 1, 0))
        maps.append(m)
    return maps


def kernel(**inputs):
    n_cores, nseq = 8, 2
    nc, _, _ = build(nseq=nseq, nlayer=2)
    maps = make_in_maps(inputs, n_cores, nseq)
    res = run_bass_kernel_spmd(nc, maps, core_ids=list(range(n_cores)))
    out = np.concatenate([np.asarray(r["out"]) for r in res.results], axis=0)
    return out.astype(np.float32)
```

```python
import contextlib
import math
import numpy as np
import concourse.bass as bass
import concourse.mybir as mybir
from concourse.bass_utils import run_bass_kernel_spmd

F32 = mybir.dt.float32
BF16 = mybir.dt.bfloat16
U32 = mybir.dt.uint32
AF = mybir.ActivationFunctionType
ALU = mybir.AluOpType
AX = mybir.AxisListType

S = 2048
D = 1024
NEG = -30000.0
EPS = 1e-6
O_AQ, O_AK, O_AV, O_BQ, O_BKV, O_BIQ, O_BIK, O_BIW, O_CQ, O_CK, O_CV, O_G = (
    0, 512, 1024, 1536, 2048, 2176, 2432, 2464, 2472, 2728, 2984, 3240)
IN_COLS = 6312
BIS_ITERS = 11
SAME_DIST = 1 << 30
CSTOP = 0
SKIP = ()


class Buf:
    __slots__ = ("name", "w", "r")

    def __init__(self, name=""):
        self.name = name
        self.w = None
        self.r = {}


class Eng:
    def __init__(self, name, h, same_sync):
        self.name = name
        self.h = h
        self.sem = None
        self.count = 0
        self.seen = {}
        self.same_sync = same_sync
        self.ninst = 0
        self.pos = 0


class Fw:
    def __init__(self, nc, stack, n_dma_sems=8, same_sync=True):
        self.nc = nc
        self.pe = Eng("pe", nc.tensor, False)
        self.act = Eng("act", nc.scalar, same_sync)
        self.dve = Eng("dve", nc.vector, same_sync)
        self.pool = Eng("pool", nc.gpsimd, same_sync)
        self.sp = Eng("sp", nc.sync, False)
        self.engs = [self.pe, self.act, self.dve, self.pool, self.sp]
        for e in self.engs:
            e.sem = stack.enter_context(nc.semaphore("s_" + e.name))
        self.dma_pools = {}
        for q in ("sp", "pool"):
            sems = [stack.enter_context(nc.semaphore("d_%s%d" % (q, i))) for i in range(n_dma_sems)]
            self.dma_pools[q] = dict(sems=sems, cnt=[0] * n_dma_sems, nxt=0)

    def _need(self, eng, tok):
        sem, val, owner = tok[0], tok[1], tok[2]
        if owner is eng:
            if not eng.same_sync:
                return False
            if eng.pos - tok[3] >= SAME_DIST:
                return False
        return eng.seen.get(id(sem), 0) < val

    def _wait(self, eng, toks):
        best = {}
        for t in toks:
            if t is None or not self._need(eng, t):
                continue
            k = id(t[0])
            if k not in best or best[k][1] < t[1]:
                best[k] = t
        for k, t in best.items():
            eng.h.wait_ge(t[0], t[1])
            eng.seen[k] = t[1]
            eng.ninst += 1

    @staticmethod
    def _deps(reads, writes):
        toks = []
        for b in reads:
            toks.append(b.w)
        for b in writes:
            toks.append(b.w)
            toks.extend(b.r.values())
        return toks

    @staticmethod
    def _record(tok, reads, writes):
        k = id(tok[0])
        for b in reads:
            o = b.r.get(k)
            if o is None or o[1] < tok[1]:
                b.r[k] = tok
        for b in writes:
            b.w = tok
            b.r = {}

    def op(self, eng, fn, reads=(), writes=(), inc=True, nosync=False):
        toks = []
        for b in reads:
            toks.append(b.w)
        for b in writes:
            rd = list(b.r.values())
            if not (b.w is not None and b.w[2] is eng and any(t[2] is not eng for t in rd)):
                toks.append(b.w)
            toks.extend(rd)
        self._wait(eng, toks)
        ins = fn()
        eng.ninst += 1
        eng.pos += 1
        if inc:
            ins.then_inc(eng.sem, 1)
            eng.count += 1
            tok = (eng.sem, eng.count, eng, eng.pos)
        else:
            tok = (eng.sem, eng.count + 1, eng, eng.pos + 1)
        self._record(tok, reads, writes)
        return tok

    def dma(self, eng, out, in_, reads=(), writes=(), **kw):
        pool = self.dma_pools[eng.name]
        self._wait(eng, self._deps(reads, writes))
        j = pool["nxt"]
        pool["nxt"] = (j + 1) % len(pool["sems"])
        sem = pool["sems"][j]
        if pool["cnt"][j] > 0 and eng.seen.get(id(sem), 0) < pool["cnt"][j]:
            eng.h.wait_ge(sem, pool["cnt"][j])
            eng.seen[id(sem)] = pool["cnt"][j]
        ins = eng.h.dma_start(out=out, in_=in_, **kw)
        ins.then_inc(sem, 16)
        eng.ninst += 1
        pool["cnt"][j] += 16
        tok = (sem, pool["cnt"][j], None, 0)
        self._record(tok, reads, writes)
        return tok

    def barrier(self):
        sp = self.sp
        toks = []
        for e in self.engs:
            if e is not sp and e.count > 0:
                toks.append((e.sem, e.count, e, e.pos))
        for q, pool in self.dma_pools.items():
            for j, sem in enumerate(pool["sems"]):
                if pool["cnt"][j] > 0:
                    toks.append((sem, pool["cnt"][j], None, 0))
        self._wait(sp, toks)
        ins = sp.h.nop()
        ins.then_inc(sp.sem, 1)
        sp.count += 1
        tok = (sp.sem, sp.count, sp, sp.pos)
        for e in self.engs:
            if e is sp:
                continue
            e.h.wait_ge(sp.sem, sp.count)
            e.seen[id(sp.sem)] = sp.count
            for t in toks:
                k = id(t[0])
                if e.seen.get(k, 0) < t[1]:
                    e.seen[k] = t[1]
        return tok


def _t5_onehot():
    dd = np.arange(1280, dtype=np.int64) - 511
    n = np.maximum(dd, 0)
    nf = np.maximum(n, 1).astype(np.float32)
    large = 16 + (np.log(nf / np.float32(16)) / np.float32(math.log(128 / 16)) * np.float32(16)).astype(np.int32)
    large = np.minimum(large, 31)
    bucket = np.where(n < 16, n, large)
    oh = np.zeros((33, 1280), np.float32)
    for j in range(1280):
        if dd[j] < 0:
            oh[32, j] = 1.0
        else:
            oh[bucket[j], j] = 1.0
    return oh


def build(nseq=2, nlayer=2, dbg=(), stop_after=None, same_sync=True):
    nc = bass.Bass("TRN2", target_bir_lowering=False)
    L = nlayer

    def din(name, shape, dt=F32):
        return nc.dram_tensor(name, list(shape), dt, kind="ExternalInput").ap()

    x_d = din("x", [nseq, S, D])
    c_d = din("c_lay", [128, 8, nseq])
    relb_d = din("rel_bias", [32, 12])
    adaw_d = din("ada_w", [L, D, 6 * D])
    adab_d = din("ada_bT", [L, 128, 48])
    nmix_d = din("norm_mixT", [L, 128, 8])
    win_d = din("w_in", [L, D, IN_COLS])
    gateb_d = din("gate_bT", [L, 128, 24])
    dlam_d = din("diff_lambda", [L, 256])
    subln_d = din("diff_subln", [L, 128, 1])
    kvn_d = din("dsa_kv_norm", [L, 128, 1])
    wuv_d = din("dsa_w_uv", [L, 4, 128, 64])
    wbra_d = din("w_br_a", [L, 512, D])
    wbrb_d = din("w_br_b", [L, 256, D])
    wbrc_d = din("w_br_c", [L, 256, D])
    wo_d = din("w_o", [L, D, D])
    nmlp_d = din("norm_mlpT", [L, 128, 8])
    wff1_d = din("w_ff1", [L, D, 4 * D])
    wff2_d = din("w_ff2", [L, 4 * D, D])
    nfin_d = din("norm_finalT", [128, 8])
    ident_d = din("k_ident", [128, 128])
    anti_d = din("k_anti", [128, 128])
    oh_d = din("k_onehot", [33, 1280])
    caus_d = din("k_caus", [128, 128])
    sel_d = din("k_sel", [32, 32 * 128])
    bm_d = din("k_bm", [8 * 32])
    own_d = din("k_own", [8 * 32])
    out_d = nc.dram_tensor("out", [nseq, S, D], F32, kind="ExternalOutput").ap()
    g_d = nc.dram_tensor("g_scr", [12, 1280], BF16, kind="Internal").ap()
    xsp_d = nc.dram_tensor("x_spill", [128, 8, S], F32, kind="Internal").ap()
    dbg_out = {}

    with contextlib.ExitStack() as st:
        fw = Fw(nc, st, same_sync=same_sync)
        pe, act, dve, pool, sp = fw.pe, fw.act, fw.dve, fw.pool, fw.sp
        T, V_, Sc, G = nc.tensor, nc.vector, nc.scalar, nc.gpsimd
        arena = st.enter_context(nc.sbuf_tensor("arena", [128, 51200], F32))
        psb = [st.enter_context(nc.psum_tensor("ps%d" % i, [128, 512], F32)) for i in range(8)]
        ps_b = [Buf("ps%d" % i) for i in range(8)]

        KW = 256

        def vf(off_k, nwords, parts=128, p0=0):
            o = int(round(off_k * KW))
            return arena[p0:p0 + parts, o:o + nwords]

        def vb(off_k, nelem, parts=128, p0=0):
            o = int(round(off_k * KW))
            return arena[p0:p0 + parts, o:o + nelem // 2].bitcast(BF16)

        identF = vf(0, 128)
        identB = vb(0.5, 128)
        antiB = vb(0.75, 128)
        onesB = vb(1.0, 128)
        epsT = vf(1.25, 1)
        halfT = vf(1.25, 1)
        neglam = [vf(1.26 + 0.01 * l, 1) for l in range(L)]
        def wv(word, n, parts=128):
            return arena[0:parts, word:word + n]
        W0 = 330
        epsT = wv(W0, 1)
        neglam = [wv(W0 + 1 + l, 1) for l in range(L)]
        subg = [wv(W0 + 4 + l, 1) for l in range(L)]
        kvg = [wv(W0 + 8 + l, 1) for l in range(L)]
        nfin = wv(W0 + 12, 8)
        zeroT = wv(W0 + 20, 1)
        c31 = wv(1000, 12)
        gateb = [wv(W0 + 24 + 24 * l, 24) for l in range(L)]
        PRM = {}
        w = W0 + 80
        for l in range(L):
            for b in range(nseq):
                PRM[(l, b)] = wv(w, 48).rearrange("p (j k) -> p j k", j=6)
                w += 48
        assert w <= 1024
        uT = vb(4, 8 * S).rearrange("p (k t) -> p k t", k=8)
        WST = [vb(36 + 8 * i, 8 * 512).rearrange("p (k n) -> p k n", k=8) for i in range(2)]
        PT = [vb(52 + i, 512) for i in range(4)]
        TMP = [vf(56 + 2 * i, 512) for i in range(8)]
        uT_b = [Buf("uT%d" % i) for i in range(4)]
        WST_b = [Buf("wst%d" % i) for i in range(2)]
        PT_b = [Buf("pt%d" % i) for i in range(4)]
        TMP_b = [Buf("tmp%d" % i) for i in range(8)]
        xT = vf(72, 8 * S).rearrange("p (k t) -> p k t", k=8)
        xT_b = [Buf("xT%d" % i) for i in range(4)]
        STR = vb(72, 12 * 1152).rearrange("p (h n) -> p h n", h=12)
        ARENA_K = 99
        merged = vb(136, 8 * S).rearrange("p (k t) -> p k t", k=8)
        merged_b = [Buf("mg%d" % i) for i in range(4)]
        oA = vb(168, 4 * S).rearrange("p (k t) -> p k t", k=4)
        oB = vb(184, 2 * S).rearrange("p (k t) -> p k t", k=2)
        oC = vb(192, 2 * S).rearrange("p (k t) -> p k t", k=2)
        oA_b, oB_b, oC_b = Buf("oA"), Buf("oB"), Buf("oC")
        xsp_b = Buf("xsp")
        gd_b = Buf("gd")

        state = {"pt": 0, "tmp": 0, "wst": 0, "lg": 0}

        def next_pt():
            i = state["pt"]
            state["pt"] = (i + 1) % 4
            return PT[i], PT_b[i]

        def next_tmp(avoid=()):
            i = state["tmp"]
            while any(TMP_b[i] is a for a in avoid):
                i = (i + 1) % 8
            state["tmp"] = (i + 1) % 8
            return TMP[i], TMP_b[i]

        def next_wst():
            i = state["wst"]
            state["wst"] = (i + 1) % 2
            return WST[i], WST_b[i]

        def dump(name, ap, shape, dt, reads):
            if name not in dbg:
                return
            d = nc.dram_tensor("dbg_" + name, list(shape), dt, kind="ExternalOutput").ap()
            dbg_out[name] = d
            fw.barrier()
            t = fw.dma(sp, d, ap, reads=reads)
            fw._wait(sp, [t])

        def load_w_slab(src_ap, ncols, eng=None):
            slab, sb = next_wst()
            fw.dma(pool, slab[:, :, 0:ncols], src_ap.rearrange("(k p) n -> p k n", p=128), writes=[sb])
            return slab, sb

        cb = Buf("consts")
        fw.dma(sp, identF, ident_d, writes=[cb])
        fw.dma(pool, identB, ident_d, writes=[cb])
        fw.dma(pool, antiB, anti_d, writes=[cb])
        fw.op(dve, lambda: V_.memset(onesB, 1.0), writes=[cb])
        fw.op(dve, lambda: V_.memset(epsT, EPS), writes=[cb])
        fw.op(dve, lambda: V_.memset(zeroT, 0.0), writes=[cb])
        fw.dma(sp, nfin, nfin_d, writes=[cb])
        fw.dma(sp, c31, relb_d[31].partition_broadcast(128), writes=[cb])
        for l in range(L):
            fw.dma(sp, subg[l], subln_d[l], writes=[cb])
            fw.dma(sp, kvg[l], kvn_d[l], writes=[cb])
            fw.dma(sp, gateb[l], gateb_d[l], writes=[cb])
        fw.barrier()
        sa = 72
        relb = vf(sa, 12, parts=33)
        ohT = vf(sa + 1, 1280, parts=33)
        grow = vb(sa + 7, 1280, parts=12)
        tb_ = Buf("setup")
        fw.op(dve, lambda: V_.memset(vf(sa, 12, parts=64)[32:64, :], NEG), writes=[tb_])
        fw.dma(sp, relb[0:32, :], relb_d, writes=[tb_])
        fw.dma(sp, ohT, oh_d, writes=[tb_])
        for ci, (c0, cn) in enumerate(((0, 512), (512, 512), (1024, 256))):
            fw.op(pe, lambda: T.matmul(psb[ci][0:12, 0:cn], relb, ohT[:, c0:c0 + cn], start=True, stop=True),
                  reads=[tb_], writes=[ps_b[ci]])
            fw.op(dve, lambda: V_.tensor_copy(grow[:, c0:c0 + cn], psb[ci][0:12, 0:cn]), reads=[ps_b[ci]], writes=[tb_])
        fw.dma(sp, g_d, grow, reads=[tb_], writes=[gd_b])
        for l in range(L):
            lam_init = 0.8 - 0.6 * math.exp(-0.3 * l)
            dl = vf(sa + 12, 256)
            pr = vf(sa + 13, 128)
            s12 = vf(sa + 14, 2)
            e12 = vf(sa + 14.5, 2)
            fw.dma(sp, dl, dlam_d[l].partition_broadcast(128), writes=[tb_])
            fw.op(dve, lambda: V_.tensor_tensor(pr[:, 0:64], dl[:, 0:64], dl[:, 64:128], op=ALU.mult), reads=[tb_], writes=[tb_])
            fw.op(dve, lambda: V_.tensor_tensor(pr[:, 64:128], dl[:, 128:192], dl[:, 192:256], op=ALU.mult), reads=[tb_], writes=[tb_])
            fw.op(dve, lambda: V_.tensor_reduce(s12, pr.rearrange("p (a b) -> p a b", a=2), axis=AX.X, op=ALU.add), reads=[tb_], writes=[tb_])
            fw.op(act, lambda: Sc.activation(e12, s12, AF.Exp), reads=[tb_], writes=[tb_])
            fw.op(dve, lambda: V_.tensor_tensor(s12[:, 0:1], e12[:, 1:2], e12[:, 0:1], op=ALU.subtract), reads=[tb_], writes=[tb_])
            fw.op(dve, lambda: V_.tensor_scalar(neglam[l], s12[:, 0:1], -lam_init, None, op0=ALU.add), reads=[tb_], writes=[tb_])
            fw.op(dve, lambda: V_.tensor_scalar(subg[l], subg[l], 1.0 - lam_init, None, op0=ALU.mult), reads=[tb_], writes=[tb_])
        cT = vf(sa + 16, 8 * nseq).rearrange("p (k b) -> p k b", k=8)
        modT = vf(sa + 17, 48 * nseq).rearrange("p (f b) -> p f b", f=48)
        adab = vf(sa + 18, 48)
        nmx = vf(sa + 19, 8)
        nml = vf(sa + 19.5, 8)
        fw.dma(sp, cT, c_d, writes=[tb_])
        fw.op(act, lambda: Sc.activation(cT, cT, AF.Silu), reads=[tb_], writes=[tb_])
        cTb = vb(sa + 16.25, 8 * nseq).rearrange("p (k b) -> p k b", k=8)
        fw.op(dve, lambda: V_.tensor_copy(cTb, cT), reads=[tb_], writes=[tb_])
        slabs = [vb(136 + 8 * i, 8 * 512).rearrange("p (k n) -> p k n", k=8) for i in range(4)]
        slab_b = [Buf("adaslab%d" % i) for i in range(4)]
        si = 0
        for l in range(L):
            fw.dma(sp, adab, adab_d[l], writes=[tb_])
            fw.dma(sp, nmx, nmix_d[l], writes=[tb_])
            fw.dma(sp, nml, nmlp_d[l], writes=[tb_])
            for sl in range(12):
                sb_, sbb = slabs[si % 4], slab_b[si % 4]
                si += 1
                fw.dma(pool, sb_, adaw_d[l][:, sl * 512:(sl + 1) * 512].rearrange("(k p) n -> p k n", p=128), writes=[sbb])
                bank = 4 + (sl % 2)
                for fc4 in range(4):
                    for k in range(8):
                        fw.op(pe, lambda: T.matmul(psb[bank][:, fc4 * nseq:(fc4 + 1) * nseq], sb_[:, k, fc4 * 128:(fc4 + 1) * 128], cTb[:, k, :],
                                                   start=(k == 0), stop=(k == 7)),
                              reads=[sbb, tb_], writes=[ps_b[bank]], inc=(k == 7 and fc4 == 3))
                for fc4 in range(4):
                    fc = sl * 4 + fc4
                    fw.op(dve, lambda: V_.tensor_scalar(modT[:, fc, :], psb[bank][:, fc4 * nseq:(fc4 + 1) * nseq], adab[:, fc:fc + 1], None, op0=ALU.add),
                          reads=[ps_b[bank], tb_], writes=[tb_])
            for b in range(nseq):
                P = PRM[(l, b)]
                fw.op(dve, lambda: V_.scalar_tensor_tensor(P[:, 0, :], modT[:, 8:16, b], 1.0, nmx, op0=ALU.add, op1=ALU.mult), reads=[tb_], writes=[tb_])
                fw.op(dve, lambda: V_.tensor_copy(P[:, 1, :], modT[:, 0:8, b]), reads=[tb_], writes=[tb_])
                fw.op(dve, lambda: V_.tensor_copy(P[:, 2, :], modT[:, 16:24, b]), reads=[tb_], writes=[tb_])
                fw.op(dve, lambda: V_.scalar_tensor_tensor(P[:, 3, :], modT[:, 32:40, b], 1.0, nml, op0=ALU.add, op1=ALU.mult), reads=[tb_], writes=[tb_])
                fw.op(dve, lambda: V_.tensor_copy(P[:, 4, :], modT[:, 24:32, b]), reads=[tb_], writes=[tb_])
                fw.op(dve, lambda: V_.tensor_copy(P[:, 5, :], modT[:, 40:48, b]), reads=[tb_], writes=[tb_])
        fw.barrier()

        def rstd_from_ss(ss_bank, inv_n):
            t1, t1b = next_tmp()
            fw.op(act, lambda: Sc.activation(t1, psb[ss_bank][:, :], AF.Ln, bias=epsT, scale=inv_n), reads=[ps_b[ss_bank]], writes=[t1b])
            t2, t2b = next_tmp()
            fw.op(act, lambda: Sc.activation(t2, t1, AF.Exp, scale=-0.5), reads=[t1b], writes=[t2b])
            return t2, t2b

        def norm_mod(Aap, Bap):
            for tc in range(4):
                ts = slice(tc * 512, (tc + 1) * 512)
                bank = 6 + (tc % 2)
                for k in range(8):
                    sq, sqb = next_pt()
                    fw.op(act, lambda: Sc.activation(sq, xT[:, k, ts], AF.Square), reads=[xT_b[tc]], writes=[sqb])
                    fw.op(pe, lambda: T.matmul(psb[bank][:, :], onesB, sq, start=(k == 0), stop=(k == 7)),
                          reads=[sqb], writes=[ps_b[bank]], inc=True)
                rstd, rb = rstd_from_ss(bank, 1.0 / D)
                for k in range(8):
                    t1, t1b = next_tmp(avoid=(rb,))
                    fw.op(dve, lambda: V_.tensor_tensor(t1, xT[:, k, ts], rstd, op=ALU.mult), reads=[xT_b[tc], rb], writes=[t1b])
                    if Bap is not None:
                        fw.op(act, lambda: Sc.activation(uT[:, k, ts], t1, AF.Identity, bias=Bap[:, k:k + 1], scale=Aap[:, k:k + 1]),
                              reads=[t1b], writes=[uT_b[tc]])
                    else:
                        fw.op(act, lambda: Sc.activation(xT[:, k, ts], t1, AF.Identity, bias=zeroT, scale=Aap[:, k:k + 1]),
                              reads=[t1b], writes=[xT_b[tc]])

        def proj_fm(slab, sb, col0, ncols, dst_fn, dst_bufs, scale=None, banks=(6, 7), post=None, dst2_fn=None):
            for tc in range(4):
                bank = banks[tc % len(banks)]
                for k in range(8):
                    fw.op(pe, lambda: T.matmul(psb[bank][0:ncols, :], slab[:, k, col0:col0 + ncols], uT[:, k, tc * 512:(tc + 1) * 512],
                                               start=(k == 0), stop=(k == 7)),
                          reads=[sb, uT_b[tc]], writes=[ps_b[bank]], inc=(k == 7))
                xr = []
                if post is not None:
                    xr = post(tc, bank)
                if dst2_fn is not None:
                    fw.op(act, lambda: Sc.copy(dst_fn(tc), psb[bank][0:64, :]), reads=[ps_b[bank]] + xr, writes=dst_bufs)
                    fw.op(act, lambda: Sc.copy(dst2_fn(tc), psb[bank][64:128, :]), reads=[ps_b[bank]] + xr, writes=dst_bufs)
                elif scale is None:
                    fw.op(act, lambda: Sc.copy(dst_fn(tc), psb[bank][0:ncols, :]), reads=[ps_b[bank]] + xr, writes=dst_bufs)
                else:
                    fw.op(act, lambda: Sc.activation(dst_fn(tc), psb[bank][0:ncols, :], AF.Copy, scale=scale), reads=[ps_b[bank]] + xr, writes=dst_bufs)

        def proj_tm(slab, sb, col0, ncols, dst_fn, dst_bufs, banks=(6, 7), dtype_copy=True):
            for tb in range(16):
                bank = banks[tb % len(banks)]
                for k in range(8):
                    fw.op(pe, lambda: T.matmul(psb[bank][:, 0:ncols], uT[:, k, tb * 128:(tb + 1) * 128], slab[:, k, col0:col0 + ncols],
                                               start=(k == 0), stop=(k == 7)),
                          reads=[sb, uT_b[tb // 4]], writes=[ps_b[bank]], inc=(k == 7))
                e = dve if tb % 2 else act
                if e is dve:
                    fw.op(dve, lambda: V_.tensor_copy(dst_fn(tb), psb[bank][:, 0:ncols]), reads=[ps_b[bank]], writes=dst_bufs)
                else:
                    fw.op(act, lambda: Sc.copy(dst_fn(tb), psb[bank][:, 0:ncols]), reads=[ps_b[bank]], writes=dst_bufs)

        def attn_core(tc, k_fn, q_ap, strip_h, v_fn, o_ap, sum_ap, o_bank, s_bank, ones_ap, reads, extra=None,
                      first=True, last_grp=True):
            nkb = 4 * tc + 4
            lbanks = (0, 1)

            def qk(kb):
                bank = lbanks[kb % 2]
                dl_ = min(4 * tc - kb, 2)
                c0 = (dl_ + 3) * 128
                cl = max(kb - 4 * tc, 0) * 128
                far = dl_ >= 2
                has_extra = extra is not None and extra(kb, bank, True, cl)
                only = far and not has_extra
                fw.op(pe, lambda: T.matmul(psb[bank][:, cl:512], k_fn(kb), q_ap[:, cl:512], start=True, stop=only),
                      reads=reads, writes=[ps_b[bank]], inc=only)
                if not far:
                    fw.op(pe, lambda: T.matmul(psb[bank][:, cl:512], antiB, STR[:, strip_h, c0 + cl:c0 + 512], start=False, stop=not has_extra),
                          reads=[], writes=[ps_b[bank]], inc=not has_extra)
                if has_extra:
                    extra(kb, bank, False, cl)
                return bank, far, cl

            pend = qk(0)
            for kb in range(nkb):
                bank, far, cl = pend
                if kb + 1 < nkb:
                    pend = qk(kb + 1)
                p, pb_ = next_pt()
                if far:
                    fw.op(act, lambda: Sc.activation(p[:, cl:512], psb[bank][:, cl:512], AF.Exp, bias=c31[:, strip_h:strip_h + 1]), reads=[ps_b[bank]], writes=[pb_])
                else:
                    fw.op(act, lambda: Sc.activation(p[:, cl:512], psb[bank][:, cl:512], AF.Exp), reads=[ps_b[bank]], writes=[pb_])
                st_ = first and kb == 0
                sp_ = last_grp and kb == nkb - 1
                fw.op(pe, lambda: T.matmul(o_ap[:, cl:512], v_fn(kb), p[:, cl:512], start=st_, stop=sp_),
                      reads=reads + [pb_], writes=[ps_b[o_bank]], inc=sp_)
                fw.op(pe, lambda: T.matmul(sum_ap[:, cl:512], ones_ap, p[:, cl:512], start=st_, stop=sp_),
                      reads=[pb_], writes=[ps_b[s_bank]], inc=True)

        def recip_sum(s_bank, sum_ap, parts=128, p0=0):
            t1, t1b = next_tmp()
            t1v = t1[p0:p0 + parts, :]
            fw.op(act, lambda: Sc.activation(t1v, sum_ap, AF.Ln), reads=[ps_b[s_bank]], writes=[t1b])
            t2, t2b = next_tmp()
            t2v = t2[p0:p0 + parts, :]
            fw.op(act, lambda: Sc.activation(t2v, t1v, AF.Exp, scale=-1.0), reads=[t1b], writes=[t2b])
            return t2v, t2b

        def load_strips(heads):
            sb_ = Buf("strips")
            for h in heads:
                src = bass.AP(tensor=g_d.tensor, offset=h * 1280, ap=[[1, 128], [1, 1152]])
                fw.dma(sp, STR[:, h, :], src, reads=[gd_b], writes=[sb_])
            return sb_

        def phase_A(l):
            ak = ARENA_K
            qT = vb(ak, 4 * S).rearrange("p (h t) -> p h t", h=4)
            kTz = vb(ak + 16, 8 * S).rearrange("p (h m t) -> p h m t", h=4, m=2)
            Vt = vb(ak + 48, 16 * 512).rearrange("p (b e) -> p b e", b=16)
            q_b, k_b, v_b = Buf("qT"), Buf("kT"), Buf("Vt")
            slab, sb = load_w_slab(win_d[l][:, O_AQ:O_AQ + 512], 512)
            for h in range(4):
                proj_fm(slab, sb, h * 128, 128, lambda tc: qT[:, h, tc * 512:(tc + 1) * 512], [q_b], scale=0.125)
            fw.op(pool, lambda: G.memset(kTz[64:128, :, 0, :], 0.0), writes=[k_b])
            fw.op(pool, lambda: G.memset(kTz[0:64, :, 1, :], 0.0), writes=[k_b])
            slab, sb = load_w_slab(win_d[l][:, O_AK:O_AK + 512], 512)
            for h in range(4):
                proj_fm(slab, sb, h * 128, 128, lambda tc: kTz[0:64, h, 0, tc * 512:(tc + 1) * 512], [k_b],
                        dst2_fn=lambda tc: kTz[64:128, h, 1, tc * 512:(tc + 1) * 512])
            slab, sb = load_w_slab(win_d[l][:, O_AV:O_AV + 512], 512)
            proj_tm(slab, sb, 0, 512, lambda tb: Vt[:, tb, :], [v_b])
            sqd = [vb(ak + 64 + i, 512) for i in range(2)]
            sqd_b = [Buf("sqd0"), Buf("sqd1")]
            deferred = []
            gi = 0
            for h in range(4):
                for tc in range(4):
                    ts = slice(tc * 512, (tc + 1) * 512)
                    R = []
                    for m in range(2):
                        ob, sbk = 2 + m, 4 + m
                        attn_core(tc, lambda kb: kTz[:, h, m, kb * 128:(kb + 1) * 128], qT[:, h, ts], h,
                                  lambda kb: Vt[:, kb, h * 128:(h + 1) * 128], psb[ob][:, :], psb[sbk][:, :], ob, sbk, onesB,
                                  reads=[q_b, k_b, v_b])
                        rs, rsb = recip_sum(sbk, psb[sbk][:, :])
                        r, rb = next_tmp()
                        fw.op(dve, lambda: V_.tensor_tensor(r, psb[ob][:, :], rs, op=ALU.mult), reads=[ps_b[ob], rsb], writes=[rb])
                        R.append((r, rb))
                        if m == 0 and deferred:
                            deferred.pop()()
                    dd, ddb = next_tmp()
                    fw.op(dve, lambda: V_.scalar_tensor_tensor(dd, R[1][0], neglam[l], R[0][0], op0=ALU.mult, op1=ALU.add),
                          reads=[R[0][1], R[1][1]], writes=[ddb])
                    sq, sqb = sqd[gi % 2], sqd_b[gi % 2]
                    gi += 1
                    fw.op(act, lambda: Sc.activation(sq, dd, AF.Square), reads=[ddb], writes=[sqb])

                    def tail(dd=dd, ddb=ddb, sq=sq, sqb=sqb, h=h, ts=ts):
                        fw.op(pe, lambda: T.matmul(psb[6][:, :], onesB, sq, start=True, stop=True), reads=[sqb], writes=[ps_b[6]])
                        rstd, rb2 = rstd_from_ss(6, 1.0 / 128)
                        fw.op(dve, lambda: V_.scalar_tensor_tensor(oA[:, h, ts], dd, subg[l], rstd, op0=ALU.mult, op1=ALU.mult),
                              reads=[ddb, rb2], writes=[oA_b])
                    deferred.append(tail)
            while deferred:
                deferred.pop()()

        def phase_B(l):
            ak = ARENA_K
            ak = 90
            kvT = vb(72, S)
            kvt = vb(76, 16 * 128).rearrange("p (b r) -> p b r", b=16)
            bqT = vb(ak, 4 * S).rearrange("p (h t) -> p h t", h=4)
            iqT = vb(ak + 16, 3 * S, parts=96).rearrange("p (c t) -> p c t", c=3)
            ikT = vb(ak + 28, S, parts=96)
            scores = vf(ak + 32, S)
            MnegAll = vb(ak + 40, 8 * S).rearrange("p (u j s) -> p u j s", u=2, j=4)
            iw = vf(ak + 72, 16 * 8).rearrange("p (b h) -> p b h", b=16)
            wuv2 = vb(ak + 72.5, 4 * 128).rearrange("p (h e) -> p h e", h=4)
            caus = vf(ak + 73.5, 128)
            bis = vf(ak + 74, 16)
            ikw = vb(ak + 74.25, 8 * 96).rearrange("p (k n) -> p k n", k=8)
            bq_b, kv_b, kvt_b, iq_b, ik_b, sc_b, iw_b, wuv_b, ca_b, bis_b, ikw_b = [Buf(n) for n in
                ("bq", "kv", "kvt", "iq", "ik", "sc", "iw", "wuv", "ca", "bis", "ikw")]
            mn_bs = [[Buf("mn%d%d" % (u, j)) for j in range(4)] for u in range(2)]
            fw.dma(sp, caus, caus_d, writes=[ca_b])
            fw.op(pool, lambda: G.memset(wuv2, 0.0), writes=[wuv_b])
            for h in range(4):
                fw.dma(pool, wuv2[:, h, (h % 2) * 64:(h % 2) * 64 + 64], wuv_d[l][h], writes=[wuv_b])
            slab, sb = load_w_slab(win_d[l][:, O_BQ:O_BQ + 512], 512)
            for h in range(4):
                proj_fm(slab, sb, h * 128, 128, lambda tc: bqT[:, h, tc * 512:(tc + 1) * 512], [bq_b], scale=128 ** -0.5)
            slab, sb = load_w_slab(win_d[l][:, O_BKV:O_BKV + 424], 424)

            def kv_post(tc, bank):
                ts = slice(tc * 512, (tc + 1) * 512)
                sq, sqb = next_pt()
                fw.op(act, lambda: Sc.activation(sq, psb[bank][:, :], AF.Square), reads=[ps_b[bank]], writes=[sqb])
                fw.op(pe, lambda: T.matmul(psb[5][:, :], onesB, sq, start=True, stop=True), reads=[sqb], writes=[ps_b[5]])
                rstd, rb = rstd_from_ss(5, 1.0 / 128)
                fw.op(dve, lambda: V_.scalar_tensor_tensor(kvT[:, ts], psb[bank][:, :], kvg[l], rstd, op0=ALU.mult, op1=ALU.mult),
                      reads=[ps_b[bank], rb], writes=[kv_b])

            for tc in range(4):
                bank = 6 + (tc % 2)
                for k in range(8):
                    fw.op(pe, lambda: T.matmul(psb[bank][:, :], slab[:, k, 0:128], uT[:, k, tc * 512:(tc + 1) * 512], start=(k == 0), stop=(k == 7)),
                          reads=[sb, uT_b[tc]], writes=[ps_b[bank]], inc=(k == 7))
                kv_post(tc, bank)
            for g4 in range(4):
                bank = 6 + (g4 % 2)
                pbf = psb[bank][:, :].bitcast(BF16)
                for j in range(4):
                    tb = g4 * 4 + j
                    fw.op(pe, lambda: T.transpose(pbf[:, j * 128:(j + 1) * 128], kvT[:, tb * 128:(tb + 1) * 128], identB),
                          reads=[kv_b], writes=[ps_b[bank]], inc=(j == 3))
                fw.op(dve, lambda: V_.tensor_copy(kvt[:, g4 * 4:(g4 + 1) * 4, :], pbf[:, 0:512].rearrange("p (a b) -> p a b", a=4)),
                      reads=[ps_b[bank]], writes=[kvt_b])
            for c in range(3):
                nh = 3 if c < 2 else 2
                proj_fm(slab, sb, 128 + c * 96, nh * 32, lambda tc: iqT[0:nh * 32, c, tc * 512:(tc + 1) * 512], [iq_b])
            for j in range(3):
                fw.op(dve, lambda: V_.tensor_copy(ikw[:, :, j * 32:(j + 1) * 32], slab[:, :, 384:416]), reads=[sb], writes=[ikw_b])
            proj_fm(ikw, ikw_b, 0, 96, lambda tc: ikT[:, tc * 512:(tc + 1) * 512], [ik_b])
            proj_tm(slab, sb, 416, 8, lambda tb: iw[:, tb, :], [iw_b])

            def indexer(qb, j):
                u = (qb // 4) % 2
                Mneg = MnegAll[:, u]
                mn_b = mn_bs[u][j]
                Wd = (qb + 1) * 128
                nsc = (Wd + 511) // 512
                for sc in range(nsc):
                    cols = min(512, Wd - sc * 512)
                    cs = slice(sc * 512, sc * 512 + cols)
                    for hh in range(8):
                        c, pb = hh // 3, (hh % 3) * 32
                        bank = 6 + (hh % 2)
                        fw.op(pe, lambda: T.matmul(psb[bank][:, 0:cols], iqT[pb:pb + 32, c, qb * 128:(qb + 1) * 128], ikT[pb:pb + 32, cs],
                                                   start=True, stop=True),
                              reads=[iq_b, ik_b], writes=[ps_b[bank]])
                        fw.op(act, lambda: Sc.activation(psb[bank][:, 0:cols], psb[bank][:, 0:cols], AF.Relu), reads=[], writes=[ps_b[bank]])
                        if hh == 0:
                            fw.op(dve, lambda: V_.tensor_scalar(scores[:, cs], psb[bank][:, 0:cols], iw[:, qb, 0:1], None, op0=ALU.mult),
                                  reads=[ps_b[bank], iw_b], writes=[sc_b])
                        else:
                            fw.op(dve, lambda: V_.scalar_tensor_tensor(scores[:, cs], psb[bank][:, 0:cols], iw[:, qb, hh:hh + 1], scores[:, cs],
                                                                       op0=ALU.mult, op1=ALU.add),
                                  reads=[ps_b[bank], iw_b, sc_b], writes=[sc_b])
                dsl = slice(qb * 128, (qb + 1) * 128)
                fw.op(dve, lambda: V_.tensor_tensor(scores[:, dsl], scores[:, dsl], caus, op=ALU.add), reads=[sc_b, ca_b], writes=[sc_b])
                lo, hi, mid, cnt, w0 = (bis[:, i:i + 1] for i in range(5))
                geu = bis[:, 6:7].bitcast(U32)
                fw.op(dve, lambda: V_.tensor_reduce(hi, scores[:, 0:Wd], axis=AX.X, op=ALU.max), reads=[sc_b], writes=[bis_b])
                fw.op(dve, lambda: V_.tensor_reduce(lo, scores[:, 0:256], axis=AX.X, op=ALU.min), reads=[sc_b], writes=[bis_b])
                fw.op(dve, lambda: V_.tensor_tensor(w0, hi, lo, op=ALU.subtract), reads=[bis_b], writes=[bis_b])
                fw.op(dve, lambda: V_.tensor_scalar(w0, w0, 1.0 + 1e-5, 1e-6, op0=ALU.mult, op1=ALU.add), reads=[bis_b], writes=[bis_b])
                junk = Mneg[:, j, 0:Wd]
                for it in range(BIS_ITERS):
                    ck = 2.0 ** -(it + 1)
                    fw.op(dve, lambda: V_.scalar_tensor_tensor(mid, w0, ck, lo, op0=ALU.mult, op1=ALU.add), reads=[bis_b], writes=[bis_b])
                    fw.op(dve, lambda: V_.tensor_scalar(junk, scores[:, 0:Wd], mid, 0.0, op0=ALU.is_ge, op1=ALU.add, accum_out=cnt),
                          reads=[sc_b, bis_b], writes=[mn_b, bis_b])
                    fw.op(dve, lambda: V_.tensor_scalar(geu, cnt, 255.5, None, op0=ALU.is_ge), reads=[bis_b], writes=[bis_b])
                    fw.op(dve, lambda: V_.copy_predicated(lo, geu, mid), reads=[bis_b], writes=[bis_b])
                fw.op(dve, lambda: V_.tensor_scalar(Mneg[:, j, 0:Wd], scores[:, 0:Wd], lo, NEG, op0=ALU.is_lt, op1=ALU.mult),
                      reads=[sc_b, bis_b], writes=[mn_b])

            ohb_b = [Buf("oh%d" % i) for i in range(4)]
            for j in range(2, 4):
                indexer(j, j)
            for tc in range(4):
                ts = slice(tc * 512, (tc + 1) * 512)
                u = tc % 2
                Mneg = MnegAll[:, u]
                oh_list = []
                for h in range(4):
                    def extra(kb, bank, query_only, cl=0):
                        js = [j for j in range(4) if (4 * tc + j) >= 2 and kb <= 4 * tc + j]
                        if query_only:
                            return len(js) > 0
                        for idx, j in enumerate(js):
                            lastj = idx == len(js) - 1
                            fw.op(pe, lambda: T.matmul(psb[bank][:, j * 128:(j + 1) * 128], Mneg[:, j, kb * 128:(kb + 1) * 128], identB,
                                                       start=False, stop=lastj),
                                  reads=[mn_bs[u][j]], writes=[ps_b[bank]], inc=lastj)
                        return True
                    ob, sbk = (2, 4) if h % 2 == 0 else (3, 5)
                    attn_core(tc, lambda kb: kvT[:, kb * 128:(kb + 1) * 128], bqT[:, h, ts], 4 + h,
                              lambda kb: kvt[:, kb, :], psb[ob][:, :], psb[sbk][:, :], ob, sbk, onesB,
                              reads=[bq_b, kv_b, kvt_b], extra=extra)
                    rs, rsb = recip_sum(sbk, psb[sbk][:, :])
                    o, ob_ = vb(36 + h, 512), ohb_b[h]
                    fw.op(dve, lambda: V_.tensor_tensor(o, psb[ob][:, :], rs, op=ALU.mult), reads=[ps_b[ob], rsb], writes=[ob_, WST_b[0]])
                    oh_list.append((o, ob_))
                    if tc + 1 < 4:
                        indexer(4 * (tc + 1) + h, h)
                for pr in range(2):
                    wb_ = 6 + pr
                    for hh in range(2):
                        h = 2 * pr + hh
                        fw.op(pe, lambda: T.matmul(psb[wb_][:, :], wuv2[:, h, :], oh_list[h][0], start=(hh == 0), stop=(hh == 1)),
                              reads=[wuv_b, oh_list[h][1], WST_b[0]], writes=[ps_b[wb_]], inc=(hh == 1))
                    fw.op(act, lambda: Sc.copy(oB[:, pr, ts], psb[wb_][:, :]), reads=[ps_b[wb_]], writes=[oB_b])

        def phase_C(l):
            ak = ARENA_K
            cqT = vb(ak, 2 * S).rearrange("p (c t) -> p c t", c=2)
            ckTz = vb(ak + 8, 4 * S).rearrange("p (c h t) -> p c h t", c=2, h=2)
            cvz = vb(ak + 24, 16 * 4 * 128).rearrange("p (b c h e) -> p b c h e", b=16, c=2, h=2)
            MT = vb(ak + 40, S, parts=32)
            selc = vb(ak + 44, 32 * 128, parts=32).rearrange("p (r s) -> p r s", r=32)
            ksum = vf(ak + 52, 16).rearrange("p (c n) -> p c n", c=2)
            bm = vf(ak + 52.5, 256).rearrange("p (j n) -> p j n", j=8)
            own = vf(ak + 53.5, 256).rearrange("p (j n) -> p j n", j=8)
            gm = vf(ak + 54.5, 32)
            m8 = vf(ak + 54.75, 8)
            selo = vf(ak + 55, 32)
            Mt = vb(ak + 55.25, 32)
            kmZ = vb(ak + 55.5, 32).rearrange("p (c h n) -> p c h n", c=2, h=2)
            onesz = vb(ak + 56, 256).rearrange("p (h e) -> p h e", h=2)
            cq_b, ck_b, cv_b, mt_b, sel_b, ks_b, km_b, bm_b, gm_b, oz_b = [Buf(n) for n in ("cq", "ck", "cv", "MT", "sel", "ks", "km", "bm", "gm", "oz")]
            fw.dma(pool, selc, sel_d.rearrange("p (r s) -> p r s", r=32), writes=[sel_b], max_dma_last_dim=2048)
            fw.dma(sp, bm, bm_d.partition_broadcast(128).rearrange("p (j n) -> p j n", j=8), writes=[bm_b])
            fw.dma(sp, own, own_d.partition_broadcast(128).rearrange("p (j n) -> p j n", j=8), writes=[bm_b])
            fw.op(pool, lambda: G.memset(ckTz[64:128, :, 0, :], 0.0), writes=[ck_b])
            fw.op(pool, lambda: G.memset(ckTz[0:64, :, 1, :], 0.0), writes=[ck_b])
            fw.op(pool, lambda: G.memset(cvz, 0.0), writes=[cv_b])
            fw.op(pool, lambda: G.memset(onesz, 0.0), writes=[oz_b])
            fw.op(pool, lambda: G.memset(onesz[:, 0, 0:64], 1.0), writes=[oz_b])
            fw.op(pool, lambda: G.memset(onesz[:, 1, 64:128], 1.0), writes=[oz_b])
            slab, sb = load_w_slab(win_d[l][:, O_CQ:O_CQ + 512], 512)
            for c in range(2):
                proj_fm(slab, sb, c * 128, 128, lambda tc: cqT[:, c, tc * 512:(tc + 1) * 512], [cq_b], scale=0.125)

            for c in range(2):
                def post(tc, bank):
                    fw.op(dve, lambda: V_.tensor_reduce(ksum[:, c, 2 * tc:2 * tc + 2], psb[bank][:, :].rearrange("p (b s) -> p b s", b=2),
                                                        axis=AX.X, op=ALU.add),
                          reads=[ps_b[bank]], writes=[ks_b])
                    return [ks_b]
                proj_fm(slab, sb, 256 + c * 128, 128, lambda tc: ckTz[0:64, c, 0, tc * 512:(tc + 1) * 512], [ck_b], post=post,
                        dst2_fn=lambda tc: ckTz[64:128, c, 1, tc * 512:(tc + 1) * 512])
            fw.op(dve, lambda: V_.memset(kmZ, 0.0), writes=[km_b])
            for hh in range(2):
                pr_ = slice(hh * 64, hh * 64 + 64)
                fw.op(dve, lambda: V_.tensor_scalar(kmZ[pr_, :, hh, :], ksum[pr_, :, :], 1.0 / 256, None, op0=ALU.mult), reads=[ks_b], writes=[km_b])
            slab, sb = load_w_slab(win_d[l][:, O_CV:O_CV + 256], 256)
            for tb in range(16):
                bank = 6 + (tb % 2)
                for k in range(8):
                    fw.op(pe, lambda: T.matmul(psb[bank][:, 0:256], uT[:, k, tb * 128:(tb + 1) * 128], slab[:, k, 0:256], start=(k == 0), stop=(k == 7)),
                          reads=[sb, uT_b[tb // 4]], writes=[ps_b[bank]], inc=(k == 7))
                src = psb[bank][:, 0:256].rearrange("p (c h e) -> p c h e", c=2, h=2)
                if tb % 2:
                    fw.op(dve, lambda: V_.tensor_copy(cvz[:, tb, :, 0, 0:64], src[:, :, 0, :]), reads=[ps_b[bank]], writes=[cv_b])
                    fw.op(dve, lambda: V_.tensor_copy(cvz[:, tb, :, 1, 64:128], src[:, :, 1, :]), reads=[ps_b[bank]], writes=[cv_b])
                else:
                    fw.op(act, lambda: Sc.copy(cvz[:, tb, :, 0, 0:64], src[:, :, 0, :]), reads=[ps_b[bank]], writes=[cv_b])
                    fw.op(act, lambda: Sc.copy(cvz[:, tb, :, 1, 64:128], src[:, :, 1, :]), reads=[ps_b[bank]], writes=[cv_b])
            for qb in range(16):
                jb = qb // 2
                bank = 6 + (qb % 2)
                for h in range(4):
                    c = h // 2
                    fw.op(pe, lambda: T.matmul(psb[bank][:, h * 8:(h + 1) * 8], cqT[:, c, qb * 128:(qb + 1) * 128], kmZ[:, c, h % 2, :],
                                               start=True, stop=True),
                          reads=[cq_b, km_b], writes=[ps_b[bank]], inc=(h == 3))
                fw.op(dve, lambda: V_.tensor_tensor(gm, psb[bank][:, 0:32], bm[:, jb, :], op=ALU.add), reads=[ps_b[bank], bm_b], writes=[gm_b])
                for h in range(4):
                    fw.op(dve, lambda: V_.max(m8, gm[:, h * 8:(h + 1) * 8]), reads=[gm_b], writes=[gm_b])
                    fw.op(dve, lambda: V_.tensor_scalar(selo[:, h * 8:(h + 1) * 8], gm[:, h * 8:(h + 1) * 8], m8[:, 2:3], None, op0=ALU.is_ge),
                          reads=[gm_b], writes=[gm_b])
                fw.op(dve, lambda: V_.tensor_tensor(selo, selo, own[:, jb, :], op=ALU.max), reads=[gm_b, bm_b], writes=[gm_b])
                fw.op(dve, lambda: V_.tensor_scalar(Mt, selo, -NEG, NEG, op0=ALU.mult, op1=ALU.add), reads=[gm_b], writes=[gm_b])
                pbf = psb[bank][:, :].bitcast(BF16)
                fw.op(pe, lambda: T.transpose(pbf[0:32, 0:128], Mt, identB), reads=[gm_b], writes=[ps_b[bank]])
                fw.op(act, lambda: Sc.copy(MT[:, qb * 128:(qb + 1) * 128], pbf[0:32, 0:128]), reads=[ps_b[bank]], writes=[mt_b])
            for c in range(2):
                for tc in range(4):
                    ts = slice(tc * 512, (tc + 1) * 512)
                    for hh in range(2):
                        h = 2 * c + hh

                        def extra(kb, bank, query_only, cl=0):
                            if query_only:
                                return True
                            fw.op(pe, lambda: T.matmul(psb[bank][:, cl:512], selc[:, h * 8 + kb // 2, :], MT[:, tc * 512 + cl:(tc + 1) * 512], start=False, stop=True),
                                  reads=[sel_b, mt_b], writes=[ps_b[bank]], inc=True)
                            return True
                        ob, sbk = (2, 4) if tc % 2 == 0 else (3, 5)
                        attn_core(tc, lambda kb: ckTz[:, c, hh, kb * 128:(kb + 1) * 128], cqT[:, c, ts], 8 + h,
                                  lambda kb: cvz[:, kb, c, hh, :], psb[ob][:, :], psb[sbk][:, :], ob, sbk, onesz[:, hh, :],
                                  reads=[cq_b, ck_b, cv_b, oz_b], extra=extra, first=(hh == 0), last_grp=(hh == 1))
                    rs, rsb = recip_sum(sbk, psb[sbk][:, :])
                    fw.op(dve, lambda: V_.tensor_tensor(oC[:, c, ts], psb[ob][:, :], rs, op=ALU.mult), reads=[ps_b[ob], rsb], writes=[oC_b])

        def phase_M(l, b):
            P = PRM[(l, b)]
            wg = [vb(36 + 8 * i, 8 * 384).rearrange("p (k n) -> p k n", k=8) for i in range(2)]
            wbr = [vb(36 + 8 * i + 6, 8 * 128).rearrange("p (k n) -> p k n", k=8) for i in range(2)]
            srcs = [(oA, oA_b, 4, wbra_d, 0), (oB, oB_b, 2, wbrb_d, 4), (oC, oC_b, 2, wbrc_d, 6)]
            def load_m(mf):
                i = mf % 2
                for j in range(3):
                    c0 = O_G + j * 1024 + mf * 128
                    fw.dma(pool, wg[i][:, :, j * 128:(j + 1) * 128], win_d[l][:, c0:c0 + 128].rearrange("(k p) n -> p k n", p=128), writes=[WST_b[i]])
                for (o_, ob, nk, wd, k0) in srcs:
                    fw.dma(pool, wbr[i][:, k0:k0 + nk, :], wd[l][:, mf * 128:(mf + 1) * 128].rearrange("(k p) n -> p k n", p=128), writes=[WST_b[i]])

            load_m(0)
            for mf in range(8):
                i = mf % 2
                wgi, wbi, wb_ = wg[i], wbr[i], WST_b[i]
                if mf + 1 < 8:
                    load_m(mf + 1)
                for tc in range(4):
                    ts = slice(tc * 512, (tc + 1) * 512)
                    sig = []
                    for bi in range(3):
                        gbank = (0, 1, 6)[bi]
                        for k in range(8):
                            fw.op(pe, lambda: T.matmul(psb[gbank][:, :], wgi[:, k, bi * 128:(bi + 1) * 128], uT[:, k, ts], start=(k == 0), stop=(k == 7)),
                                  reads=[wb_, uT_b[tc]], writes=[ps_b[gbank]], inc=(k == 7))
                        sg, sgb = next_pt()
                        fw.op(act, lambda: Sc.activation(sg, psb[gbank][:, :], AF.Sigmoid, bias=gateb[l][:, bi * 8 + mf:bi * 8 + mf + 1]),
                              reads=[ps_b[gbank]], writes=[sgb])
                        sig.append((sg, sgb))
                    terms = []
                    for bi, (o_, ob, nk, wd, k0) in enumerate(srcs):
                        ybank = (2, 3, 4)[bi]
                        for k in range(nk):
                            fw.op(pe, lambda: T.matmul(psb[ybank][:, :], wbi[:, k0 + k, :], o_[:, k, ts], start=(k == 0), stop=(k == nk - 1)),
                                  reads=[wb_, ob], writes=[ps_b[ybank]], inc=(k == nk - 1))
                        tm, tmb = next_tmp()
                        fw.op(dve, lambda: V_.tensor_tensor(tm, psb[ybank][:, :], sig[bi][0], op=ALU.mult), reads=[ps_b[ybank], sig[bi][1]], writes=[tmb])
                        terms.append((tm, tmb))
                    fw.op(pool, lambda: G.tensor_tensor(terms[0][0], terms[0][0], terms[1][0], op=ALU.add), reads=[terms[1][1]], writes=[terms[0][1]])
                    fw.op(pool, lambda: G.tensor_tensor(merged[:, mf, ts], terms[0][0], terms[2][0], op=ALU.add),
                          reads=[terms[0][1], terms[2][1]], writes=[merged_b[tc]])
            for half in range(2):
                slab, sb = load_w_slab(wo_d[l][:, half * 512:(half + 1) * 512], 512)
                for f4 in range(4):
                    f = half * 4 + f4
                    for tc in range(4):
                        ts = slice(tc * 512, (tc + 1) * 512)
                        bank = (f4 * 4 + tc) % 8
                        for k in range(8):
                            fw.op(pe, lambda: T.matmul(psb[bank][:, :], slab[:, k, f4 * 128:(f4 + 1) * 128], merged[:, k, ts], start=(k == 0), stop=(k == 7)),
                                  reads=[sb, merged_b[tc]], writes=[ps_b[bank]], inc=(k == 7))
                        fw.op(dve, lambda: V_.scalar_tensor_tensor(xT[:, f, ts], psb[bank][:, :], P[:, 2, f:f + 1], xT[:, f, ts], op0=ALU.mult, op1=ALU.add),
                              reads=[ps_b[bank], xT_b[tc]], writes=[xT_b[tc]])

        ffw1 = [vb(136 + 32 * i, 8 * 1024).rearrange("p (k n) -> p k n", k=8) for i in range(2)]
        ffw2 = [vb(152 + 32 * i, 8 * 1024).rearrange("p (k n) -> p k n", k=8) for i in range(2)]
        ffw_b = [Buf("ffw0"), Buf("ffw1")]

        def load_f(l, cg):
            i = cg % 2
            for hf in range(2):
                fw.dma(pool, ffw1[i][:, :, hf * 512:(hf + 1) * 512],
                       wff1_d[l][:, cg * 1024 + hf * 512:cg * 1024 + (hf + 1) * 512].rearrange("(k p) n -> p k n", p=128), writes=[ffw_b[i]])
            for hf in range(2):
                fw.dma(pool, ffw2[i][:, :, hf * 512:(hf + 1) * 512],
                       wff2_d[l][cg * 1024:(cg + 1) * 1024, hf * 512:(hf + 1) * 512].rearrange("(k p) n -> p k n", p=128), writes=[ffw_b[i]])

        def phase_F(l, b):
            P = PRM[(l, b)]
            w1, w2, w_b = ffw1, ffw2, ffw_b
            hT = [vb(56 + i, 512) for i in range(8)]
            h_b = Buf("hT")
            rT = [TMP[4 + i] for i in range(4)]
            r_b = [TMP_b[4 + i] for i in range(4)]
            for cg in range(4):
                i = cg % 2
                if 1 <= cg and cg + 1 < 4:
                    load_f(l, cg + 1)
                for tc in range(4):
                    ts = slice(tc * 512, (tc + 1) * 512)
                    for c in range(8):
                        bank = c % 2
                        for k in range(8):
                            fw.op(pe, lambda: T.matmul(psb[bank][:, :], w1[i][:, k, c * 128:(c + 1) * 128], uT[:, k, ts], start=(k == 0), stop=(k == 7)),
                                  reads=[w_b[i], uT_b[tc]], writes=[ps_b[bank]], inc=(k == 7))
                        r, rb = rT[c % 4], r_b[c % 4]
                        fw.op(act, lambda: Sc.activation(r, psb[bank][:, :], AF.Relu), reads=[ps_b[bank]], writes=[rb])
                        fw.op(pool, lambda: G.tensor_tensor(hT[c], r, r, op=ALU.mult), reads=[rb], writes=[h_b])
                    for f in range(8):
                        bank = 2 + (f % 6)
                        for c in range(8):
                            fw.op(pe, lambda: T.matmul(psb[bank][:, :], w2[i][:, c, f * 128:(f + 1) * 128], hT[c], start=(c == 0), stop=(c == 7)),
                                  reads=[w_b[i], h_b], writes=[ps_b[bank]], inc=(c == 7))
                        fw.op(dve, lambda: V_.scalar_tensor_tensor(xT[:, f, ts], psb[bank][:, :], P[:, 5, f:f + 1], xT[:, f, ts], op0=ALU.mult, op1=ALU.add),
                              reads=[ps_b[bank], xT_b[tc]], writes=[xT_b[tc]])

        def load_x(b):
            xin = [vf(56 + 4 * i, 1024) for i in range(2)]
            xin_b = [Buf("xin0"), Buf("xin1")]
            for tb in range(16):
                i = tb % 2
                fw.dma(sp, xin[i], x_d[b, tb * 128:(tb + 1) * 128, :], writes=[xin_b[i]])
                for half in range(2):
                    bank = 2 * i + half
                    for j in range(4):
                        kc = half * 4 + j
                        fw.op(pe, lambda: T.transpose(psb[bank][:, j * 128:(j + 1) * 128], xin[i][:, kc * 128:(kc + 1) * 128], identF),
                              reads=[xin_b[i]], writes=[ps_b[bank]], inc=(j == 3))
                    dst = xT[:, half * 4:(half + 1) * 4, tb * 128:(tb + 1) * 128]
                    src = psb[bank][:, :].rearrange("p (a c) -> p a c", a=4)
                    if half == 0:
                        fw.op(act, lambda: Sc.copy(dst, src), reads=[ps_b[bank]], writes=[xT_b[tb // 4]])
                    else:
                        fw.op(dve, lambda: V_.tensor_copy(dst, src), reads=[ps_b[bank]], writes=[xT_b[tb // 4]])

        def store_out(b):
            ot = [vf(56 + 4 * i, 1024) for i in range(2)]
            ot_b = [Buf("ot0"), Buf("ot1")]
            toks = []
            for tb in range(16):
                i = tb % 2
                for half in range(2):
                    bank = 2 * i + half
                    for j in range(4):
                        kc = half * 4 + j
                        fw.op(pe, lambda: T.transpose(psb[bank][:, j * 128:(j + 1) * 128], xT[:, kc, tb * 128:(tb + 1) * 128], identF),
                              reads=[xT_b[tb // 4]], writes=[ps_b[bank]], inc=(j == 3))
                    dst = ot[i][:, half * 512:(half + 1) * 512]
                    if half == 0:
                        fw.op(act, lambda: Sc.copy(dst, psb[bank][:, :]), reads=[ps_b[bank]], writes=[ot_b[i]])
                    else:
                        fw.op(dve, lambda: V_.tensor_copy(dst, psb[bank][:, :]), reads=[ps_b[bank]], writes=[ot_b[i]])
                toks.append(fw.dma(sp, out_d[b, tb * 128:(tb + 1) * 128, :], ot[i], reads=[ot_b[i]]))
            return toks

        def spill_x():
            for k in range(8):
                fw.dma(sp, xsp_d[:, k, :], xT[:, k, :], reads=xT_b, writes=[xsp_b])

        def reload_x():
            for k in range(8):
                fw.dma(sp, xT[:, k, :], xsp_d[:, k, :], reads=[xsp_b], writes=xT_b)

        out_toks = []
        for b in range(nseq):
            load_x(b)
            fw.barrier()
            P0 = PRM[(0, b)]
            norm_mod(P0[:, 0, :], P0[:, 1, :])
            if b == 0:
                dump("u0", uT, [128, 8, S], BF16, uT_b)
            spill_x()
            fw.barrier()
            for l in range(L):
                if stop_after != "pre" and "A" not in SKIP:
                    load_strips(range(0, 4))
                    fw.barrier()
                    phase_A(l)
                    fw.barrier()
                    if b == 0 and l == 0:
                        dump("oA", oA, [128, 4, S], BF16, [oA_b])
                if stop_after not in ("pre", "A") and "B" not in SKIP:
                    load_strips(range(4, 8))
                    fw.barrier()
                    phase_B(l)
                    fw.barrier()
                    if b == 0 and l == 0:
                        dump("oB", oB, [128, 2, S], BF16, [oB_b])
                if stop_after not in ("pre", "A", "B"):
                    load_strips(range(8, 12))
                    fw.barrier()
                    phase_C(l)
                    fw.barrier()
                    if b == 0 and l == 0:
                        dump("oC", oC, [128, 2, S], BF16, [oC_b])
                reload_x()
                fw.barrier()
                if stop_after not in ("pre", "A", "B", "C"):
                    phase_M(l, b)
                    fw.barrier()
                    if b == 0 and l == 0:
                        dump("x1", xT, [128, 8, S], F32, xT_b)
                    P = PRM[(l, b)]
                    load_f(l, 0)
                    load_f(l, 1)
                    norm_mod(P[:, 3, :], P[:, 4, :])
                    fw.barrier()
                    phase_F(l, b)
                    fw.barrier()
                    if b == 0 and l == 0:
                        dump("x2", xT, [128, 8, S], F32, xT_b)
                if l + 1 < L:
                    Pn = PRM[(l + 1, b)]
                    norm_mod(Pn[:, 0, :], Pn[:, 1, :])
                    spill_x()
                    fw.barrier()
            norm_mod(nfin, None)
            fw.barrier()
            out_toks += store_out(b)
            fw.barrier()
        fw._wait(sp, out_toks)
        fw.barrier()
        stats = {e.name: e.ninst for e in fw.engs}
    return nc, dbg_out, stats


_CONSTS = None


def _consts():
    global _CONSTS
    if _CONSTS is None:
        ident = np.eye(128, dtype=np.float32)
        anti = np.ascontiguousarray(ident[::-1])
        caus = np.where(np.arange(128)[None, :] <= np.arange(128)[:, None], 0.0, -1e30).astype(np.float32)
        sel = np.zeros((32, 32, 128), np.float32)
        for r in range(32):
            sel[r, r, :] = 1.0
        bm = np.zeros((8, 4, 8), np.float32)
        own = np.zeros((8, 4, 8), np.float32)
        for j in range(8):
            bm[j, :, j:] = -1e30
            own[j, :, j] = 1.0
        _CONSTS = dict(k_ident=ident, k_anti=anti, k_onehot=_t5_onehot(), k_caus=caus,
                       k_sel=sel.reshape(32, 32 * 128), k_bm=bm.reshape(-1), k_own=own.reshape(-1))
    return _CONSTS


def make_in_maps(inputs, n_cores, nseq, nlayer=2):
    f = lambda a: np.ascontiguousarray(np.asarray(a, dtype=np.float32))
    L = nlayer
    x = f(inputs["x"])
    c = f(inputs["c"])
    shared = dict(
        rel_bias=f(inputs["rel_bias"]),
        ada_w=f(inputs["ada_w"])[:L],
        ada_bT=f(f(inputs["ada_b"])[:L].reshape(L, 48, 128).transpose(0, 2, 1)),
        norm_mixT=f(f(inputs["norm_mix"])[:L].reshape(L, 8, 128).transpose(0, 2, 1)),
        w_in=f(inputs["w_in"])[:L],
        gate_bT=f(f(inputs["gate_b"])[:L].reshape(L, 24, 128).transpose(0, 2, 1)),
        diff_lambda=f(inputs["diff_lambda"])[:L].reshape(L, 256),
        diff_subln=f(inputs["diff_subln"])[:L].reshape(L, 128, 1),
        dsa_kv_norm=f(inputs["dsa_kv_norm"])[:L].reshape(L, 128, 1),
        dsa_w_uv=f(inputs["dsa_w_uv"])[:L],
        w_br_a=f(inputs["w_br_a"])[:L], w_br_b=f(inputs["w_br_b"])[:L], w_br_c=f(inputs["w_br_c"])[:L],
        w_o=f(inputs["w_o"])[:L],
        norm_mlpT=f(f(inputs["norm_mlp"])[:L].reshape(L, 8, 128).transpose(0, 2, 1)),
        w_ff1=f(inputs["w_ff1"])[:L], w_ff2=f(inputs["w_ff2"])[:L],
        norm_finalT=f(f(inputs["norm_final"]).reshape(8, 128).T),
    )
    shared.update(_consts())
    maps = []
    for i in range(n_cores):
        m = dict(shared)
        m["x"] = f(x[i * nseq:(i + 1) * nseq])
        m["c_lay"] = f(c[i * nseq:(i + 1) * nseq].reshape(nseq, 8, 128).transpose(2, 1, 0))
        maps.append(m)
    return maps


def kernel(**inputs):
    n_cores, nseq = 8, 2
    nc, _, _ = build(nseq=nseq, nlayer=2)
    maps = make_in_maps(inputs, n_cores, nseq)
    res = run_bass_kernel_spmd(nc, maps, core_ids=list(range(n_cores)))
    out = np.concatenate([np.asarray(r["out"]) for r in res.results], axis=0)
    return out.astype(np.float32)
```
